# Optimizing a Trainium2 kernel written in Bass

```python
import math
import jax, jax.numpy as jnp
from jax import lax
import numpy as np

D_MODEL = 1024
BATCH = 16
SEQ = 2048
DEPTH = 2

D_MIX = D_MODEL
LRU_WIDTH = D_MIX // 4
LRU_BLOCKS = 4
LRU_BLOCK = LRU_WIDTH // LRU_BLOCKS
CONV_WIDTH = 4
LRU_C = 8.0
HG_HEADS = 4
HG_DK = 64
HG_DV = 64
HG_KDIM = HG_HEADS * HG_DK
HG_WIDTH = HG_HEADS * HG_DV
HG_CHUNK = 64
MLA_HEADS = 8
MLA_NOPE = 64
MLA_ROPE = 32
MLA_V = 64
MLA_QK = MLA_NOPE + MLA_ROPE
MLA_Q_RANK = 256
MLA_KV_RANK = 128
MLA_WIDTH = MLA_HEADS * MLA_V
ATTN_BLOCK = 128
ROPE_THETA = 10000.0
D_FF = 2752
N_EXPERTS = 8
TOP_K = 2
D_EXPERT = 3584
N_DENSE = (DEPTH + 1) // 2
N_MOE = DEPTH // 2
EPS = 1e-6
IN_SIZES = (LRU_WIDTH, LRU_WIDTH, HG_KDIM, HG_KDIM, HG_KDIM, HG_WIDTH, HG_WIDTH, MLA_Q_RANK, MLA_KV_RANK, MLA_ROPE)
D_IN = LRU_WIDTH * 2 + HG_KDIM * 3 + HG_WIDTH * 2 + MLA_Q_RANK + MLA_KV_RANK + MLA_ROPE

kernel_name = 'hybrid_rglru_hgrn2_mla_moe_encoder'


def rms_norm(x, g):
    xf = x.astype(jnp.float32)
    y = xf * lax.rsqrt(jnp.mean(xf * xf, axis=-1, keepdims=True) + EPS)
    return (y * g.astype(jnp.float32)).astype(x.dtype)


def split_columns(proj):
    outs, start = [], 0
    for size in IN_SIZES:
        outs.append(proj[..., start:start + size])
        start += size
    return outs


def centred_dwconv(x, w, b):
    S = x.shape[1]
    pad_l = CONV_WIDTH // 2
    xp = jnp.pad(x, ((0, 0), (pad_l, CONV_WIDTH - 1 - pad_l), (0, 0)))
    y = b
    for k in range(CONV_WIDTH):
        y = y + xp[:, k:k + S] * w[k]
    return y


def linear_scan(a, b, reverse):
    def combine(l, r):
        return r[0] * l[0], r[0] * l[1] + r[1]
    return lax.associative_scan(combine, (a, b), axis=1, reverse=reverse)[1]


def rg_lru(x, w_a, b_a, w_x, b_x, lam, reverse):
    B, S, W = x.shape
    f32 = jnp.float32
    xb = x.reshape(B, S, LRU_BLOCKS, LRU_BLOCK)
    r = jax.nn.sigmoid(jnp.einsum('bsnd,nde->bsne', xb, w_a.astype(f32)).reshape(B, S, W) + b_a.astype(f32))
    i = jax.nn.sigmoid(jnp.einsum('bsnd,nde->bsne', xb, w_x.astype(f32)).reshape(B, S, W) + b_x.astype(f32))
    log_a = -LRU_C * r * jax.nn.softplus(-lam.astype(f32))
    a = jnp.exp(log_a)
    mult = jnp.sqrt(-jnp.expm1(2.0 * log_a))
    return linear_scan(a, mult * (i * x), reverse)


def rglru_mixer(xa, gate, conv_w, conv_b, w_a, b_a, w_x, b_x, lam):
    xc = centred_dwconv(xa, conv_w, conv_b).astype(jnp.float32)
    h = (rg_lru(xc, w_a[0], b_a[0], w_x[0], b_x[0], lam[0], False)
         + rg_lru(xc, w_a[1], b_a[1], w_x[1], b_x[1], lam[1], True))
    return (h * jax.nn.gelu(gate.astype(jnp.float32))).astype(xa.dtype)


def gla_chunked(q, k, v, logf):
    B, H, S, dk = q.shape
    dv = v.shape[-1]
    n = S // HG_CHUNK

    def to_chunks(t):
        return jnp.moveaxis(t.reshape(B, H, n, HG_CHUNK, t.shape[-1]), 2, 0)

    lower = jnp.tril(jnp.ones((HG_CHUNK, HG_CHUNK), dtype=bool))[:, :, None]

    def step(state, inp):
        qc, kc, vc, gc = inp
        bcum = jnp.cumsum(gc, axis=2)
        b_last = bcum[:, :, -1:, :]
        o_inter = jnp.einsum('bhtd,bhde->bhte', qc * jnp.exp(bcum), state)
        diff = bcum[:, :, :, None, :] - bcum[:, :, None, :, :]
        decay = jnp.exp(jnp.where(lower, diff, -jnp.inf))
        scores = jnp.einsum('bhtd,bhsd,bhtsd->bhts', qc, kc, decay)
        o = o_inter + jnp.einsum('bhts,bhse->bhte', scores, vc)
        new_state = (state * jnp.exp(b_last[:, :, 0, :, None])
                     + jnp.einsum('bhsd,bhse->bhde', kc * jnp.exp(b_last - bcum), vc))
        return new_state, o

    init = jnp.zeros((B, H, dk, dv), jnp.float32)
    _, o = lax.scan(step, init, (to_chunks(q), to_chunks(k), to_chunks(v), to_chunks(logf)))
    return jnp.moveaxis(o, 0, 2).reshape(B, H, S, dv)


def hgrn2_mixer(q_in, ff_in, fb_in, i_in, g_in, lb, norm_g):
    B, S, _ = q_in.shape
    f32 = jnp.float32

    def heads(t):
        return t.astype(f32).reshape(B, S, HG_HEADS, -1).transpose(0, 2, 1, 3)

    def gates(z, lb_dir):
        f = lb_dir.astype(f32) + (1.0 - lb_dir.astype(f32)) * jax.nn.sigmoid(z.astype(f32))
        return heads(jnp.log(f)), heads(1.0 - f)

    q = heads(jax.nn.silu(q_in.astype(f32)) * HG_DK ** -0.5)
    v = heads(i_in)
    logf_f, k_f = gates(ff_in, lb[0])
    logf_b, k_b = gates(fb_in, lb[1])
    rev = lambda t: jnp.flip(t, axis=2)
    o_fwd = gla_chunked(q, k_f, v, logf_f)
    o_bwd = rev(gla_chunked(rev(q), rev(k_b), rev(v), rev(logf_b)))
    o = (o_fwd + o_bwd).transpose(0, 2, 1, 3)
    o = rms_norm(o, norm_g).reshape(B, S, HG_WIDTH)
    return (o * jax.nn.silu(g_in.astype(f32))).astype(q_in.dtype)


def apply_rope_tail(t, cos, sin):
    t_nope, t_rope = t[..., :MLA_NOPE], t[..., MLA_NOPE:]
    half = MLA_ROPE // 2
    x1, x2 = t_rope[..., :half].astype(jnp.float32), t_rope[..., half:].astype(jnp.float32)
    rot = jnp.concatenate([x1 * cos - x2 * sin, x2 * cos + x1 * sin], axis=-1).astype(t.dtype)
    return jnp.concatenate([t_nope, rot], axis=-1)


def dense_attention_blocked(q, k, v):
    B, S, H, dq = q.shape
    n = S // ATTN_BLOCK
    scale = dq ** -0.5
    qb = jnp.moveaxis(q.reshape(B, n, ATTN_BLOCK, H, dq), 1, 0)

    def one_block(q_blk):
        s = jnp.einsum('bqhd,bkhd->bhqk', q_blk, k).astype(jnp.float32) * scale
        p = jax.nn.softmax(s, axis=-1).astype(v.dtype)
        return jnp.einsum('bhqk,bkhd->bqhd', p, v)

    o = lax.map(one_block, qb)
    return jnp.moveaxis(o, 0, 1).reshape(B, S, H, v.shape[-1])


def mla_mixer(c_q, c_kv, k_rope, positions, q_norm_g, w_uq, kv_norm_g, w_ukv, qk_g_q, qk_g_k):
    B, S, _ = c_q.shape
    q = (rms_norm(c_q, q_norm_g) @ w_uq).reshape(B, S, MLA_HEADS, MLA_QK)
    kv = (rms_norm(c_kv, kv_norm_g) @ w_ukv).reshape(B, S, MLA_HEADS, MLA_NOPE + MLA_V)
    k_nope, v = kv[..., :MLA_NOPE], kv[..., MLA_NOPE:]
    k = jnp.concatenate([k_nope, jnp.broadcast_to(k_rope[:, :, None, :], (B, S, MLA_HEADS, MLA_ROPE))], axis=-1)
    q = rms_norm(q, qk_g_q)
    k = rms_norm(k, qk_g_k)
    inv_freq = ROPE_THETA ** (-jnp.arange(0, MLA_ROPE // 2, dtype=jnp.float32) * (2.0 / MLA_ROPE))
    ang = positions.astype(jnp.float32)[..., None] * inv_freq
    cos, sin = jnp.cos(ang)[:, :, None, :], jnp.sin(ang)[:, :, None, :]
    q = apply_rope_tail(q, cos, sin)
    k = apply_rope_tail(k, cos, sin)
    o = dense_attention_blocked(q, k, v)
    return o.reshape(B, S, MLA_WIDTH)


def swiglu_dense(h, w_gate_up, w_down):
    gu = h @ w_gate_up
    g, u = gu[..., :D_FF], gu[..., D_FF:]
    return (jax.nn.silu(g) * u) @ w_down


def moe_swiglu(h, router, w1, w3, w2):
    B, S, D = h.shape
    ht = h.reshape(B * S, D)
    logits = (ht @ router).astype(jnp.float32)
    top_v, top_i = lax.top_k(logits, TOP_K)
    top_w = jax.nn.softmax(top_v, axis=-1)
    combine = jnp.sum(jax.nn.one_hot(top_i, N_EXPERTS, dtype=jnp.float32) * top_w[..., None], axis=1).astype(h.dtype)
    y = jnp.zeros_like(ht)
    for e in range(N_EXPERTS):
        act = jax.nn.silu(ht @ w1[e]) * (ht @ w3[e])
        y = y + (act * combine[:, e:e + 1]) @ w2[e]
    return y.reshape(B, S, D)


def setup_inputs(seed: int = 0) -> dict:
    key = jax.random.key(seed)
    ks = jax.random.split(key, 32)
    f32 = jnp.float32
    nrm = lambda k, shape, scale: jax.random.normal(k, shape, f32) * scale
    gain = lambda k, shape: 1.0 + 0.02 * jax.random.normal(k, shape, f32)
    u = jax.random.uniform(ks[10], (DEPTH, 2, LRU_WIDTH), f32, minval=0.9, maxval=0.999)
    a0 = u ** (1.0 / LRU_C)
    lam = jnp.log(a0) - jnp.log1p(-a0)
    positions = jnp.arange(SEQ, dtype=jnp.int32)[None, :] + jax.random.randint(ks[1], (BATCH, 1), 0, 4096, dtype=jnp.int32)
    return {
        'x': jax.random.normal(ks[0], (BATCH, SEQ, D_MODEL), f32),
        'positions': positions,
        'norm_mix': gain(ks[2], (DEPTH, D_MODEL)),
        'w_in': nrm(ks[3], (DEPTH, D_MODEL, D_IN), D_MODEL ** -0.5),
        'conv_w': nrm(ks[4], (DEPTH, CONV_WIDTH, LRU_WIDTH), CONV_WIDTH ** -0.5),
        'conv_b': nrm(ks[5], (DEPTH, LRU_WIDTH), 0.01),
        'lru_wa': nrm(ks[6], (DEPTH, 2, LRU_BLOCKS, LRU_BLOCK, LRU_BLOCK), LRU_BLOCK ** -0.5),
        'lru_ba': nrm(ks[7], (DEPTH, 2, LRU_WIDTH), 0.01),
        'lru_wx': nrm(ks[8], (DEPTH, 2, LRU_BLOCKS, LRU_BLOCK, LRU_BLOCK), LRU_BLOCK ** -0.5),
        'lru_bx': nrm(ks[9], (DEPTH, 2, LRU_WIDTH), 0.01),
        'lru_lam': lam,
        'out_g_a': gain(ks[11], (DEPTH, LRU_WIDTH)),
        'hg_lb_logits': nrm(ks[12], (DEPTH, 2, HG_KDIM), 0.1),
        'hg_norm_g': gain(ks[13], (DEPTH, HG_DV)),
        'mla_q_norm': gain(ks[14], (DEPTH, MLA_Q_RANK)),
        'mla_w_uq': nrm(ks[15], (DEPTH, MLA_Q_RANK, MLA_HEADS * MLA_QK), MLA_Q_RANK ** -0.5),
        'mla_kv_norm': gain(ks[16], (DEPTH, MLA_KV_RANK)),
        'mla_w_ukv': nrm(ks[17], (DEPTH, MLA_KV_RANK, MLA_HEADS * (MLA_NOPE + MLA_V)), MLA_KV_RANK ** -0.5),
        'qk_norm_q': gain(ks[18], (DEPTH, MLA_QK)),
        'qk_norm_k': gain(ks[19], (DEPTH, MLA_QK)),
        'out_g_c': gain(ks[20], (DEPTH, MLA_WIDTH)),
        'w_out': nrm(ks[21], (DEPTH, D_MIX, D_MODEL), D_MIX ** -0.5),
        'norm_ffn': gain(ks[22], (DEPTH, D_MODEL)),
        'ffn_w_gate_up': nrm(ks[23], (N_DENSE, D_MODEL, 2 * D_FF), D_MODEL ** -0.5),
        'ffn_w_down': nrm(ks[24], (N_DENSE, D_FF, D_MODEL), D_FF ** -0.5),
        'moe_router': nrm(ks[25], (N_MOE, D_MODEL, N_EXPERTS), D_MODEL ** -0.5),
        'moe_w1': nrm(ks[26], (N_MOE, N_EXPERTS, D_MODEL, D_EXPERT), D_MODEL ** -0.5),
        'moe_w3': nrm(ks[27], (N_MOE, N_EXPERTS, D_MODEL, D_EXPERT), D_MODEL ** -0.5),
        'moe_w2': nrm(ks[28], (N_MOE, N_EXPERTS, D_EXPERT, D_MODEL), D_EXPERT ** -0.5),
    }


def reference(x, positions, norm_mix, w_in, conv_w, conv_b, lru_wa, lru_ba, lru_wx, lru_bx, lru_lam,
              out_g_a, hg_lb_logits, hg_norm_g, mla_q_norm, mla_w_uq, mla_kv_norm, mla_w_ukv,
              qk_norm_q, qk_norm_k, out_g_c, w_out, norm_ffn, ffn_w_gate_up, ffn_w_down,
              moe_router, moe_w1, moe_w3, moe_w2):
    p = jax.nn.softmax(hg_lb_logits.astype(jnp.float32), axis=0)
    lower_bounds = jnp.cumsum(p, axis=0) - p[0:1]
    for l in range(DEPTH):
        h = rms_norm(x, norm_mix[l])
        (xa, ga, hq, hff, hfb, hi, hg, cq, ckv, kr) = split_columns(h @ w_in[l])
        y_a = rglru_mixer(xa, ga, conv_w[l], conv_b[l], lru_wa[l], lru_ba[l], lru_wx[l], lru_bx[l], lru_lam[l])
        y_b = hgrn2_mixer(hq, hff, hfb, hi, hg, lower_bounds[l], hg_norm_g[l])
        y_c = mla_mixer(cq, ckv, kr, positions, mla_q_norm[l], mla_w_uq[l], mla_kv_norm[l], mla_w_ukv[l],
                        qk_norm_q[l], qk_norm_k[l])
        mixed = jnp.concatenate([rms_norm(y_a, out_g_a[l]), y_b, rms_norm(y_c, out_g_c[l])], axis=-1)
        x = x + mixed @ w_out[l]
        h = rms_norm(x, norm_ffn[l])
        if l % 2 == 0:
            x = x + swiglu_dense(h, ffn_w_gate_up[l // 2], ffn_w_down[l // 2])
        else:
            x = x + moe_swiglu(h, moe_router[l // 2], moe_w1[l // 2], moe_w3[l // 2], moe_w2[l // 2])
    return x
```

```python
import contextlib
import numpy as np
import concourse.bass as bass
import concourse.mybir as mybir
from concourse.bass_utils import run_bass_kernel_spmd

F32 = mybir.dt.float32
BF16 = mybir.dt.bfloat16
I32 = mybir.dt.int32
ALU = mybir.AluOpType
AF = mybir.ActivationFunctionType
AX = mybir.AxisListType

T = 2048
D = 1024
NT = 4
EPS = 1e-6
D_IN = 2208
D_FF = 2752
D_EXP = 3584
NEXP = 8
ARW = 26112
CSTOP = 99


class _Op:
    __slots__ = ("eng", "fn", "deps", "signals", "ticket", "kind", "dsem", "dtarget", "prev_same_sem")

    def __init__(self, eng, fn, kind):
        self.eng = eng
        self.fn = fn
        self.deps = []
        self.signals = False
        self.ticket = None
        self.kind = kind
        self.dsem = None
        self.dtarget = None
        self.prev_same_sem = None


class Prog:
    ENGS = ("pe", "act", "dve", "pool", "sp")

    def __init__(self, nc, n_dma_sems=8):
        self.nc = nc
        self.ops = {e: [] for e in self.ENGS}
        self.last_w = {}
        self.readers = {}
        self.n_dma_sems = n_dma_sems
        self.dma_count = {e: 0 for e in self.ENGS}
        self.dma_last_on_sem = {}
        self.fence_deps = None
        self.fenced = set()

    def _add_dep(self, op, p):
        if p is None or p is op:
            return
        if p.eng == op.eng and p.kind == "c" and op.kind == "c" and p.eng == "pe":
            return
        op.deps.append(p)
        p.signals = True

    def fence(self):
        deps = []
        for e in self.ENGS:
            for o in reversed(self.ops[e]):
                if o.kind == "c":
                    deps.append(o)
                    break
        deps.extend(self.dma_last_on_sem.values())
        self.fence_deps = deps
        self.fenced = set()

    def op(self, eng, fn, reads=(), writes=(), kind="c"):
        o = _Op(eng, fn, kind)
        if eng != "pe":
            extra = [r for r in reads if isinstance(r, str) and r.startswith("ps") and r[2:].isdigit() and r not in writes]
            if extra:
                writes = list(writes) + extra
        if self.fence_deps is not None and eng not in self.fenced:
            self.fenced.add(eng)
            for p in self.fence_deps:
                if p.eng == eng and p.kind == "c":
                    continue
                o.deps.append(p)
                p.signals = True
        for r in reads:
            self._add_dep(o, self.last_w.get(r))
        for w in writes:
            self._add_dep(o, self.last_w.get(w))
            rd = self.readers.get(w)
            if rd:
                for p in rd.values():
                    self._add_dep(o, p)
        for r in reads:
            d = self.readers.setdefault(r, {})
            if kind == "c":
                d[eng] = o
            else:
                d[("dma", id(o))] = o
        for w in writes:
            self.last_w[w] = o
            self.readers[w] = {}
        if kind == "d":
            i = self.dma_count[eng]
            self.dma_count[eng] = i + 1
            slot = (eng, i % self.n_dma_sems)
            o.dsem = slot
            o.dtarget = 16 * (i // self.n_dma_sems + 1)
            o.prev_same_sem = self.dma_last_on_sem.get(slot)
            self.dma_last_on_sem[slot] = o
        self.ops[eng].append(o)
        return o

    def dma(self, eng, out, in_, reads=(), writes=(), **kw):
        return self.op(eng, lambda e: e.dma_start(out=out, in_=in_, **kw), reads, writes, kind="d")

    def emit(self, final_wait_eng="sp"):
        nc = self.nc
        with contextlib.ExitStack() as st:
            esem = {}
            for e in self.ENGS:
                if any(o.kind == "c" for o in self.ops[e]):
                    esem[e] = st.enter_context(nc.semaphore("s_" + e))
            dsem = {}
            for e in self.ENGS:
                for j in range(min(self.dma_count[e], self.n_dma_sems)):
                    dsem[(e, j)] = st.enter_context(nc.semaphore("d_%s%d" % (e, j)))
            for e in self.ENGS:
                t = 0
                for o in self.ops[e]:
                    if o.kind == "c" and o.signals:
                        t += 1
                        o.ticket = t
            final_dmas = list(self.dma_last_on_sem.values())
            block = st.enter_context(nc.Block())

            def make(e):
                def body(eng):
                    waited = {}

                    def wait(key, sem, val):
                        if waited.get(key, 0) >= val:
                            return
                        waited[key] = val
                        eng.wait_ge(sem, val)

                    for o in self.ops[e]:
                        for p in o.deps:
                            if p.kind == "c":
                                wait(("c", p.eng), esem[p.eng], p.ticket)
                            else:
                                wait(("d", p.dsem), dsem[p.dsem], p.dtarget)
                        if o.kind == "d" and o.prev_same_sem is not None:
                            p = o.prev_same_sem
                            wait(("d", p.dsem), dsem[p.dsem], p.dtarget)
                        ins = o.fn(eng)
                        if o.kind == "d":
                            ins.then_inc(dsem[o.dsem], 16)
                        elif o.signals:
                            ins.then_inc(esem[e], 1)
                    if e == final_wait_eng:
                        for p in final_dmas:
                            wait(("d", p.dsem), dsem[p.dsem], p.dtarget)
                return body

            names = {"pe": "tensor", "act": "scalar", "dve": "vector", "pool": "gpsimd", "sp": "sync"}
            for e in self.ENGS:
                if not self.ops[e] and e != final_wait_eng:
                    continue
                getattr(block, names[e])(make(e))


def rev_ap(a):
    (ps, pn), (s, n) = a.ap
    return bass.AP(a.tensor, a.offset + (n - 1) * s, [[ps, pn], [-s, n]])


def fview(a, dims):
    return bass.AP(a.tensor, a.offset, [list(a.ap[0])] + [list(d) for d in dims])


PV_GMIX, PV_GFFN, PV_CW, PV_CB, PV_BA, PV_BX, PV_LAM, PV_GA, PV_LB, PV_HGN, PV_GQ, PV_GKV, PV_GC = \
    0, 8, 16, 24, 26, 30, 34, 38, 40, 48, 49, 51, 52
NPV = 60
C_ID, C_MLOW, C_MUP, C_BONES, C_IFQ = 0, 128, 192, 256, 384
NCONST = C_IFQ + 16


def build_nc(nseq=2, stop=None, phases="abc"):
    nc = bass.Bass("TRN2", target_bir_lowering=False)
    dr = lambda n, s, dt=F32, kind="ExternalInput": nc.dram_tensor(n, list(s), dt, kind=kind).ap()
    x_d = dr("x", [nseq, T, D])
    pos_d = dr("pos", [nseq, 128, 16], I32)
    consts_d = dr("consts", [128, NCONST])
    pv_d = dr("pv", [2, 128, NPV])
    pbq_d = dr("pbq", [2, 128, 192])
    w_in_d = dr("w_in", [2, D, D_IN])
    wa_d = dr("lru_wa", [2, 2, 4, 64, 64])
    wx_d = dr("lru_wx", [2, 2, 4, 64, 64])
    wuq_d = dr("mla_w_uq", [2, 256, 768])
    wukv_d = dr("mla_w_ukv", [2, 128, 1024])
    wout_d = dr("w_out", [2, D, D])
    wgu_d = dr("ffn_w_gate_up", [1, D, 2 * D_FF])
    wdn_d = dr("ffn_w_down", [1, D_FF, D])
    rt_d = dr("moe_router", [1, D, NEXP])
    w1_d = dr("moe_w1", [1, NEXP, D, D_EXP])
    w3_d = dr("moe_w3", [1, NEXP, D, D_EXP])
    w2_d = dr("moe_w2", [1, NEXP, D_EXP, D])
    y_d = dr("y", [nseq, T, D], kind="ExternalOutput")

    with contextlib.ExitStack() as st:
        def sb(name, shape, dt=F32):
            return st.enter_context(nc.sbuf_tensor(name, list(shape), dt))

        X = sb("X", [128, 8, T])
        HT = sb("HT", [128, 8, T], BF16)
        AR = sb("AR", [128, ARW])
        CONST = sb("CONST", [128, NCONST])
        PV = sb("PV", [128, 2, NPV])
        PBQ = sb("PBQ", [128, 2, 192])
        IDB = sb("IDB", [128, 128], BF16)
        ONESB = sb("ONESB", [128, 128], BF16)
        BONESB = sb("BONESB", [128, 128], BF16)
        MASKS = sb("MASKS", [128, 2, 64])
        SM = sb("SM", [128, 512])
        ROPE = sb("ROPE", [128, 2, 16, 16])
        psb = [st.enter_context(nc.psum_tensor("ps%d" % i, [128, 512], F32)) for i in range(8)]

        P = Prog(nc)
        ident = CONST[:, C_ID:C_ID + 128]

        def MM(out, lhsT, rhs, start, stop_, r, w):
            P.op("pe", lambda e: e.matmul(out, lhsT=lhsT, rhs=rhs, start=start, stop=stop_), r, w)

        def TR(out, in_, idn, r, w):
            P.op("pe", lambda e: e.transpose(out, in_, idn), r, w)

        def ACT(out, in_, func, r, w, bias=0.0, scale=1.0):
            P.op("act", lambda e: e.activation(out=out, in_=in_, func=func, bias=bias, scale=scale), r, w)

        def TT(eng, out, in0, in1, op, r, w):
            P.op(eng, lambda e: e.tensor_tensor(out=out, in0=in0, in1=in1, op=op), r, w)

        def TS(eng, out, in0, s1, s2, op0, op1, r, w):
            P.op(eng, lambda e: e.tensor_scalar(out=out, in0=in0, scalar1=s1, scalar2=s2, op0=op0, op1=op1), r, w)

        def STT(eng, out, in0, scalar, in1, op0, op1, r, w):
            eng = "dve"
            P.op(eng, lambda e: e.scalar_tensor_tensor(out=out, in0=in0, scalar=scalar, in1=in1, op0=op0, op1=op1), r, w)

        def CP(eng, out, in_, r, w):
            if eng == "act":
                P.op("act", lambda e: e.copy(out=out, in_=in_), r, w)
            else:
                P.op(eng, lambda e: e.tensor_copy(out=out, in_=in_), r, w)

        def RECIP(out, in_, r, w):
            P.op("dve", lambda e: e.reciprocal(out=out, in_=in_), r, w)

        def MEMSET(eng, ap, val, w):
            P.op(eng, lambda e: e.memset(ap, val), (), w)

        bank_rr = [0]

        def nb(lo=0, hi=8):
            i = bank_rr[0]
            i = lo + (i - lo + 1) % (hi - lo) if lo <= i < hi else lo
            bank_rr[0] = i
            return i

        def arf(off, n):
            return AR[:, off:off + n]

        def arb(off, n):
            return AR[:, off:off + n // 2].bitcast(BF16)

        P.dma("sp", CONST[:], consts_d, writes=["CONST"])
        P.dma("sp", PV[:], pv_d.rearrange("l p n -> p l n"), writes=["PV"])
        P.dma("sp", PBQ[:], pbq_d.rearrange("l p n -> p l n"), writes=["PBQ"])
        CP("dve", IDB[:], ident, ["CONST"], ["IDB"])
        MEMSET("dve", ONESB[:], 1.0, ["ONESB"])
        CP("dve", BONESB[:], CONST[:, C_BONES:C_BONES + 128], ["CONST"], ["BONESB"])
        CP("dve", MASKS[:, 0, :], CONST[:, C_MLOW:C_MLOW + 64], ["CONST"], ["MASKS"])
        CP("dve", MASKS[:, 1, :], CONST[:, C_MUP:C_MUP + 64], ["CONST"], ["MASKS"])

        def rsqrt_bc(out, ps, scale, r, w):
            ACT(out, ps, AF.Sqrt, r, w, bias=SM[:, 0:1], scale=scale)
            RECIP(out, out, w, w)

        MEMSET("dve", SM[:, 0:1], EPS, ["SM0"])
        MEMSET("dve", SM[:, 1:2], 1.0, ["SM1"])

        def load_x(s):
            stg = [arf(0, 1024), arf(1024, 1024)]
            for i in range(16):
                sg = stg[i % 2]
                P.dma("sp", sg, x_d[s, i * 128:(i + 1) * 128, :], writes=["stg%d" % (i % 2)])
                for h in range(2):
                    b = nb()
                    for q in range(4):
                        k = h * 4 + q
                        TR(psb[b][:, q * 128:(q + 1) * 128], sg[:, k * 128:(k + 1) * 128], ident,
                           ["stg%d" % (i % 2), "CONST"], ["ps%d" % b])
                    eng = "act" if h == 0 else "dve"
                    CP(eng, X[:, h * 4:(h + 1) * 4, i * 128:(i + 1) * 128],
                       psb[b][:].rearrange("p (q t) -> p q t", q=4), ["ps%d" % b], ["X"])

        def store_x(s):
            stg = [arf(0, 1024), arf(1024, 1024)]
            for i in range(16):
                sg = stg[i % 2]
                for h in range(2):
                    b = nb()
                    for q in range(4):
                        k = h * 4 + q
                        TR(psb[b][:, q * 128:(q + 1) * 128], X[:, k, i * 128:(i + 1) * 128], ident,
                           ["X", "CONST"], ["ps%d" % b])
                    eng = "act" if h == 0 else "dve"
                    CP(eng, sg[:, h * 512:(h + 1) * 512], psb[b][:], ["ps%d" % b], ["stg%d" % (i % 2)])
                P.dma("sp", y_d[s, i * 128:(i + 1) * 128, :], sg, reads=["stg%d" % (i % 2)])

        def rmsnorm_to_HT(l, gcol, scr_off):
            RB = arf(scr_off, T)
            for k in range(8):
                ACT(HT[:, k, :], X[:, k, :], AF.Square, ["X"], ["HT%d" % k])
            for tt in range(NT):
                b = nb()
                for k in range(8):
                    MM(psb[b][:], ONESB[:], HT[:, k, tt * 512:(tt + 1) * 512], k == 0, k == 7,
                       ["ONESB", "HT%d" % k], ["ps%d" % b])
                rsqrt_bc(RB[:, tt * 512:(tt + 1) * 512], psb[b][:], 1.0 / D, ["ps%d" % b, "SM0"], ["RB%d" % tt])
            for k in range(8):
                eng = "dve" if k % 2 == 0 else "pool"
                STT(eng, HT[:, k, :], X[:, k, :], PV[:, l, gcol + k:gcol + k + 1], RB[:], ALU.mult, ALU.mult,
                    ["X", "PV"] + ["RB%d" % t_ for t_ in range(NT)], ["HT%d" % k])

        HTk = ["HT%d" % k for k in range(8)]

        def proj_fm(WIN, col0, m, tt, b, pbase=0):
            for k in range(8):
                MM(psb[b][pbase:pbase + m, :], WIN[:, k, col0:col0 + m], HT[:, k, tt * 512:(tt + 1) * 512],
                   k == 0, k == 7, ["WIN", "HT%d" % k], ["ps%d" % b])

        def add_to_x(ps, dk, tt, r):
            TT("dve", X[:, dk, tt * 512:(tt + 1) * 512], ps, X[:, dk, tt * 512:(tt + 1) * 512], ALU.add,
               r + ["X"], ["X"])

        def mixer(l, s):
            WIN = arb(0, 8 * D_IN).rearrange("p (k n) -> p k n", k=8)
            P.dma("pool", WIN[:, 0:4, :], w_in_d[l, 0:512, :].rearrange("(k p) n -> p k n", p=128), writes=["WIN"])
            P.dma("pool", WIN[:, 4:8, :], w_in_d[l, 512:1024, :].rearrange("(k p) n -> p k n", p=128), writes=["WIN"])
            SC0 = 8832
            rmsnorm_to_HT(l, PV_GMIX, SC0)
            if "a" in phases:
                phase_a(l, WIN, SC0)
                P.fence()
            if "b" in phases:
                phase_b(l, WIN, SC0)
                P.fence()
            if "c" in phases:
                phase_c(l, s, WIN, SC0)
                P.fence()

        def phase_a(l, WIN, o0):
            S = [arf(o0 + i * T, T) for i in range(6)]
            XCB = arb(o0 + 6 * T, T)
            YAB = [arb(o0 + 6 * T + 1024 + j * 1024, T) for j in range(2)]
            WOA = arb(o0 + 6 * T + 3072, 2 * D).rearrange("p (k n) -> p k n", k=2)
            BD = arb(o0 + 6 * T + 3072 + 1024, 8 * 128).rearrange("p (g n) -> p g n", g=8)
            P.dma("pool", WOA, wout_d[l, 0:256, :].rearrange("(k p) n -> p k n", p=128), writes=["WOA"])
            MEMSET("pool", BD, 0.0, ["BD"])
            for g, wd in enumerate((wa_d, wx_d)):
                for d in range(2):
                    for j in range(2):
                        gi = g * 4 + d * 2 + j
                        for hb in range(2):
                            P.dma("pool", BD[hb * 64:(hb + 1) * 64, gi, hb * 64:(hb + 1) * 64],
                                  wd[l, d, 2 * j + hb], writes=["BD"])
            ACT(SM[:, 8:12], PV[:, l, PV_LAM:PV_LAM + 4], AF.Exp, ["PV"], ["SMA"], scale=-1.0)
            ACT(SM[:, 8:12], SM[:, 8:12], AF.Ln, ["SMA"], ["SMA"], bias=SM[:, 1:2], scale=1.0)
            TS("dve", SM[:, 12:16], SM[:, 8:12], -16.0, None, ALU.mult, ALU.mult, ["SMA"], ["SMB"]) if False else None
            P.op("dve", lambda e: e.tensor_scalar_mul(out=SM[:, 12:16], in0=SM[:, 8:12], scalar1=-16.0), ["SMA"], ["SMB"])
            P.op("dve", lambda e: e.tensor_scalar_mul(out=SM[:, 8:12], in0=SM[:, 8:12], scalar1=-8.0), ["SMA", "SMB"], ["SMA"])
            for j in range(2):
                XA, XC, R, I, A2, HF = S
                cw = lambda kk: PV[:, l, PV_CW + j * 4 + kk:PV_CW + j * 4 + kk + 1]
                for tt in range(NT):
                    b = nb()
                    proj_fm(WIN, j * 128, 128, tt, b)
                    CP("act", XA[:, tt * 512:(tt + 1) * 512], psb[b][:], ["ps%d" % b], ["XA"])
                TS("dve", XC[:], XA[:], cw(2), PV[:, l, PV_CB + j:PV_CB + j + 1], ALU.mult, ALU.add, ["XA", "PV"], ["XC"])
                STT("dve", XC[:, 2:T], XA[:, 0:T - 2], cw(0), XC[:, 2:T], ALU.mult, ALU.add, ["XA", "PV", "XC"], ["XC"])
                STT("dve", XC[:, 1:T], XA[:, 0:T - 1], cw(1), XC[:, 1:T], ALU.mult, ALU.add, ["XA", "PV", "XC"], ["XC"])
                STT("dve", XC[:, 0:T - 1], XA[:, 1:T], cw(3), XC[:, 0:T - 1], ALU.mult, ALU.add, ["XA", "PV", "XC"], ["XC"])
                CP("pool", XCB[:], XC[:], ["XC"], ["XCB"])
                HB = XA
                for d in range(2):
                    for tt in range(NT):
                        sl = slice(tt * 512, (tt + 1) * 512)
                        b = nb()
                        MM(psb[b][:], BD[:, 0 * 4 + d * 2 + j, :], XCB[:, sl], True, True, ["BD", "XCB"], ["ps%d" % b])
                        ACT(R[:, sl], psb[b][:], AF.Sigmoid, ["ps%d" % b, "PV"], ["R"],
                            bias=PV[:, l, PV_BA + d * 2 + j:PV_BA + d * 2 + j + 1])
                        b = nb()
                        MM(psb[b][:], BD[:, 1 * 4 + d * 2 + j, :], XCB[:, sl], True, True, ["BD", "XCB"], ["ps%d" % b])
                        ACT(I[:, sl], psb[b][:], AF.Sigmoid, ["ps%d" % b, "PV"], ["I"],
                            bias=PV[:, l, PV_BX + d * 2 + j:PV_BX + d * 2 + j + 1])
                    ci = d * 2 + j
                    ACT(A2[:], R[:], AF.Exp, ["R", "SMB"], ["A2"], scale=SM[:, 12 + ci:13 + ci])
                    ACT(R[:], R[:], AF.Exp, ["R", "SMA"], ["R"], scale=SM[:, 8 + ci:9 + ci])
                    TS("dve", A2[:], A2[:], -1.0, 1.0, ALU.mult, ALU.add, ["A2"], ["A2"])
                    P.op("dve", lambda e: e.tensor_scalar_max(out=A2[:], in0=A2[:], scalar1=0.0), ["A2"], ["A2"])
                    ACT(A2[:], A2[:], AF.Sqrt, ["A2"], ["A2"])
                    TT("pool", I[:], I[:], XC[:], ALU.mult, ["I", "XC"], ["I"])
                    TT("dve", I[:], I[:], A2[:], ALU.mult, ["I", "A2"], ["I"])
                    if d == 0:
                        P.op("dve", lambda e: e.tensor_tensor_scan(out=HF[:], data0=R[:], data1=I[:], initial=0.0,
                                                                  op0=ALU.mult, op1=ALU.add), ["R", "I"], ["HF"])
                    else:
                        P.op("dve", lambda e: e.tensor_tensor_scan(out=rev_ap(HB[:]), data0=rev_ap(R[:]), data1=rev_ap(I[:]),
                                                                  initial=0.0, op0=ALU.mult, op1=ALU.add), ["R", "I", "XA"], ["XA"])
                TT("pool", HF[:], HF[:], HB[:], ALU.add, ["HF", "XA"], ["HF"])
                G, G2 = R, I
                for tt in range(NT):
                    b = nb()
                    proj_fm(WIN, 256 + j * 128, 128, tt, b)
                    CP("act", G[:, tt * 512:(tt + 1) * 512], psb[b][:], ["ps%d" % b], ["R"])
                TT("pool", G2[:], G[:], G[:], ALU.mult, ["R"], ["I"])
                TS("dve", G2[:], G2[:], 0.044715, 1.0, ALU.mult, ALU.add, ["I"], ["I"])
                TT("pool", G2[:], G2[:], G[:], ALU.mult, ["I", "R"], ["I"])
                ACT(G2[:], G2[:], AF.Sigmoid, ["I"], ["I"], scale=1.5957691216057308)
                TT("dve", G[:], G[:], G2[:], ALU.mult, ["R", "I"], ["R"])
                TT("dve", YAB[j][:], HF[:], G[:], ALU.mult, ["HF", "R"], ["YAB%d" % j])
            SQ = [arb(o0 + jj * 1024, T) for jj in range(2)]
            RB = S[1]
            for j in range(2):
                ACT(SQ[j][:], YAB[j][:], AF.Square, ["YAB%d" % j], ["XA"])
            for tt in range(NT):
                sl = slice(tt * 512, (tt + 1) * 512)
                b = nb()
                for j in range(2):
                    MM(psb[b][:], ONESB[:], SQ[j][:, sl], j == 0, j == 1, ["ONESB", "XA"], ["ps%d" % b])
                rsqrt_bc(RB[:, sl], psb[b][:], 1.0 / 256, ["ps%d" % b, "SM0"], ["XC"])
            for j in range(2):
                STT("dve", YAB[j][:], YAB[j][:], PV[:, l, PV_GA + j:PV_GA + j + 1], RB[:], ALU.mult, ALU.mult,
                    ["YAB%d" % j, "PV", "XC"], ["YAB%d" % j])
            for dk in range(8):
                for tt in range(NT):
                    b = nb()
                    for j in range(2):
                        MM(psb[b][:], WOA[:, j, dk * 128:(dk + 1) * 128], YAB[j][:, tt * 512:(tt + 1) * 512],
                           j == 0, j == 1, ["WOA", "YAB%d" % j], ["ps%d" % b])
                    add_to_x(psb[b][:], dk, tt, ["ps%d" % b])

        def phase_b(l, WIN, o0):
            HN = 1024
            bb = [arf(o0 + i * HN, HN) for i in range(4)]
            o1 = o0 + 4 * HN
            QT = arb(o1, T)
            KZ = [arb(o1 + 1024 + hh * 1024, T) for hh in range(2)]
            KTOK = arb(o1 + 3072, T).rearrange("p (i n) -> p i n", i=16)
            VZ = [arb(o1 + 4096 + cp * 1024, T).rearrange("p (i n) -> p i n", i=16) for cp in range(2)]
            o3 = o1 + 6144
            O = arf(o3, T)
            UF = arf(o3 + 2048, 2048)
            SBFall = arb(o3 + 2048, 32 * 128).rearrange("p (r n) -> p r n", r=32)
            D3 = arb(o3 + 4096, 2048)
            SALL = arb(o3 + 5120, 2048)
            WOB = arb(o3 + 6144, D)
            SCB = arb(o3 + 6656, 4 * 128).rearrange("p (u n) -> p u n", u=4)
            assert o3 + 6912 <= ARW
            YB = arb(o1 + 3072, T)
            FAC = SM[:, 64:224].rearrange("p (f c) -> p f c", f=5)
            FACR = SM[:, 224:320].rearrange("p (f c) -> p f c", f=3)
            MEMSET("pool", KZ[0][64:128, :], 0.0, ["KZ"])
            MEMSET("pool", KZ[1][0:64, :], 0.0, ["KZ"])
            MEMSET("pool", VZ[0][64:128, :, :], 0.0, ["VZ"])
            MEMSET("pool", VZ[1][0:64, :, :], 0.0, ["VZ"])
            MEMSET("pool", SCB[64:128, 0:2, :], 0.0, ["SCB0", "SCB1"])
            MEMSET("pool", SCB[0:64, 2:4, :], 0.0, ["SCB2", "SCB3"])
            LBT = SM[:, 16:32]
            ACT(LBT[:, 0:8], PV[:, l, PV_LB:PV_LB + 8], AF.Exp, ["PV"], ["LBT"])
            TT("dve", LBT[:, 8:12], LBT[:, 0:4], LBT[:, 4:8], ALU.add, ["LBT"], ["LBT"])
            RECIP(LBT[:, 8:12], LBT[:, 8:12], ["LBT"], ["LBT"])
            if l == 0:
                MEMSET("dve", LBT[:, 12:16], 0.0, ["LBT"])
            else:
                TT("dve", LBT[:, 12:16], LBT[:, 4:8], LBT[:, 8:12], ALU.mult, ["LBT"], ["LBT"])
            TS("dve", LBT[:, 8:12], LBT[:, 12:16], -1.0, 1.0, ALU.mult, ALU.add, ["LBT"], ["LBT"])
            for jp in range(2):
                P.dma("pool", WOB, wout_d[l, 256 + jp * 128:256 + (jp + 1) * 128, :], writes=["WOB"])
                for i in range(16):
                    b = nb(0, 4)
                    for k in range(8):
                        MM(psb[b][:, 0:128], HT[:, k, i * 128:(i + 1) * 128], WIN[:, k, 1280 + jp * 128:1280 + (jp + 1) * 128],
                           k == 0, k == 7, ["WIN", "HT%d" % k], ["ps%d" % b])
                    CP("act", VZ[0][0:64, i, :], psb[b][0:64, 0:128], ["ps%d" % b], ["VZ"])
                    CP("dve", VZ[1][64:128, i, :], psb[b][64:128, 0:128], ["ps%d" % b], ["VZ"])
                for d in range(2):
                    zc = (768 if d == 0 else 1024) + jp * 128
                    lbc = d * 2 + jp
                    for hf in range(2):
                        t0 = hf * HN
                        F_, LG_, B_, X_ = bb
                        for t2 in range(2):
                            tt = hf * 2 + t2
                            sl = slice(t2 * 512, (t2 + 1) * 512)
                            b = nb(0, 4)
                            proj_fm(WIN, zc, 128, tt, b)
                            ACT(F_[:, sl], psb[b][:], AF.Sigmoid, ["ps%d" % b], ["bF"])
                        TS("dve", F_[:], F_[:], LBT[:, 8 + lbc:9 + lbc], LBT[:, 12 + lbc:13 + lbc], ALU.mult, ALU.add,
                           ["bF", "LBT"], ["bF"])
                        ACT(LG_[:], F_[:], AF.Ln, ["bF"], ["bL"])
                        TS("pool", F_[:], F_[:], -1.0, 1.0, ALU.mult, ALU.add, ["bF"], ["bF"])
                        init = 0.0 if hf == 0 else SM[:, 40:41]
                        ones_bc = fview(SM[:, 1:2], [(0, HN)])
                        P.op("dve", lambda e, init=init, B_=B_, LG_=LG_, ones_bc=ones_bc: e.tensor_tensor_scan(
                            out=B_[:], data0=ones_bc, data1=LG_[:], initial=init, op0=ALU.mult, op1=ALU.add),
                            ["SM1", "bL", "SMc"], ["bB"])
                        CP("dve", SM[:, 40:41], B_[:, HN - 1:HN], ["bB"], ["SMc"])
                        B3 = B_[:].rearrange("p (c n) -> p c n", n=64)
                        LG3 = LG_[:].rearrange("p (c n) -> p c n", n=64)
                        c0 = hf * 16
                        if d == 1:
                            CP("dve", FAC[:, 2, c0:c0 + 16], B3[:, :, 63], ["bB"], ["FAC"])
                            TT("dve", B_[:], B_[:], LG_[:], ALU.subtract, ["bB", "bL"], ["bB"])
                        CP("dve", FAC[:, 0, c0:c0 + 16], B3[:, :, 32], ["bB"], ["FAC"])
                        if d == 0:
                            CP("dve", FAC[:, 1, c0:c0 + 16], B3[:, :, 63], ["bB"], ["FAC"])
                        else:
                            CP("dve", FAC[:, 1, c0:c0 + 16], B3[:, :, 0], ["bB"], ["FAC"])
                        TT("dve", B3, B3, FAC[:, 0, c0:c0 + 16].to_broadcast([128, 16, 64]), ALU.subtract,
                           ["bB", "FAC"], ["bB"])
                        sgn_q = 1.0 if d == 0 else -1.0
                        ACT(LG_[:], B_[:], AF.Exp, ["bB"], ["bL"], scale=sgn_q)
                        for t2 in range(2):
                            tt = hf * 2 + t2
                            sl = slice(t2 * 512, (t2 + 1) * 512)
                            b = nb(0, 4)
                            proj_fm(WIN, 512 + jp * 128, 128, tt, b)
                            ACT(X_[:, sl], psb[b][:], AF.Silu, ["ps%d" % b], ["bX"])
                        STT("dve", QT[:, t0:t0 + HN], X_[:], 0.125, LG_[:], ALU.mult, ALU.mult, ["bX", "bL"], ["QT"])
                        ACT(LG_[:], B_[:], AF.Exp, ["bB", "QT"], ["bL"], scale=-sgn_q)
                        TT("pool", KZ[0][0:64, t0:t0 + HN], F_[0:64, :], LG_[0:64, :], ALU.mult, ["bF", "bL"], ["KZ"])
                        TT("dve", KZ[1][64:128, t0:t0 + HN], F_[64:128, :], LG_[64:128, :], ALU.mult, ["bF", "bL"], ["KZ"])
                    Fm, Fl = FAC[:, 0, :], FAC[:, 1, :]
                    if d == 0:
                        CP("dve", FAC[:, 2, 0:1], Fm[:, 0:1], ["FAC"], ["FAC"])
                        TT("dve", FAC[:, 2, 1:32], Fm[:, 1:32], Fl[:, 0:31], ALU.subtract, ["FAC"], ["FAC"])
                        TT("dve", FAC[:, 3, :], Fl, Fm, ALU.subtract, ["FAC"], ["FAC"])
                        CP("dve", FAC[:, 4, 0:1], Fl[:, 0:1], ["FAC"], ["FAC"])
                        TT("dve", FAC[:, 4, 1:32], Fl[:, 1:32], Fl[:, 0:31], ALU.subtract, ["FAC"], ["FAC"])
                    else:
                        TT("dve", FAC[:, 3, :], Fm, Fl, ALU.subtract, ["FAC"], ["FAC"])
                        TT("dve", FAC[:, 4, :], FAC[:, 2, :], Fl, ALU.subtract, ["FAC"], ["FAC"])
                        TT("dve", FAC[:, 2, :], FAC[:, 2, :], Fm, ALU.subtract, ["FAC"], ["FAC"])
                    ACT(FAC[:, 2:5, :], FAC[:, 2:5, :], AF.Exp, ["FAC"], ["FAC"])
                    for i in range(16):
                        b = nb(0, 4)
                        for hh in range(2):
                            MM(psb[b][:, 0:128], KZ[hh][:, i * 128:(i + 1) * 128], IDB[:], hh == 0, hh == 1, ["KZ", "IDB"], ["ps%d" % b])
                        CP("act" if i % 2 else "dve", KTOK[:, i, :], psb[b][:, 0:128], ["ps%d" % b], ["KTOK"])
                    if d == 0:
                        CP("dve", FACR[:, :, :], FAC[:, 2:5, :], ["FAC"], ["FACR"])
                    else:
                        f0 = FAC[:, 2, 31:32]
                        CP("dve", FACR[:, :, :], bass.AP(f0.tensor, f0.offset, [list(f0.ap[0]), [32, 3], [-1, 32]]), ["FAC"], ["FACR"])
                    MEMSET("dve", FACR[:, 2, 0:1], 0.0, ["FACR"])
                    CP("dve", D3[:].rearrange("p (e r) -> p e r", r=32), fview(FACR[:, 2, :], [(0, 64), (1, 32)]), ["FACR"], ["D3"])
                    for g in range(8):
                        b = nb(0, 4)
                        for q in range(4):
                            r = g * 4 + q
                            c = r if d == 0 else 31 - r
                            i, cp = c // 2, c % 2
                            MM(psb[b][:, q * 128:(q + 1) * 128], KTOK[:, i, :], VZ[cp][:, i, :], True, True, ["KTOK", "VZ"], ["ps%d" % b])
                        for hh in range(2):
                            pb = 64 * hh
                            src = fview(psb[b][pb:pb + 64, hh * 64:hh * 64 + 1], [(128, 4), (1, 64)])
                            dst = fview(UF[pb:pb + 64, g * 4:g * 4 + 1], [(1, 4), (32, 64)])
                            CP("act" if hh == 0 else "dve", dst, src, ["ps%d" % b], ["UF"])
                    UF3 = UF[:].rearrange("p (e r) -> p e r", r=32)
                    TT("dve", UF3, UF3, fview(FACR[:, 1, :], [(0, 64), (1, 32)]), ALU.mult, ["UF", "FACR"], ["UF"])
                    P.op("dve", lambda e: e.tensor_tensor_scan(out=SALL[:], data0=D3[:], data1=UF[:], initial=0.0,
                                                              op0=ALU.mult, op1=ALU.add), ["D3", "UF"], ["SALL"])
                    MEMSET("pool", SBFall[0:64, :, 64:128], 0.0, ["UF"])
                    MEMSET("pool", SBFall[64:128, :, 0:64], 0.0, ["UF"])
                    for hh in range(2):
                        pb = 64 * hh
                        src = fview(SALL[pb:pb + 64, 0:1], [(1, 31), (32, 64)])
                        TT("dve", SBFall[pb:pb + 64, 1:32, hh * 64:(hh + 1) * 64], src,
                           fview(FACR[pb:pb + 64, 0, 1:2], [(1, 31), (0, 64)]), ALU.mult, ["SALL", "FACR"], ["UF"])
                    pso_b = None
                    for r in range(32):
                        c = r if d == 0 else 31 - r
                        i, cp = c // 2, c % 2
                        tb = 64 * cp
                        cs = slice(c * 64, (c + 1) * 64)
                        if r % 8 == 0:
                            pso_b = 4 + (r // 8) % 4
                        pso = psb[pso_b]
                        ocol = (c % 8) * 64
                        su = cp * 2 + (r // 2) % 2
                        bs = nb(0, 4)
                        for hh in range(2):
                            MM(psb[bs][tb:tb + 64, hh * 64:(hh + 1) * 64], KZ[hh][:, cs], QT[:, cs], True, True,
                               ["KZ", "QT"], ["ps%d" % bs])
                        TT("dve", SCB[tb:tb + 64, su, :].rearrange("p (h n) -> p h n", h=2),
                           psb[bs][tb:tb + 64, 0:128].rearrange("p (h n) -> p h n", h=2),
                           fview(MASKS[tb:tb + 64, d, :], [(0, 2), (1, 64)]), ALU.mult,
                           ["ps%d" % bs, "MASKS"], ["SCB%d" % su])
                        for hh in range(2):
                            pb = 64 * hh
                            MM(pso[pb:pb + 64, ocol:ocol + 64], VZ[cp][:, i, pb:pb + 64], SCB[:, su, hh * 64:(hh + 1) * 64],
                               True, r == 0, ["VZ", "SCB%d" % su], ["ps%d" % pso_b])
                        if r > 0:
                            MM(pso[:, ocol:ocol + 64], SBFall[:, r, :], QT[:, cs], False, True, ["UF", "QT"], ["ps%d" % pso_b])
                        if r % 8 == 7:
                            g0 = (c // 8) * 512
                            if d == 0:
                                CP("act", O[:, g0:g0 + 512], pso[:], ["ps%d" % pso_b], ["O"])
                            else:
                                TT("dve", O[:, g0:g0 + 512], pso[:], O[:, g0:g0 + 512], ALU.add, ["ps%d" % pso_b, "O"], ["O"])
                SQ = arb(o0 + 2048, T)
                RB = arf(o0, T)
                GG = arf(o0 + 3072, 1024)
                ACT(SQ[:], O[:], AF.Square, ["O"], ["bB"])
                for tt in range(NT):
                    sl = slice(tt * 512, (tt + 1) * 512)
                    b = nb(0, 4)
                    MM(psb[b][:], BONESB[:], SQ[:, sl], True, True, ["BONESB", "bB"], ["ps%d" % b])
                    rsqrt_bc(RB[:, sl], psb[b][:], 1.0 / 64, ["ps%d" % b, "SM0"], ["bF", "bL"])
                    STT("dve", O[:, sl], O[:, sl], PV[:, l, PV_HGN:PV_HGN + 1], RB[:, sl], ALU.mult, ALU.mult, ["O", "PV", "bF", "bL"], ["O"])
                    b = nb(0, 4)
                    proj_fm(WIN, 1536 + jp * 128, 128, tt, b)
                    ACT(GG[:, 0:512], psb[b][:], AF.Silu, ["ps%d" % b], ["bX"])
                    TT("dve", YB[:, sl], O[:, sl], GG[:, 0:512], ALU.mult, ["O", "bX"], ["KTOK"])
                for dk in range(8):
                    for tt in range(NT):
                        b = nb(0, 4)
                        MM(psb[b][:], WOB[:, dk * 128:(dk + 1) * 128], YB[:, tt * 512:(tt + 1) * 512], True, True,
                           ["WOB", "KTOK"], ["ps%d" % b])
                        add_to_x(psb[b][:], dk, tt, ["ps%d" % b])
                for kk_ in ("bF", "bL", "bB", "bX"):
                    pass

        def rope_tables(s, R0):
            PI = arf(R0, 16).bitcast(I32)
            PF = arf(R0 + 16, 16)
            ANG = arf(R0 + 32, 512).rearrange("p (c i f) -> p c i f", c=2, i=16)
            KK = arf(R0 + 544, 512)
            KI = arf(R0 + 1056, 512).bitcast(I32)
            P.dma("sp", PI, pos_d[s], writes=["PI"])
            CP("dve", PF, PI, ["PI"], ["PF"])
            ifq = CONST[:, C_IFQ:C_IFQ + 16]
            TT("dve", ANG[:, 1], fview(PF, [(1, 16), (0, 16)]), fview(ifq, [(0, 16), (1, 16)]), ALU.mult, ["PF", "CONST"], ["ANG"])
            P.op("dve", lambda e: e.tensor_scalar_add(out=ANG[:, 0], in0=ANG[:, 1], scalar1=float(np.pi / 2)), ["ANG"], ["ANG"])
            A = ANG[:].rearrange("p c i f -> p (c i f)")
            K = KK
            TS("dve", K, A, float(1.0 / (2 * np.pi)), 0.5, ALU.mult, ALU.add, ["ANG"], ["KK"])
            CP("dve", KI, K, ["KK"], ["KI"])
            CP("dve", K, KI, ["KI"], ["KK"])
            C1 = 6.28125
            C2 = float(2 * np.pi - 6.28125)
            STT("dve", A, K, -C1, A, ALU.mult, ALU.add, ["KK", "ANG"], ["ANG"])
            STT("dve", A, K, -C2, A, ALU.mult, ALU.add, ["KK", "ANG"], ["ANG"])
            TS("dve", K, A, float(np.pi), float(-2 * np.pi), ALU.is_gt, ALU.mult, ["ANG"], ["KK"])
            TT("dve", A, A, K, ALU.add, ["ANG", "KK"], ["ANG"])
            TS("dve", K, A, float(-np.pi), float(2 * np.pi), ALU.is_lt, ALU.mult, ["ANG"], ["KK"])
            TT("dve", A, A, K, ALU.add, ["ANG", "KK"], ["ANG"])
            P.op("dve", lambda e: e.tensor_scalar_min(out=A, in0=A, scalar1=3.1415925), ["ANG"], ["ANG"])
            P.op("dve", lambda e: e.tensor_scalar_max(out=A, in0=A, scalar1=-3.1415925), ["ANG"], ["ANG"])
            ACT(ROPE[:].rearrange("p c i f -> p (c i f)"), A, AF.Sin, ["ANG"], ["ROPE"])

        def phase_c(l, s, WIN, base):
            KTh = HT
            VT = arb(0, 16 * 4 * 192).rearrange("p (i m e) -> p i m e", i=16, m=4)
            RD = arf(6144, 512)
            CQG = arb(base + 4096, 2 * T).rearrange("p (k t) -> p k t", k=2)
            WUQ = arb(base + 6144, 2 * 768).rearrange("p (k n) -> p k n", k=2)
            WOC = arb(base + 6912, 4 * D).rearrange("p (m n) -> p m n", m=4)
            sm0 = base + 8960
            RS = arf(sm0, 64)
            T8 = arf(sm0 + 64, 64)
            TA = arf(sm0 + 128, 128)
            TB = arf(sm0 + 256, 128)
            QS = arf(sm0 + 384, 1024)
            QQ = arf(sm0 + 1408, 1024)
            QB = arb(sm0 + 2432, 1024)
            KB = QB
            R0 = sm0 + 2944
            assert R0 + 5376 <= ARW, R0
            CKG = arb(R0, T)
            CSQ = arb(R0 + 1024, 3 * T).rearrange("p (k t) -> p k t", k=3)
            WUK = arb(R0 + 4096, 1024)
            KR = arf(R0 + 4608, 512).rearrange("p (i n) -> p i n", i=16)
            QTh = arb(R0, 8 * 512).rearrange("p (h t) -> p h t", h=8)
            PT = arb(R0 + 2048, 3 * 512).rearrange("p (u t) -> p u t", u=3)
            YC = arb(R0 + 2816, 4 * 512).rearrange("p (m t) -> p m t", m=4)
            YSQ = arb(R0 + 3840, 4 * 512).rearrange("p (m t) -> p m t", m=4)
            YN = YSQ
            RBC = arf(R0 + 4864, 512)

            if l == 0:
                rope_tables(s, R0)
                P.fence()
            if CSTOP <= 0.1:
                return
            P.dma("pool", WUQ, wuq_d[l].rearrange("(k p) n -> p k n", p=128), writes=["WUQ"])
            P.dma("pool", WUK, wukv_d[l], writes=["WUK"])
            P.dma("pool", WOC, wout_d[l, 512:1024, :].rearrange("(m p) n -> p m n", p=128), writes=["WOC"])
            for kc in range(3):
                col = 1792 + kc * 128
                for tt in range(NT):
                    sl = slice(tt * 512, (tt + 1) * 512)
                    b = nb()
                    proj_fm(WIN, col, 128, tt, b)
                    ACT(CSQ[:, kc, sl], psb[b][:], AF.Square, ["ps%d" % b], ["CSQ"])
                    gcol = (PV_GQ + kc) if kc < 2 else PV_GKV
                    dst = CQG[:, kc, sl] if kc < 2 else CKG[:, sl]
                    P.op("dve", lambda e, dst=dst, b=b, gcol=gcol: e.tensor_scalar_mul(out=dst, in0=psb[b][:], scalar1=PV[:, l, gcol:gcol + 1]),
                         ["ps%d" % b, "PV"], ["CQG"])
            if CSTOP <= 0.2:
                return
            b = nb()
            for i in range(16):
                for kc in range(2):
                    MM(psb[b][:, i:i + 1], CSQ[:, kc, i * 128:(i + 1) * 128], ONESB[:, 0:1], kc == 0, kc == 1, ["CSQ", "ONESB"], ["ps%d" % b])
                MM(psb[b][:, 16 + i:17 + i], CSQ[:, 2, i * 128:(i + 1) * 128], ONESB[:, 0:1], True, True, ["CSQ", "ONESB"], ["ps%d" % b])
            rsqrt_bc(RS[:, 0:16], psb[b][:, 0:16], 1.0 / 256, ["ps%d" % b, "SM0"], ["RS"])
            rsqrt_bc(RS[:, 16:32], psb[b][:, 16:32], 1.0 / 128, ["ps%d" % b, "SM0"], ["RS"])
            if CSTOP <= 0.3:
                return
            b = nb()
            for i in range(16):
                for k in range(8):
                    MM(psb[b][:, i * 32:(i + 1) * 32], HT[:, k, i * 128:(i + 1) * 128], WIN[:, k, 2176:2208], k == 0, k == 7,
                       ["WIN", "HT%d" % k], ["ps%d" % b])
            CP("act", KR[:].rearrange("p i n -> p (i n)"), psb[b][:], ["ps%d" % b], ["KR"])
            P.fence()
            if CSTOP <= 1:
                return
            gq = PBQ[:, l, 0:96]
            gk = PBQ[:, l, 96:192]

            def rope_apply(dst3, src3, i, nh, rkeys, wkey):
                cos = fview(ROPE[:, 0, i, :], [(0, nh), (1, 16)])
                sin = fview(ROPE[:, 1, i, :], [(0, nh), (1, 16)])
                x1, x2 = src3[:, :, 0:16], src3[:, :, 16:32]
                a = TA[:, 0:nh * 16].rearrange("p (h n) -> p h n", h=nh)
                bq = TB[:, 0:nh * 16].rearrange("p (h n) -> p h n", h=nh)
                TT("dve", a, x1, cos, ALU.mult, rkeys + ["ROPE"], ["rpA"])
                TT("pool", bq, x2, sin, ALU.mult, rkeys + ["ROPE"], ["rpB"])
                TT("dve", dst3[:, :, 0:16], a, bq, ALU.subtract, ["rpA", "rpB"], [wkey])
                TT("dve", a, x2, cos, ALU.mult, rkeys + ["ROPE"], ["rpA"])
                TT("pool", bq, x1, sin, ALU.mult, rkeys + ["ROPE"], ["rpB"])
                TT("dve", dst3[:, :, 16:32], a, bq, ALU.add, ["rpA", "rpB"], [wkey])

            def norm_rope_T(i, src_q3, g_bc, dstT, dcols, dkey):
                SQv = QS[:, 0:768].rearrange("p (h n) -> p h n", h=8)
                TT("dve", SQv, src_q3, src_q3, ALU.mult, ["QQ"], ["QS"])
                P.op("dve", lambda e, SQv=SQv: e.tensor_reduce(out=T8[:, 0:8], in_=SQv, axis=AX.X, op=ALU.add), ["QS"], ["T8"])
                rsqrt_bc(T8[:, 0:8], T8[:, 0:8], 1.0 / 96, ["T8", "SM0"], ["T8"])
                TT("dve", src_q3, src_q3, fview(T8[:, 0:8], [(1, 8), (0, 96)]), ALU.mult, ["QQ", "T8"], ["QQ"])
                TT("pool", src_q3, src_q3, fview(g_bc, [(0, 8), (1, 96)]), ALU.mult, ["QQ", "PBQ"], ["QQ"])
                Q3b = QB[:, 0:768].rearrange("p (h n) -> p h n", h=8)
                CP("dve", Q3b[:, :, 0:64], src_q3[:, :, 0:64], ["QQ"], ["QB"])
                rope_apply(Q3b[:, :, 64:96], src_q3[:, :, 64:96], i, 8, ["QQ"], "QB")
                for h in range(8):
                    bt = nb(0, 4)
                    pst = psb[bt][:].bitcast(BF16)
                    TR(pst[0:96, 0:128], Q3b[:, h, :], IDB[:], ["QB", "IDB"], ["ps%d" % bt])
                    CP("act" if h % 2 else "dve", dstT[0:96, h, dcols], pst[0:96, 0:128], ["ps%d" % bt], [dkey if dkey != "KTh" else "HT%d" % h])

            def prep_k():
                for i in range(16):
                    tsl = slice(i * 128, (i + 1) * 128)
                    b0, b1 = nb(0, 4), nb(0, 4)
                    for half, bnk in ((0, b0), (1, b1)):
                        MM(psb[bnk][:], CKG[:, tsl], WUK[:, half * 512:(half + 1) * 512], True, True, ["CQG", "WUK"], ["ps%d" % bnk])
                    for half, bnk in ((0, b0), (1, b1)):
                        P.op("act", lambda e, half=half, bnk=bnk, i=i: e.activation(out=QS[:, half * 512:(half + 1) * 512], in_=psb[bnk][:],
                                                                                   func=AF.Copy, scale=RS[:, 16 + i:17 + i]),
                             ["ps%d" % bnk, "RS"], ["QS"])
                    KV = QS[:].rearrange("p (h n) -> p h n", h=8)
                    CP("pool", VT[:, i, :, 0:64], fview(QS[:, 64:65], [(256, 4), (1, 64)]), ["QS"], ["VT"])
                    CP("pool", VT[:, i, :, 128:192], fview(QS[:, 192:193], [(256, 4), (1, 64)]), ["QS"], ["VT"])
                    K3 = QQ[:, 0:768].rearrange("p (h n) -> p h n", h=8)
                    CP("dve", K3[:, :, 0:64], KV[:, :, 0:64], ["QS"], ["QQ"])
                    CP("dve", K3[:, :, 64:96], fview(KR[:, i, :], [(0, 8), (1, 32)]), ["KR"], ["QQ"])
                    norm_rope_T(i, K3, gk, KTh, tsl, "KTh")

            def prep_q(qt):
                for i4 in range(4):
                    i = qt * 4 + i4
                    tsl = slice(i * 128, (i + 1) * 128)
                    b0, b1 = nb(0, 4), nb(0, 4)
                    for kc in range(2):
                        MM(psb[b0][:], CQG[:, kc, tsl], WUQ[:, kc, 0:512], kc == 0, kc == 1, ["CQG", "WUQ"], ["ps%d" % b0])
                    for kc in range(2):
                        MM(psb[b1][:, 0:256], CQG[:, kc, tsl], WUQ[:, kc, 512:768], kc == 0, kc == 1, ["CQG", "WUQ"], ["ps%d" % b1])
                    P.op("act", lambda e, i=i, b0=b0: e.activation(out=QQ[:, 0:512], in_=psb[b0][:], func=AF.Copy, scale=RS[:, i:i + 1]),
                         ["ps%d" % b0, "RS"], ["QQ"])
                    P.op("act", lambda e, i=i, b1=b1: e.activation(out=QQ[:, 512:768], in_=psb[b1][:, 0:256], func=AF.Copy, scale=RS[:, i:i + 1]),
                         ["ps%d" % b1, "RS"], ["QQ"])
                    Q3 = QQ[:, 0:768].rearrange("p (h n) -> p h n", h=8)
                    norm_rope_T(i, Q3, gq, QTh, slice(i4 * 128, (i4 + 1) * 128), "QTh")

            MEMSET("pool", VT[:, :, :, 64:128], 1.0, ["VT"])
            prep_k()
            P.fence()
            if CSTOP <= 2:
                return
            scale = 96.0 ** -0.5
            for qt in range(NT):
                prep_q(qt)
                if CSTOP <= 3:
                    return
                for h in range(8):
                    m, hb = h // 2, h % 2
                    bo = 4 + (h % 4)
                    ko = "ps%d" % bo
                    pend = None
                    for kt in range(17):
                        if kt < 16:
                            bs = nb(0, 4)
                            MM(psb[bs][:], KTh[0:96, h, kt * 128:(kt + 1) * 128], QTh[0:96, h, :], True, True, ["HT%d" % h, "QTh"], ["ps%d" % bs])
                            u = kt % 3
                            ACT(PT[:, u, :], psb[bs][:], AF.Exp, ["ps%d" % bs], ["PT%d" % u], scale=scale)
                        if pend is not None:
                            pk, pu = pend
                            lw = VT[:, pk, m, 0:128] if hb == 0 else VT[:, pk, m, 64:192]
                            MM(psb[bo][:, :], lw, PT[:, pu, :], pk == 0, pk == 15, ["VT", "PT%d" % pu], [ko])
                        pend = (kt, kt % 3) if kt < 16 else None
                    pn, pd = 64 * hb, 64 * (1 - hb)
                    RECIP(RD[pd:pd + 64, :], psb[bo][pd:pd + 64, :], [ko], ["RD%d" % hb])
                    TT("dve", YC[pn:pn + 64, m, :], psb[bo][pn:pn + 64, :], RD[pd:pd + 64, :], ALU.mult, [ko, "RD%d" % hb], ["YC%d" % m])
                    if h % 2 == 1:
                        ACT(YSQ[:, m, :], YC[:, m, :], AF.Square, ["YC%d" % m], ["YSQ%d" % m])
                b = nb(0, 4)
                for m in range(4):
                    MM(psb[b][:], ONESB[:], YSQ[:, m, :], m == 0, m == 3, ["ONESB", "YSQ%d" % m], ["ps%d" % b])
                rsqrt_bc(RBC[:], psb[b][:], 1.0 / 512, ["ps%d" % b, "SM0"], ["RBC"])
                for m in range(4):
                    STT("dve", YN[:, m, :], YC[:, m, :], PV[:, l, PV_GC + m:PV_GC + m + 1], RBC[:], ALU.mult, ALU.mult,
                        ["YC%d" % m, "PV", "RBC"], ["YSQ%d" % m])
                for dk in range(8):
                    b = nb(0, 4)
                    for m in range(4):
                        MM(psb[b][:], WOC[:, m, dk * 128:(dk + 1) * 128], YN[:, m, :], m == 0, m == 3, ["WOC", "YSQ%d" % m], ["ps%d" % b])
                    add_to_x(psb[b][:], dk, qt, ["ps%d" % b])

        def ffn(l):
            rmsnorm_to_HT(l, PV_GFFN, 16384)
            P.fence()
            moe = (l % 2 == 1)
            WS = [(arb(sl_ * 6144, 8 * 512).rearrange("p (k n) -> p k n", k=8),
                   arb(sl_ * 6144 + 2048, 8 * 512).rearrange("p (k n) -> p k n", k=8),
                   arb(sl_ * 6144 + 4096, 4 * D).rearrange("p (c n) -> p c n", c=4)) for sl_ in range(2)]
            o1 = 12288
            ACTB = [arb(o1 + a_ * 4096, 4 * T).rearrange("p (c t) -> p c t", c=4) for a_ in range(2)]
            o2 = o1 + 8192
            SG = arb(o2, 2 * 512).rearrange("p (u t) -> p u t", u=2)
            TU = arf(o2 + 512, 2 * 512).rearrange("p (u t) -> p u t", u=2)
            CBC = arb(o2 + 1536, 2 * T).rearrange("p (u t) -> p u t", u=2)
            o3 = o2 + 3584
            RB = arf(0, T)
            groups = []
            if not moe:
                nfull = D_FF // 512
                for g in range(nfull):
                    groups.append((None, g * 512, [128] * 4))
                groups.append((None, nfull * 512, [128, 64]))
            else:
                for e_ in range(NEXP):
                    for g in range(D_EXP // 512):
                        groups.append((e_, g * 512, [128] * 4))
            if moe:
                ot = 12288
                RG = arf(ot, 64).rearrange("p (k e) -> p k e", k=8)
                LG = arf(ot + 64, 128).rearrange("p (i e) -> p i e", i=16)
                MX = arf(ot + 192, 128).rearrange("p (i e) -> p i e", i=16)
                W12 = arf(ot + 320, 32).rearrange("p (a i) -> p a i", a=2)
                EQ = arf(ot + 352, 128).rearrange("p (i e) -> p i e", i=16)
                CMB = arf(ot + 480, 128).rearrange("p (i e) -> p i e", i=16)
                RSQ = arf(ot + 608, 16)
                SQB = arb(ot + 624, 128)
                CT = arf(o3, T)
                assert o3 + T <= ARW
                P.dma("sp", RG, rt_d[0].rearrange("(k p) e -> p k e", p=128), writes=["RG"])
                TT("dve", RG, RG, fview(PV[:, l, PV_GFFN:PV_GFFN + 8], [(1, 8), (0, 8)]), ALU.mult, ["RG", "PV"], ["RG"])
                bl = nb()
                for i in range(16):
                    for k in range(8):
                        MM(psb[bl][:, i * 8:(i + 1) * 8], X[:, k, i * 128:(i + 1) * 128], RG[:, k, :], k == 0, k == 7, ["X", "RG"], ["ps%d" % bl])
                br = nb()
                for i in range(16):
                    for k in range(8):
                        ACT(SQB[:], X[:, k, i * 128:(i + 1) * 128], AF.Square, ["X"], ["SQB"])
                        MM(psb[br][:, i:i + 1], SQB[:], ONESB[:, 0:1], k == 0, k == 7, ["SQB", "ONESB"], ["ps%d" % br])
                rsqrt_bc(RSQ[:], psb[br][:, 0:16], 1.0 / D, ["ps%d" % br, "SM0"], ["RSQ"])
                TT("dve", LG, psb[bl][:, 0:128].rearrange("p (i e) -> p i e", i=16), fview(RSQ, [(1, 16), (0, 8)]), ALU.mult,
                   ["ps%d" % bl, "RSQ"], ["LG"])
                for i in range(16):
                    P.op("dve", lambda e, i=i: e.max(out=MX[:, i, :], in_=LG[:, i, :]), ["LG"], ["MX"])
                TT("dve", W12[:, 0, :], MX[:, :, 0], MX[:, :, 1], ALU.subtract, ["MX"], ["W12"])
                TT("dve", W12[:, 1, :], MX[:, :, 1], MX[:, :, 0], ALU.subtract, ["MX"], ["W12"])
                ACT(W12[:].rearrange("p a i -> p (a i)"), W12[:].rearrange("p a i -> p (a i)"), AF.Sigmoid, ["W12"], ["W12"])
                TT("dve", EQ, LG, fview(MX[:, :, 0], [(8, 16), (0, 8)]), ALU.is_equal, ["LG", "MX"], ["EQ"])
                TT("dve", CMB, EQ, fview(W12[:, 0, :], [(1, 16), (0, 8)]), ALU.mult, ["EQ", "W12"], ["CMB"])
                TT("dve", EQ, LG, fview(MX[:, :, 1], [(8, 16), (0, 8)]), ALU.is_equal, ["LG", "MX", "CMB"], ["EQ"])
                TT("dve", EQ, EQ, fview(W12[:, 1, :], [(1, 16), (0, 8)]), ALU.mult, ["EQ", "W12"], ["EQ"])
                TT("dve", CMB, CMB, EQ, ALU.add, ["CMB", "EQ"], ["CMB"])
                for i in range(16):
                    bt = nb()
                    TR(psb[bt][0:8, 0:128], CMB[:, i, :], ident, ["CMB", "CONST"], ["ps%d" % bt])
                    CP("act", CT[0:8, i * 128:(i + 1) * 128], psb[bt][0:8, 0:128], ["ps%d" % bt], ["CT"])
            if moe:
                P.fence()
            rr = [0]

            def load13(gi):
                e_, c0, chunks = groups[gi]
                w1b, w3b, w2b = WS[gi % 2]
                n = sum(chunks)
                key = "WSA%d" % (gi % 2)
                if e_ is None:
                    P.dma("pool", w1b[:, :, 0:n], wgu_d[0, :, c0:c0 + n].rearrange("(k p) n -> p k n", p=128), writes=[key])
                    P.dma("pool", w3b[:, :, 0:n], wgu_d[0, :, D_FF + c0:D_FF + c0 + n].rearrange("(k p) n -> p k n", p=128), writes=[key])
                else:
                    P.dma("pool", w1b[:], w1_d[0, e_, :, c0:c0 + 512].rearrange("(k p) n -> p k n", p=128), writes=[key])
                    P.dma("pool", w3b[:], w3_d[0, e_, :, c0:c0 + 512].rearrange("(k p) n -> p k n", p=128), writes=[key])

            def load2(gi):
                e_, c0, chunks = groups[gi]
                w1b, w3b, w2b = WS[gi % 2]
                key = "WSB%d" % (gi % 2)
                if e_ is None:
                    r0 = c0
                    for ci, cs_ in enumerate(chunks):
                        if cs_ < 128:
                            MEMSET("pool", w2b[cs_:128, ci, :], 0.0, [key])
                            MEMSET("pool", ACTB[gi % 2][cs_:128, ci, :], 0.0, ["ACT%d" % (gi % 2)])
                        P.dma("pool", w2b[0:cs_, ci, :], wdn_d[0, r0:r0 + cs_, :], writes=[key])
                        r0 += cs_
                else:
                    P.dma("pool", w2b[:], w2_d[0, e_, c0:c0 + 512, :].rearrange("(c p) n -> p c n", p=128), writes=[key])

            def gu(gi):
                e_, c0, chunks = groups[gi]
                w1b, w3b, w2b = WS[gi % 2]
                key = "WSA%d" % (gi % 2)
                AB = ACTB[gi % 2]
                akey = "ACT%d" % (gi % 2)
                if e_ is not None and c0 == 0:
                    for tt in range(NT):
                        bt = nb(0, 4)
                        MM(psb[bt][:], fview(CONST[0:8, C_ID + e_:C_ID + e_ + 1], [(0, 128)]), CT[0:8, tt * 512:(tt + 1) * 512], True, True,
                           ["CONST", "CT"], ["ps%d" % bt])
                        CP("act", CBC[:, e_ % 2, tt * 512:(tt + 1) * 512], psb[bt][:], ["ps%d" % bt], ["CBC%d" % (e_ % 2)])
                off = 0
                for ci, cs_ in enumerate(chunks):
                    for tt in range(NT):
                        sl = slice(tt * 512, (tt + 1) * 512)
                        bg, bu = nb(0, 4), nb(0, 4)
                        for k in range(8):
                            MM(psb[bg][0:cs_, :], w1b[:, k, off:off + cs_], HT[:, k, sl], k == 0, k == 7, [key, "HT%d" % k], ["ps%d" % bg])
                        for k in range(8):
                            MM(psb[bu][0:cs_, :], w3b[:, k, off:off + cs_], HT[:, k, sl], k == 0, k == 7, [key, "HT%d" % k], ["ps%d" % bu])
                        u = rr[0] % 2
                        rr[0] += 1
                        ACT(SG[0:cs_, u, :], psb[bg][0:cs_, :], AF.Silu, ["ps%d" % bg], ["SG%d" % u])
                        if e_ is None:
                            TT("dve", AB[0:cs_, ci, sl], psb[bu][0:cs_, :], SG[0:cs_, u, :], ALU.mult, ["ps%d" % bu, "SG%d" % u], [akey])
                        else:
                            TT("dve", TU[:, u, :], psb[bu][:], CBC[:, e_ % 2, sl], ALU.mult, ["ps%d" % bu, "CBC%d" % (e_ % 2)], ["TU%d" % u])
                            TT("pool", AB[:, ci, sl], TU[:, u, :], SG[:, u, :], ALU.mult, ["TU%d" % u, "SG%d" % u], [akey])
                    off += cs_

            def down(gi):
                e_, c0, chunks = groups[gi]
                w1b, w3b, w2b = WS[gi % 2]
                key = "WSB%d" % (gi % 2)
                AB = ACTB[gi % 2]
                akey = "ACT%d" % (gi % 2)
                for dk in range(8):
                    for tt in range(NT):
                        b = nb(4, 8)
                        for ci, cs_ in enumerate(chunks):
                            MM(psb[b][:], w2b[:, ci, dk * 128:(dk + 1) * 128], AB[:, ci, tt * 512:(tt + 1) * 512],
                               ci == 0, ci == len(chunks) - 1, [key, akey], ["ps%d" % b])
                        add_to_x(psb[b][:], dk, tt, ["ps%d" % b])

            ng = len(groups)
            load13(0)
            load2(0)
            for gi in range(ng):
                if gi + 1 < ng:
                    load13(gi + 1)
                gu(gi)
                if gi >= 1:
                    down(gi - 1)
                if gi + 1 < ng:
                    load2(gi + 1)
            down(ng - 1)

        for s in range(nseq):
            load_x(s)
            P.fence()
            for l in range(2):
                if stop is not None and stop[1] == "norm":
                    rmsnorm_to_HT(l, PV_GMIX, 8832)
                    P.fence()
                    break
                mixer(l, s)
                if stop == (l, "mix"):
                    break
                ffn(l)
                P.fence()
                if stop == (l, "ffn"):
                    break
            store_x(s)
            P.fence()
        P.emit()
    return nc


def _consts():
    c = np.zeros((128, NCONST), np.float32)
    c[:, C_ID:C_ID + 128] = np.eye(128, dtype=np.float32)
    s = (np.arange(128) % 64)[:, None]
    t = np.arange(64)[None, :]
    c[:, C_MLOW:C_MLOW + 64] = (t >= s)
    c[:, C_MUP:C_MUP + 64] = (t <= s)
    bo = np.zeros((128, 128), np.float32)
    bo[:64, :64] = 1
    bo[64:, 64:] = 1
    c[:, C_BONES:C_BONES + 128] = bo
    inv = (10000.0 ** (-np.arange(0, 16, dtype=np.float32) * np.float32(2.0 / 32))).astype(np.float32)
    c[:, C_IFQ:C_IFQ + 16] = inv[None, :]
    return c


def _pack(inp):
    pv = np.zeros((2, 128, NPV), np.float32)
    pbq = np.zeros((2, 128, 192), np.float32)
    f = lambda a: np.asarray(a, np.float32)
    for l in range(2):
        pv[l, :, PV_GMIX:PV_GMIX + 8] = f(inp["norm_mix"][l]).reshape(8, 128).T
        pv[l, :, PV_GFFN:PV_GFFN + 8] = f(inp["norm_ffn"][l]).reshape(8, 128).T
        cw = f(inp["conv_w"][l])
        for j in range(2):
            pv[l, :, PV_CW + j * 4:PV_CW + j * 4 + 4] = cw[:, j * 128:(j + 1) * 128].T
            pv[l, :, PV_CB + j] = f(inp["conv_b"][l])[j * 128:(j + 1) * 128]
            pv[l, :, PV_GA + j] = f(inp["out_g_a"][l])[j * 128:(j + 1) * 128]
            pv[l, :, PV_GQ + j] = f(inp["mla_q_norm"][l])[j * 128:(j + 1) * 128]
            for d in range(2):
                pv[l, :, PV_BA + d * 2 + j] = f(inp["lru_ba"][l, d])[j * 128:(j + 1) * 128]
                pv[l, :, PV_BX + d * 2 + j] = f(inp["lru_bx"][l, d])[j * 128:(j + 1) * 128]
                pv[l, :, PV_LAM + d * 2 + j] = f(inp["lru_lam"][l, d])[j * 128:(j + 1) * 128]
                for ll in range(2):
                    pv[l, :, PV_LB + ll * 4 + d * 2 + j] = f(inp["hg_lb_logits"][ll, d])[j * 128:(j + 1) * 128]
        pv[l, :, PV_HGN] = np.tile(f(inp["hg_norm_g"][l]), 2)
        pv[l, :, PV_GKV] = f(inp["mla_kv_norm"][l])
        pv[l, :, PV_GC:PV_GC + 4] = f(inp["out_g_c"][l]).reshape(4, 128).T
        pbq[l, :, 0:96] = f(inp["qk_norm_q"][l])[None, :]
        pbq[l, :, 96:192] = f(inp["qk_norm_k"][l])[None, :]
    return pv, pbq


_NC_CACHE = {}


def kernel(**inp):
    ncores = 8
    nseq = 2
    key = (nseq, None)
    if key not in _NC_CACHE:
        _NC_CACHE[key] = build_nc(nseq, None)
    nc = _NC_CACHE[key]
    x = np.ascontiguousarray(np.asarray(inp["x"], np.float32))
    pos = np.asarray(inp["positions"], np.int32)
    pv, pbq = _pack(inp)
    consts = _consts()
    shared = {
        "consts": consts, "pv": pv, "pbq": pbq,
    }
    for nme in ("w_in", "lru_wa", "lru_wx", "mla_w_uq", "mla_w_ukv", "w_out", "ffn_w_gate_up", "ffn_w_down",
                "moe_router", "moe_w1", "moe_w3", "moe_w2"):
        shared[nme] = np.ascontiguousarray(np.asarray(inp[nme], np.float32))
    in_maps = []
    for c in range(ncores):
        m = dict(shared)
        m["x"] = x[c * nseq:(c + 1) * nseq]
        m["pos"] = np.ascontiguousarray(pos[c * nseq:(c + 1) * nseq].reshape(nseq, 16, 128).transpose(0, 2, 1))
        in_maps.append(m)
    res = run_bass_kernel_spmd(nc, in_maps, core_ids=list(range(ncores)))
    return np.concatenate([np.asarray(r["y"], np.float32) for r in res.results], axis=0)
```

```python
import contextlib
import numpy as np
import concourse.bass as bass
import concourse.mybir as mybir
from concourse.bass_utils import run_bass_kernel_spmd

F32 = mybir.dt.float32
BF16 = mybir.dt.bfloat16
I32 = mybir.dt.int32
ALU = mybir.AluOpType
AF = mybir.ActivationFunctionType
AX = mybir.AxisListType

T = 2048
D = 1024
NT = 4
EPS = 1e-6
D_IN = 2208
D_FF = 2752
D_EXP = 3584
NEXP = 8
ARW = 26112
CSTOP = 99


class _Op:
    __slots__ = ("eng", "fn", "deps", "signals", "ticket", "kind", "dsem", "dtarget", "prev_same_sem")

    def __init__(self, eng, fn, kind):
        self.eng = eng
        self.fn = fn
        self.deps = []
        self.signals = False
        self.ticket = None
        self.kind = kind
        self.dsem = None
        self.dtarget = None
        self.prev_same_sem = None


class Prog:
    ENGS = ("pe", "act", "dve", "pool", "sp")

    def __init__(self, nc, n_dma_sems=8):
        self.nc = nc
        self.ops = {e: [] for e in self.ENGS}
        self.last_w = {}
        self.readers = {}
        self.n_dma_sems = n_dma_sems
        self.dma_count = {e: 0 for e in self.ENGS}
        self.dma_last_on_sem = {}
        self.fence_deps = None
        self.fenced = set()

    def _add_dep(self, op, p):
        if p is None or p is op:
            return
        if p.eng == op.eng and p.kind == "c" and op.kind == "c" and p.eng == "pe":
            return
        op.deps.append(p)
        p.signals = True

    def fence(self):
        deps = []
        for e in self.ENGS:
            for o in reversed(self.ops[e]):
                if o.kind == "c":
                    deps.append(o)
                    break
        deps.extend(self.dma_last_on_sem.values())
        self.fence_deps = deps
        self.fenced = set()

    def op(self, eng, fn, reads=(), writes=(), kind="c"):
        o = _Op(eng, fn, kind)
        if eng != "pe":
            extra = [r for r in reads if isinstance(r, str) and r.startswith("ps") and r[2:].isdigit() and r not in writes]
            if extra:
                writes = list(writes) + extra
        if self.fence_deps is not None and eng not in self.fenced:
            self.fenced.add(eng)
            for p in self.fence_deps:
                if p.eng == eng and p.kind == "c":
                    continue
                o.deps.append(p)
                p.signals = True
        for r in reads:
            self._add_dep(o, self.last_w.get(r))
        for w in writes:
            self._add_dep(o, self.last_w.get(w))
            rd = self.readers.get(w)
            if rd:
                for p in rd.values():
                    self._add_dep(o, p)
        for r in reads:
            d = self.readers.setdefault(r, {})
            if kind == "c":
                d[eng] = o
            else:
                d[("dma", id(o))] = o
        for w in writes:
            self.last_w[w] = o
            self.readers[w] = {}
        if kind == "d":
            i = self.dma_count[eng]
            self.dma_count[eng] = i + 1
            slot = (eng, i % self.n_dma_sems)
            o.dsem = slot
            o.dtarget = 16 * (i // self.n_dma_sems + 1)
            o.prev_same_sem = self.dma_last_on_sem.get(slot)
            self.dma_last_on_sem[slot] = o
        self.ops[eng].append(o)
        return o

    def dma(self, eng, out, in_, reads=(), writes=(), **kw):
        return self.op(eng, lambda e: e.dma_start(out=out, in_=in_, **kw), reads, writes, kind="d")

    def emit(self, final_wait_eng="sp"):
        nc = self.nc
        with contextlib.ExitStack() as st:
            esem = {}
            for e in self.ENGS:
                if any(o.kind == "c" for o in self.ops[e]):
                    esem[e] = st.enter_context(nc.semaphore("s_" + e))
            dsem = {}
            for e in self.ENGS:
                for j in range(min(self.dma_count[e], self.n_dma_sems)):
                    dsem[(e, j)] = st.enter_context(nc.semaphore("d_%s%d" % (e, j)))
            for e in self.ENGS:
                t = 0
                for o in self.ops[e]:
                    if o.kind == "c" and o.signals:
                        t += 1
                        o.ticket = t
            final_dmas = list(self.dma_last_on_sem.values())
            block = st.enter_context(nc.Block())

            def make(e):
                def body(eng):
                    waited = {}

                    def wait(key, sem, val):
                        if waited.get(key, 0) >= val:
                            return
                        waited[key] = val
                        eng.wait_ge(sem, val)

                    for o in self.ops[e]:
                        for p in o.deps:
                            if p.kind == "c":
                                wait(("c", p.eng), esem[p.eng], p.ticket)
                            else:
                                wait(("d", p.dsem), dsem[p.dsem], p.dtarget)
                        if o.kind == "d" and o.prev_same_sem is not None:
                            p = o.prev_same_sem
                            wait(("d", p.dsem), dsem[p.dsem], p.dtarget)
                        ins = o.fn(eng)
                        if o.kind == "d":
                            ins.then_inc(dsem[o.dsem], 16)
                        elif o.signals:
                            ins.then_inc(esem[e], 1)
                    if e == final_wait_eng:
                        for p in final_dmas:
                            wait(("d", p.dsem), dsem[p.dsem], p.dtarget)
                return body

            names = {"pe": "tensor", "act": "scalar", "dve": "vector", "pool": "gpsimd", "sp": "sync"}
            for e in self.ENGS:
                if not self.ops[e] and e != final_wait_eng:
                    continue
                getattr(block, names[e])(make(e))


def rev_ap(a):
    (ps, pn), (s, n) = a.ap
    return bass.AP(a.tensor, a.offset + (n - 1) * s, [[ps, pn], [-s, n]])


def fview(a, dims):
    return bass.AP(a.tensor, a.offset, [list(a.ap[0])] + [list(d) for d in dims])


PV_GMIX, PV_GFFN, PV_CW, PV_CB, PV_BA, PV_BX, PV_LAM, PV_GA, PV_LB, PV_HGN, PV_GQ, PV_GKV, PV_GC = \
    0, 8, 16, 24, 26, 30, 34, 38, 40, 48, 49, 51, 52
NPV = 60
C_ID, C_MLOW, C_MUP, C_BONES, C_IFQ = 0, 128, 192, 256, 384
NCONST = C_IFQ + 16


def build_nc(nseq=2, stop=None, phases="abc"):
    nc = bass.Bass("TRN2", target_bir_lowering=False)
    dr = lambda n, s, dt=F32, kind="ExternalInput": nc.dram_tensor(n, list(s), dt, kind=kind).ap()
    x_d = dr("x", [nseq, T, D])
    pos_d = dr("pos", [nseq, 128, 16], I32)
    consts_d = dr("consts", [128, NCONST])
    pv_d = dr("pv", [2, 128, NPV])
    pbq_d = dr("pbq", [2, 128, 192])
    w_in_d = dr("w_in", [2, D, D_IN])
    wa_d = dr("lru_wa", [2, 2, 4, 64, 64])
    wx_d = dr("lru_wx", [2, 2, 4, 64, 64])
    wuq_d = dr("mla_w_uq", [2, 256, 768])
    wukv_d = dr("mla_w_ukv", [2, 128, 1024])
    wout_d = dr("w_out", [2, D, D])
    wgu_d = dr("ffn_w_gate_up", [1, D, 2 * D_FF])
    wdn_d = dr("ffn_w_down", [1, D_FF, D])
    rt_d = dr("moe_router", [1, D, NEXP])
    w1_d = dr("moe_w1", [1, NEXP, D, D_EXP])
    w3_d = dr("moe_w3", [1, NEXP, D, D_EXP])
    w2_d = dr("moe_w2", [1, NEXP, D_EXP, D])
    y_d = dr("y", [nseq, T, D], kind="ExternalOutput")

    with contextlib.ExitStack() as st:
        def sb(name, shape, dt=F32):
            return st.enter_context(nc.sbuf_tensor(name, list(shape), dt))

        X = sb("X", [128, 8, T])
        HT = sb("HT", [128, 8, T], BF16)
        AR = sb("AR", [128, ARW])
        CONST = sb("CONST", [128, NCONST])
        PV = sb("PV", [128, 2, NPV])
        PBQ = sb("PBQ", [128, 2, 192])
        IDB = sb("IDB", [128, 128], BF16)
        ONESB = sb("ONESB", [128, 128], BF16)
        BONESB = sb("BONESB", [128, 128], BF16)
        MASKS = sb("MASKS", [128, 2, 64])
        SM = sb("SM", [128, 512])
        ROPE = sb("ROPE", [128, 2, 16, 16])
        psb = [st.enter_context(nc.psum_tensor("ps%d" % i, [128, 512], F32)) for i in range(8)]

        P = Prog(nc)
        ident = CONST[:, C_ID:C_ID + 128]

        def MM(out, lhsT, rhs, start, stop_, r, w):
            P.op("pe", lambda e: e.matmul(out, lhsT=lhsT, rhs=rhs, start=start, stop=stop_), r, w)

        def TR(out, in_, idn, r, w):
            P.op("pe", lambda e: e.transpose(out, in_, idn), r, w)

        def ACT(out, in_, func, r, w, bias=0.0, scale=1.0):
            P.op("act", lambda e: e.activation(out=out, in_=in_, func=func, bias=bias, scale=scale), r, w)

        def TT(eng, out, in0, in1, op, r, w):
            P.op(eng, lambda e: e.tensor_tensor(out=out, in0=in0, in1=in1, op=op), r, w)

        def TS(eng, out, in0, s1, s2, op0, op1, r, w):
            P.op(eng, lambda e: e.tensor_scalar(out=out, in0=in0, scalar1=s1, scalar2=s2, op0=op0, op1=op1), r, w)

        def STT(eng, out, in0, scalar, in1, op0, op1, r, w):
            eng = "dve"
            P.op(eng, lambda e: e.scalar_tensor_tensor(out=out, in0=in0, scalar=scalar, in1=in1, op0=op0, op1=op1), r, w)

        def CP(eng, out, in_, r, w):
            if eng == "act":
                P.op("act", lambda e: e.copy(out=out, in_=in_), r, w)
            else:
                P.op(eng, lambda e: e.tensor_copy(out=out, in_=in_), r, w)

        def RECIP(out, in_, r, w):
            P.op("dve", lambda e: e.reciprocal(out=out, in_=in_), r, w)

        def MEMSET(eng, ap, val, w):
            P.op(eng, lambda e: e.memset(ap, val), (), w)

        bank_rr = [0]

        def nb(lo=0, hi=8):
            i = bank_rr[0]
            i = lo + (i - lo + 1) % (hi - lo) if lo <= i < hi else lo
            bank_rr[0] = i
            return i

        def arf(off, n):
            return AR[:, off:off + n]

        def arb(off, n):
            return AR[:, off:off + n // 2].bitcast(BF16)

        P.dma("sp", CONST[:], consts_d, writes=["CONST"])
        P.dma("sp", PV[:], pv_d.rearrange("l p n -> p l n"), writes=["PV"])
        P.dma("sp", PBQ[:], pbq_d.rearrange("l p n -> p l n"), writes=["PBQ"])
        CP("dve", IDB[:], ident, ["CONST"], ["IDB"])
        MEMSET("dve", ONESB[:], 1.0, ["ONESB"])
        CP("dve", BONESB[:], CONST[:, C_BONES:C_BONES + 128], ["CONST"], ["BONESB"])
        CP("dve", MASKS[:, 0, :], CONST[:, C_MLOW:C_MLOW + 64], ["CONST"], ["MASKS"])
        CP("dve", MASKS[:, 1, :], CONST[:, C_MUP:C_MUP + 64], ["CONST"], ["MASKS"])

        def rsqrt_bc(out, ps, scale, r, w):
            ACT(out, ps, AF.Sqrt, r, w, bias=SM[:, 0:1], scale=scale)
            RECIP(out, out, w, w)

        MEMSET("dve", SM[:, 0:1], EPS, ["SM0"])
        MEMSET("dve", SM[:, 1:2], 1.0, ["SM1"])

        def load_x(s):
            stg = [arf(0, 1024), arf(1024, 1024)]
            for i in range(16):
                sg = stg[i % 2]
                P.dma("sp", sg, x_d[s, i * 128:(i + 1) * 128, :], writes=["stg%d" % (i % 2)])
                for h in range(2):
                    b = nb()
                    for q in range(4):
                        k = h * 4 + q
                        TR(psb[b][:, q * 128:(q + 1) * 128], sg[:, k * 128:(k + 1) * 128], ident,
                           ["stg%d" % (i % 2), "CONST"], ["ps%d" % b])
                    eng = "act" if h == 0 else "dve"
                    CP(eng, X[:, h * 4:(h + 1) * 4, i * 128:(i + 1) * 128],
                       psb[b][:].rearrange("p (q t) -> p q t", q=4), ["ps%d" % b], ["X"])

        def store_x(s):
            stg = [arf(0, 1024), arf(1024, 1024)]
            for i in range(16):
                sg = stg[i % 2]
                for h in range(2):
                    b = nb()
                    for q in range(4):
                        k = h * 4 + q
                        TR(psb[b][:, q * 128:(q + 1) * 128], X[:, k, i * 128:(i + 1) * 128], ident,
                           ["X", "CONST"], ["ps%d" % b])
                    eng = "act" if h == 0 else "dve"
                    CP(eng, sg[:, h * 512:(h + 1) * 512], psb[b][:], ["ps%d" % b], ["stg%d" % (i % 2)])
                P.dma("sp", y_d[s, i * 128:(i + 1) * 128, :], sg, reads=["stg%d" % (i % 2)])

        def rmsnorm_to_HT(l, gcol, scr_off):
            RB = arf(scr_off, T)
            for k in range(8):
                ACT(HT[:, k, :], X[:, k, :], AF.Square, ["X"], ["HT%d" % k])
            for tt in range(NT):
                b = nb()
                for k in range(8):
                    MM(psb[b][:], ONESB[:], HT[:, k, tt * 512:(tt + 1) * 512], k == 0, k == 7,
                       ["ONESB", "HT%d" % k], ["ps%d" % b])
                rsqrt_bc(RB[:, tt * 512:(tt + 1) * 512], psb[b][:], 1.0 / D, ["ps%d" % b, "SM0"], ["RB%d" % tt])
            for k in range(8):
                eng = "dve" if k % 2 == 0 else "pool"
                STT(eng, HT[:, k, :], X[:, k, :], PV[:, l, gcol + k:gcol + k + 1], RB[:], ALU.mult, ALU.mult,
                    ["X", "PV"] + ["RB%d" % t_ for t_ in range(NT)], ["HT%d" % k])

        HTk = ["HT%d" % k for k in range(8)]

        def proj_fm(WIN, col0, m, tt, b, pbase=0):
            for k in range(8):
                MM(psb[b][pbase:pbase + m, :], WIN[:, k, col0:col0 + m], HT[:, k, tt * 512:(tt + 1) * 512],
                   k == 0, k == 7, ["WIN", "HT%d" % k], ["ps%d" % b])

        def add_to_x(ps, dk, tt, r):
            TT("dve", X[:, dk, tt * 512:(tt + 1) * 512], ps, X[:, dk, tt * 512:(tt + 1) * 512], ALU.add,
               r + ["X"], ["X"])

        def mixer(l, s):
            WIN = arb(0, 8 * D_IN).rearrange("p (k n) -> p k n", k=8)
            P.dma("pool", WIN[:, 0:4, :], w_in_d[l, 0:512, :].rearrange("(k p) n -> p k n", p=128), writes=["WIN"])
            P.dma("pool", WIN[:, 4:8, :], w_in_d[l, 512:1024, :].rearrange("(k p) n -> p k n", p=128), writes=["WIN"])
            SC0 = 8832
            rmsnorm_to_HT(l, PV_GMIX, SC0)
            if "a" in phases:
                phase_a(l, WIN, SC0)
                P.fence()
            if "b" in phases:
                phase_b(l, WIN, SC0)
                P.fence()
            if "c" in phases:
                phase_c(l, s, WIN, SC0)
                P.fence()

        def phase_a(l, WIN, o0):
            S = [arf(o0 + i * T, T) for i in range(6)]
            XCB = arb(o0 + 6 * T, T)
            YAB = [arb(o0 + 6 * T + 1024 + j * 1024, T) for j in range(2)]
            WOA = arb(o0 + 6 * T + 3072, 2 * D).rearrange("p (k n) -> p k n", k=2)
            BD = arb(o0 + 6 * T + 3072 + 1024, 8 * 128).rearrange("p (g n) -> p g n", g=8)
            P.dma("pool", WOA, wout_d[l, 0:256, :].rearrange("(k p) n -> p k n", p=128), writes=["WOA"])
            MEMSET("pool", BD, 0.0, ["BD"])
            for g, wd in enumerate((wa_d, wx_d)):
                for d in range(2):
                    for j in range(2):
                        gi = g * 4 + d * 2 + j
                        for hb in range(2):
                            P.dma("pool", BD[hb * 64:(hb + 1) * 64, gi, hb * 64:(hb + 1) * 64],
                                  wd[l, d, 2 * j + hb], writes=["BD"])
            ACT(SM[:, 8:12], PV[:, l, PV_LAM:PV_LAM + 4], AF.Exp, ["PV"], ["SMA"], scale=-1.0)
            ACT(SM[:, 8:12], SM[:, 8:12], AF.Ln, ["SMA"], ["SMA"], bias=SM[:, 1:2], scale=1.0)
            TS("dve", SM[:, 12:16], SM[:, 8:12], -16.0, None, ALU.mult, ALU.mult, ["SMA"], ["SMB"]) if False else None
            P.op("dve", lambda e: e.tensor_scalar_mul(out=SM[:, 12:16], in0=SM[:, 8:12], scalar1=-16.0), ["SMA"], ["SMB"])
            P.op("dve", lambda e: e.tensor_scalar_mul(out=SM[:, 8:12], in0=SM[:, 8:12], scalar1=-8.0), ["SMA", "SMB"], ["SMA"])
            for j in range(2):
                XA, XC, R, I, A2, HF = S
                cw = lambda kk: PV[:, l, PV_CW + j * 4 + kk:PV_CW + j * 4 + kk + 1]
                for tt in range(NT):
                    b = nb()
                    proj_fm(WIN, j * 128, 128, tt, b)
                    CP("act", XA[:, tt * 512:(tt + 1) * 512], psb[b][:], ["ps%d" % b], ["XA"])
                TS("dve", XC[:], XA[:], cw(2), PV[:, l, PV_CB + j:PV_CB + j + 1], ALU.mult, ALU.add, ["XA", "PV"], ["XC"])
                STT("dve", XC[:, 2:T], XA[:, 0:T - 2], cw(0), XC[:, 2:T], ALU.mult, ALU.add, ["XA", "PV", "XC"], ["XC"])
                STT("dve", XC[:, 1:T], XA[:, 0:T - 1], cw(1), XC[:, 1:T], ALU.mult, ALU.add, ["XA", "PV", "XC"], ["XC"])
                STT("dve", XC[:, 0:T - 1], XA[:, 1:T], cw(3), XC[:, 0:T - 1], ALU.mult, ALU.add, ["XA", "PV", "XC"], ["XC"])
                CP("pool", XCB[:], XC[:], ["XC"], ["XCB"])
                HB = XA
                for d in range(2):
                    for tt in range(NT):
                        sl = slice(tt * 512, (tt + 1) * 512)
                        b = nb()
                        MM(psb[b][:], BD[:, 0 * 4 + d * 2 + j, :], XCB[:, sl], True, True, ["BD", "XCB"], ["ps%d" % b])
                        ACT(R[:, sl], psb[b][:], AF.Sigmoid, ["ps%d" % b, "PV"], ["R"],
                            bias=PV[:, l, PV_BA + d * 2 + j:PV_BA + d * 2 + j + 1])
                        b = nb()
                        MM(psb[b][:], BD[:, 1 * 4 + d * 2 + j, :], XCB[:, sl], True, True, ["BD", "XCB"], ["ps%d" % b])
                        ACT(I[:, sl], psb[b][:], AF.Sigmoid, ["ps%d" % b, "PV"], ["I"],
                            bias=PV[:, l, PV_BX + d * 2 + j:PV_BX + d * 2 + j + 1])
                    ci = d * 2 + j
                    ACT(A2[:], R[:], AF.Exp, ["R", "SMB"], ["A2"], scale=SM[:, 12 + ci:13 + ci])
                    ACT(R[:], R[:], AF.Exp, ["R", "SMA"], ["R"], scale=SM[:, 8 + ci:9 + ci])
                    TS("dve", A2[:], A2[:], -1.0, 1.0, ALU.mult, ALU.add, ["A2"], ["A2"])
                    P.op("dve", lambda e: e.tensor_scalar_max(out=A2[:], in0=A2[:], scalar1=0.0), ["A2"], ["A2"])
                    ACT(A2[:], A2[:], AF.Sqrt, ["A2"], ["A2"])
                    TT("pool", I[:], I[:], XC[:], ALU.mult, ["I", "XC"], ["I"])
                    TT("dve", I[:], I[:], A2[:], ALU.mult, ["I", "A2"], ["I"])
                    if d == 0:
                        P.op("dve", lambda e: e.tensor_tensor_scan(out=HF[:], data0=R[:], data1=I[:], initial=0.0,
                                                                  op0=ALU.mult, op1=ALU.add), ["R", "I"], ["HF"])
                    else:
                        P.op("dve", lambda e: e.tensor_tensor_scan(out=rev_ap(HB[:]), data0=rev_ap(R[:]), data1=rev_ap(I[:]),
                                                                  initial=0.0, op0=ALU.mult, op1=ALU.add), ["R", "I", "XA"], ["XA"])
                TT("pool", HF[:], HF[:], HB[:], ALU.add, ["HF", "XA"], ["HF"])
                G, G2 = R, I
                for tt in range(NT):
                    b = nb()
                    proj_fm(WIN, 256 + j * 128, 128, tt, b)
                    CP("act", G[:, tt * 512:(tt + 1) * 512], psb[b][:], ["ps%d" % b], ["R"])
                TT("pool", G2[:], G[:], G[:], ALU.mult, ["R"], ["I"])
                TS("dve", G2[:], G2[:], 0.044715, 1.0, ALU.mult, ALU.add, ["I"], ["I"])
                TT("pool", G2[:], G2[:], G[:], ALU.mult, ["I", "R"], ["I"])
                ACT(G2[:], G2[:], AF.Sigmoid, ["I"], ["I"], scale=1.5957691216057308)
                TT("dve", G[:], G[:], G2[:], ALU.mult, ["R", "I"], ["R"])
                TT("dve", YAB[j][:], HF[:], G[:], ALU.mult, ["HF", "R"], ["YAB%d" % j])
            SQ = [arb(o0 + jj * 1024, T) for jj in range(2)]
            RB = S[1]
            for j in range(2):
                ACT(SQ[j][:], YAB[j][:], AF.Square, ["YAB%d" % j], ["XA"])
            for tt in range(NT):
                sl = slice(tt * 512, (tt + 1) * 512)
                b = nb()
                for j in range(2):
                    MM(psb[b][:], ONESB[:], SQ[j][:, sl], j == 0, j == 1, ["ONESB", "XA"], ["ps%d" % b])
                rsqrt_bc(RB[:, sl], psb[b][:], 1.0 / 256, ["ps%d" % b, "SM0"], ["XC"])
            for j in range(2):
                STT("dve", YAB[j][:], YAB[j][:], PV[:, l, PV_GA + j:PV_GA + j + 1], RB[:], ALU.mult, ALU.mult,
                    ["YAB%d" % j, "PV", "XC"], ["YAB%d" % j])
            for dk in range(8):
                for tt in range(NT):
                    b = nb()
                    for j in range(2):
                        MM(psb[b][:], WOA[:, j, dk * 128:(dk + 1) * 128], YAB[j][:, tt * 512:(tt + 1) * 512],
                           j == 0, j == 1, ["WOA", "YAB%d" % j], ["ps%d" % b])
                    add_to_x(psb[b][:], dk, tt, ["ps%d" % b])

        def phase_b(l, WIN, o0):
            HN = 1024
            bb = [arf(o0 + i * HN, HN) for i in range(4)]
            o1 = o0 + 4 * HN
            QT = arb(o1, T)
            KZ = [arb(o1 + 1024 + hh * 1024, T) for hh in range(2)]
            KTOK = arb(o1 + 3072, T).rearrange("p (i n) -> p i n", i=16)
            VZ = [arb(o1 + 4096 + cp * 1024, T).rearrange("p (i n) -> p i n", i=16) for cp in range(2)]
            o3 = o1 + 6144
            O = arf(o3, T)
            UF = arf(o3 + 2048, 2048)
            SBFall = arb(o3 + 2048, 32 * 128).rearrange("p (r n) -> p r n", r=32)
            D3 = arb(o3 + 4096, 2048)
            SALL = arb(o3 + 5120, 2048)
            WOB = arb(o3 + 6144, D)
            SCB = arb(o3 + 6656, 4 * 128).rearrange("p (u n) -> p u n", u=4)
            assert o3 + 6912 <= ARW
            YB = arb(o1 + 3072, T)
            FAC = SM[:, 64:224].rearrange("p (f c) -> p f c", f=5)
            FACR = SM[:, 224:320].rearrange("p (f c) -> p f c", f=3)
            MEMSET("pool", KZ[0][64:128, :], 0.0, ["KZ"])
            MEMSET("pool", KZ[1][0:64, :], 0.0, ["KZ"])
            MEMSET("pool", VZ[0][64:128, :, :], 0.0, ["VZ"])
            MEMSET("pool", VZ[1][0:64, :, :], 0.0, ["VZ"])
            MEMSET("pool", SCB[64:128, 0:2, :], 0.0, ["SCB0", "SCB1"])
            MEMSET("pool", SCB[0:64, 2:4, :], 0.0, ["SCB2", "SCB3"])
            LBT = SM[:, 16:32]
            ACT(LBT[:, 0:8], PV[:, l, PV_LB:PV_LB + 8], AF.Exp, ["PV"], ["LBT"])
            TT("dve", LBT[:, 8:12], LBT[:, 0:4], LBT[:, 4:8], ALU.add, ["LBT"], ["LBT"])
            RECIP(LBT[:, 8:12], LBT[:, 8:12], ["LBT"], ["LBT"])
            if l == 0:
                MEMSET("dve", LBT[:, 12:16], 0.0, ["LBT"])
            else:
                TT("dve", LBT[:, 12:16], LBT[:, 4:8], LBT[:, 8:12], ALU.mult, ["LBT"], ["LBT"])
            TS("dve", LBT[:, 8:12], LBT[:, 12:16], -1.0, 1.0, ALU.mult, ALU.add, ["LBT"], ["LBT"])
            for jp in range(2):
                P.dma("pool", WOB, wout_d[l, 256 + jp * 128:256 + (jp + 1) * 128, :], writes=["WOB"])
                for i in range(16):
                    b = nb(0, 4)
                    for k in range(8):
                        MM(psb[b][:, 0:128], HT[:, k, i * 128:(i + 1) * 128], WIN[:, k, 1280 + jp * 128:1280 + (jp + 1) * 128],
                           k == 0, k == 7, ["WIN", "HT%d" % k], ["ps%d" % b])
                    CP("act", VZ[0][0:64, i, :], psb[b][0:64, 0:128], ["ps%d" % b], ["VZ"])
                    CP("dve", VZ[1][64:128, i, :], psb[b][64:128, 0:128], ["ps%d" % b], ["VZ"])
                for d in range(2):
                    zc = (768 if d == 0 else 1024) + jp * 128
                    lbc = d * 2 + jp
                    for hf in range(2):
                        t0 = hf * HN
                        F_, LG_, B_, X_ = bb
                        for t2 in range(2):
                            tt = hf * 2 + t2
                            sl = slice(t2 * 512, (t2 + 1) * 512)
                            b = nb(0, 4)
                            proj_fm(WIN, zc, 128, tt, b)
                            ACT(F_[:, sl], psb[b][:], AF.Sigmoid, ["ps%d" % b], ["bF"])
                        TS("dve", F_[:], F_[:], LBT[:, 8 + lbc:9 + lbc], LBT[:, 12 + lbc:13 + lbc], ALU.mult, ALU.add,
                           ["bF", "LBT"], ["bF"])
                        ACT(LG_[:], F_[:], AF.Ln, ["bF"], ["bL"])
                        TS("pool", F_[:], F_[:], -1.0, 1.0, ALU.mult, ALU.add, ["bF"], ["bF"])
                        init = 0.0 if hf == 0 else SM[:, 40:41]
                        ones_bc = fview(SM[:, 1:2], [(0, HN)])
                        P.op("dve", lambda e, init=init, B_=B_, LG_=LG_, ones_bc=ones_bc: e.tensor_tensor_scan(
                            out=B_[:], data0=ones_bc, data1=LG_[:], initial=init, op0=ALU.mult, op1=ALU.add),
                            ["SM1", "bL", "SMc"], ["bB"])
                        CP("dve", SM[:, 40:41], B_[:, HN - 1:HN], ["bB"], ["SMc"])
                        B3 = B_[:].rearrange("p (c n) -> p c n", n=64)
                        LG3 = LG_[:].rearrange("p (c n) -> p c n", n=64)
                        c0 = hf * 16
                        if d == 1:
                            CP("dve", FAC[:, 2, c0:c0 + 16], B3[:, :, 63], ["bB"], ["FAC"])
                            TT("dve", B_[:], B_[:], LG_[:], ALU.subtract, ["bB", "bL"], ["bB"])
                        CP("dve", FAC[:, 0, c0:c0 + 16], B3[:, :, 32], ["bB"], ["FAC"])
                        if d == 0:
                            CP("dve", FAC[:, 1, c0:c0 + 16], B3[:, :, 63], ["bB"], ["FAC"])
                        else:
                            CP("dve", FAC[:, 1, c0:c0 + 16], B3[:, :, 0], ["bB"], ["FAC"])
                        TT("dve", B3, B3, FAC[:, 0, c0:c0 + 16].to_broadcast([128, 16, 64]), ALU.subtract,
                           ["bB", "FAC"], ["bB"])
                        sgn_q = 1.0 if d == 0 else -1.0
                        ACT(LG_[:], B_[:], AF.Exp, ["bB"], ["bL"], scale=sgn_q)
                        for t2 in range(2):
                            tt = hf * 2 + t2
                            sl = slice(t2 * 512, (t2 + 1) * 512)
                            b = nb(0, 4)
                            proj_fm(WIN, 512 + jp * 128, 128, tt, b)
                            ACT(X_[:, sl], psb[b][:], AF.Silu, ["ps%d" % b], ["bX"])
                        STT("dve", QT[:, t0:t0 + HN], X_[:], 0.125, LG_[:], ALU.mult, ALU.mult, ["bX", "bL"], ["QT"])
                        ACT(LG_[:], B_[:], AF.Exp, ["bB", "QT"], ["bL"], scale=-sgn_q)
                        TT("pool", KZ[0][0:64, t0:t0 + HN], F_[0:64, :], LG_[0:64, :], ALU.mult, ["bF", "bL"], ["KZ"])
                        TT("dve", KZ[1][64:128, t0:t0 + HN], F_[64:128, :], LG_[64:128, :], ALU.mult, ["bF", "bL"], ["KZ"])
                    Fm, Fl = FAC[:, 0, :], FAC[:, 1, :]
                    if d == 0:
                        CP("dve", FAC[:, 2, 0:1], Fm[:, 0:1], ["FAC"], ["FAC"])
                        TT("dve", FAC[:, 2, 1:32], Fm[:, 1:32], Fl[:, 0:31], ALU.subtract, ["FAC"], ["FAC"])
                        TT("dve", FAC[:, 3, :], Fl, Fm, ALU.subtract, ["FAC"], ["FAC"])
                        CP("dve", FAC[:, 4, 0:1], Fl[:, 0:1], ["FAC"], ["FAC"])
                        TT("dve", FAC[:, 4, 1:32], Fl[:, 1:32], Fl[:, 0:31], ALU.subtract, ["FAC"], ["FAC"])
                    else:
                        TT("dve", FAC[:, 3, :], Fm, Fl, ALU.subtract, ["FAC"], ["FAC"])
                        TT("dve", FAC[:, 4, :], FAC[:, 2, :], Fl, ALU.subtract, ["FAC"], ["FAC"])
                        TT("dve", FAC[:, 2, :], FAC[:, 2, :], Fm, ALU.subtract, ["FAC"], ["FAC"])
                    ACT(FAC[:, 2:5, :], FAC[:, 2:5, :], AF.Exp, ["FAC"], ["FAC"])
                    for i in range(16):
                        b = nb(0, 4)
                        for hh in range(2):
                            MM(psb[b][:, 0:128], KZ[hh][:, i * 128:(i + 1) * 128], IDB[:], hh == 0, hh == 1, ["KZ", "IDB"], ["ps%d" % b])
                        CP("act" if i % 2 else "dve", KTOK[:, i, :], psb[b][:, 0:128], ["ps%d" % b], ["KTOK"])
                    if d == 0:
                        CP("dve", FACR[:, :, :], FAC[:, 2:5, :], ["FAC"], ["FACR"])
                    else:
                        f0 = FAC[:, 2, 31:32]
                        CP("dve", FACR[:, :, :], bass.AP(f0.tensor, f0.offset, [list(f0.ap[0]), [32, 3], [-1, 32]]), ["FAC"], ["FACR"])
                    MEMSET("dve", FACR[:, 2, 0:1], 0.0, ["FACR"])
                    CP("dve", D3[:].rearrange("p (e r) -> p e r", r=32), fview(FACR[:, 2, :], [(0, 64), (1, 32)]), ["FACR"], ["D3"])
                    for g in range(8):
                        b = nb(0, 4)
                        for q in range(4):
                            r = g * 4 + q
                            c = r if d == 0 else 31 - r
                            i, cp = c // 2, c % 2
                            MM(psb[b][:, q * 128:(q + 1) * 128], KTOK[:, i, :], VZ[cp][:, i, :], True, True, ["KTOK", "VZ"], ["ps%d" % b])
                        for hh in range(2):
                            pb = 64 * hh
                            src = fview(psb[b][pb:pb + 64, hh * 64:hh * 64 + 1], [(128, 4), (1, 64)])
                            dst = fview(UF[pb:pb + 64, g * 4:g * 4 + 1], [(1, 4), (32, 64)])
                            CP("act" if hh == 0 else "dve", dst, src, ["ps%d" % b], ["UF"])
                    UF3 = UF[:].rearrange("p (e r) -> p e r", r=32)
                    TT("dve", UF3, UF3, fview(FACR[:, 1, :], [(0, 64), (1, 32)]), ALU.mult, ["UF", "FACR"], ["UF"])
                    P.op("dve", lambda e: e.tensor_tensor_scan(out=SALL[:], data0=D3[:], data1=UF[:], initial=0.0,
                                                              op0=ALU.mult, op1=ALU.add), ["D3", "UF"], ["SALL"])
                    MEMSET("pool", SBFall[0:64, :, 64:128], 0.0, ["UF"])
                    MEMSET("pool", SBFall[64:128, :, 0:64], 0.0, ["UF"])
                    for hh in range(2):
                        pb = 64 * hh
                        src = fview(SALL[pb:pb + 64, 0:1], [(1, 31), (32, 64)])
                        TT("dve", SBFall[pb:pb + 64, 1:32, hh * 64:(hh + 1) * 64], src,
                           fview(FACR[pb:pb + 64, 0, 1:2], [(1, 31), (0, 64)]), ALU.mult, ["SALL", "FACR"], ["UF"])
                    pso_b = None
                    for r in range(32):
                        c = r if d == 0 else 31 - r
                        i, cp = c // 2, c % 2
                        tb = 64 * cp
                        cs = slice(c * 64, (c + 1) * 64)
                        if r % 8 == 0:
                            pso_b = 4 + (r // 8) % 4
                        pso = psb[pso_b]
                        ocol = (c % 8) * 64
                        su = cp * 2 + (r // 2) % 2
                        bs = nb(0, 4)
                        for hh in range(2):
                            MM(psb[bs][tb:tb + 64, hh * 64:(hh + 1) * 64], KZ[hh][:, cs], QT[:, cs], True, True,
                               ["KZ", "QT"], ["ps%d" % bs])
                        TT("dve", SCB[tb:tb + 64, su, :].rearrange("p (h n) -> p h n", h=2),
                           psb[bs][tb:tb + 64, 0:128].rearrange("p (h n) -> p h n", h=2),
                           fview(MASKS[tb:tb + 64, d, :], [(0, 2), (1, 64)]), ALU.mult,
                           ["ps%d" % bs, "MASKS"], ["SCB%d" % su])
                        for hh in range(2):
                            pb = 64 * hh
                            MM(pso[pb:pb + 64, ocol:ocol + 64], VZ[cp][:, i, pb:pb + 64], SCB[:, su, hh * 64:(hh + 1) * 64],
                               True, r == 0, ["VZ", "SCB%d" % su], ["ps%d" % pso_b])
                        if r > 0:
                            MM(pso[:, ocol:ocol + 64], SBFall[:, r, :], QT[:, cs], False, True, ["UF", "QT"], ["ps%d" % pso_b])
                        if r % 8 == 7:
                            g0 = (c // 8) * 512
                            if d == 0:
                                CP("act", O[:, g0:g0 + 512], pso[:], ["ps%d" % pso_b], ["O"])
                            else:
                                TT("dve", O[:, g0:g0 + 512], pso[:], O[:, g0:g0 + 512], ALU.add, ["ps%d" % pso_b, "O"], ["O"])
                SQ = arb(o0 + 2048, T)
                RB = arf(o0, T)
                GG = arf(o0 + 3072, 1024)
                ACT(SQ[:], O[:], AF.Square, ["O"], ["bB"])
                for tt in range(NT):
                    sl = slice(tt * 512, (tt + 1) * 512)
                    b = nb(0, 4)
                    MM(psb[b][:], BONESB[:], SQ[:, sl], True, True, ["BONESB", "bB"], ["ps%d" % b])
                    rsqrt_bc(RB[:, sl], psb[b][:], 1.0 / 64, ["ps%d" % b, "SM0"], ["bF", "bL"])
                    STT("dve", O[:, sl], O[:, sl], PV[:, l, PV_HGN:PV_HGN + 1], RB[:, sl], ALU.mult, ALU.mult, ["O", "PV", "bF", "bL"], ["O"])
                    b = nb(0, 4)
                    proj_fm(WIN, 1536 + jp * 128, 128, tt, b)
                    ACT(GG[:, 0:512], psb[b][:], AF.Silu, ["ps%d" % b], ["bX"])
                    TT("dve", YB[:, sl], O[:, sl], GG[:, 0:512], ALU.mult, ["O", "bX"], ["KTOK"])
                for dk in range(8):
                    for tt in range(NT):
                        b = nb(0, 4)
                        MM(psb[b][:], WOB[:, dk * 128:(dk + 1) * 128], YB[:, tt * 512:(tt + 1) * 512], True, True,
                           ["WOB", "KTOK"], ["ps%d" % b])
                        add_to_x(psb[b][:], dk, tt, ["ps%d" % b])
                for kk_ in ("bF", "bL", "bB", "bX"):
                    pass

        def rope_tables(s, R0):
            PI = arf(R0, 16).bitcast(I32)
            PF = arf(R0 + 16, 16)
            ANG = arf(R0 + 32, 512).rearrange("p (c i f) -> p c i f", c=2, i=16)
            KK = arf(R0 + 544, 512)
            KI = arf(R0 + 1056, 512).bitcast(I32)
            P.dma("sp", PI, pos_d[s], writes=["PI"])
            CP("dve", PF, PI, ["PI"], ["PF"])
            ifq = CONST[:, C_IFQ:C_IFQ + 16]
            TT("dve", ANG[:, 1], fview(PF, [(1, 16), (0, 16)]), fview(ifq, [(0, 16), (1, 16)]), ALU.mult, ["PF", "CONST"], ["ANG"])
            P.op("dve", lambda e: e.tensor_scalar_add(out=ANG[:, 0], in0=ANG[:, 1], scalar1=float(np.pi / 2)), ["ANG"], ["ANG"])
            A = ANG[:].rearrange("p c i f -> p (c i f)")
            K = KK
            TS("dve", K, A, float(1.0 / (2 * np.pi)), 0.5, ALU.mult, ALU.add, ["ANG"], ["KK"])
            CP("dve", KI, K, ["KK"], ["KI"])
            CP("dve", K, KI, ["KI"], ["KK"])
            C1 = 6.28125
            C2 = float(2 * np.pi - 6.28125)
            STT("dve", A, K, -C1, A, ALU.mult, ALU.add, ["KK", "ANG"], ["ANG"])
            STT("dve", A, K, -C2, A, ALU.mult, ALU.add, ["KK", "ANG"], ["ANG"])
            TS("dve", K, A, float(np.pi), float(-2 * np.pi), ALU.is_gt, ALU.mult, ["ANG"], ["KK"])
            TT("dve", A, A, K, ALU.add, ["ANG", "KK"], ["ANG"])
            TS("dve", K, A, float(-np.pi), float(2 * np.pi), ALU.is_lt, ALU.mult, ["ANG"], ["KK"])
            TT("dve", A, A, K, ALU.add, ["ANG", "KK"], ["ANG"])
            P.op("dve", lambda e: e.tensor_scalar_min(out=A, in0=A, scalar1=3.1415925), ["ANG"], ["ANG"])
            P.op("dve", lambda e: e.tensor_scalar_max(out=A, in0=A, scalar1=-3.1415925), ["ANG"], ["ANG"])
            ACT(ROPE[:].rearrange("p c i f -> p (c i f)"), A, AF.Sin, ["ANG"], ["ROPE"])

        def phase_c(l, s, WIN, base):
            KTh = HT
            VT = arb(0, 16 * 4 * 192).rearrange("p (i m e) -> p i m e", i=16, m=4)
            RD = arf(6144, 512)
            CQG = arb(base + 4096, 2 * T).rearrange("p (k t) -> p k t", k=2)
            WUQ = arb(base + 6144, 2 * 768).rearrange("p (k n) -> p k n", k=2)
            WOC = arb(base + 6912, 4 * D).rearrange("p (m n) -> p m n", m=4)
            sm0 = base + 8960
            RS = arf(sm0, 64)
            T8 = arf(sm0 + 64, 64)
            TA = arf(sm0 + 128, 128)
            TB = arf(sm0 + 256, 128)
            QS = arf(sm0 + 384, 1024)
            QQ = arf(sm0 + 1408, 1024)
            QB = arb(sm0 + 2432, 1024)
            KB = QB
            R0 = sm0 + 2944
            assert R0 + 5376 <= ARW, R0
            CKG = arb(R0, T)
            CSQ = arb(R0 + 1024, 3 * T).rearrange("p (k t) -> p k t", k=3)
            WUK = arb(R0 + 4096, 1024)
            KR = arf(R0 + 4608, 512).rearrange("p (i n) -> p i n", i=16)
            QTh = arb(R0, 8 * 512).rearrange("p (h t) -> p h t", h=8)
            PT = arb(R0 + 2048, 3 * 512).rearrange("p (u t) -> p u t", u=3)
            YC = arb(R0 + 2816, 4 * 512).rearrange("p (m t) -> p m t", m=4)
            YSQ = arb(R0 + 3840, 4 * 512).rearrange("p (m t) -> p m t", m=4)
            YN = YSQ
            RBC = arf(R0 + 4864, 512)

            if l == 0:
                rope_tables(s, R0)
                P.fence()
            if CSTOP <= 0.1:
                return
            P.dma("pool", WUQ, wuq_d[l].rearrange("(k p) n -> p k n", p=128), writes=["WUQ"])
            P.dma("pool", WUK, wukv_d[l], writes=["WUK"])
            P.dma("pool", WOC, wout_d[l, 512:1024, :].rearrange("(m p) n -> p m n", p=128), writes=["WOC"])
            for kc in range(3):
                col = 1792 + kc * 128
                for tt in range(NT):
                    sl = slice(tt * 512, (tt + 1) * 512)
                    b = nb()
                    proj_fm(WIN, col, 128, tt, b)
                    ACT(CSQ[:, kc, sl], psb[b][:], AF.Square, ["ps%d" % b], ["CSQ"])
                    gcol = (PV_GQ + kc) if kc < 2 else PV_GKV
                    dst = CQG[:, kc, sl] if kc < 2 else CKG[:, sl]
                    P.op("dve", lambda e, dst=dst, b=b, gcol=gcol: e.tensor_scalar_mul(out=dst, in0=psb[b][:], scalar1=PV[:, l, gcol:gcol + 1]),
                         ["ps%d" % b, "PV"], ["CQG"])
            if CSTOP <= 0.2:
                return
            b = nb()
            for i in range(16):
                for kc in range(2):
                    MM(psb[b][:, i:i + 1], CSQ[:, kc, i * 128:(i + 1) * 128], ONESB[:, 0:1], kc == 0, kc == 1, ["CSQ", "ONESB"], ["ps%d" % b])
                MM(psb[b][:, 16 + i:17 + i], CSQ[:, 2, i * 128:(i + 1) * 128], ONESB[:, 0:1], True, True, ["CSQ", "ONESB"], ["ps%d" % b])
            rsqrt_bc(RS[:, 0:16], psb[b][:, 0:16], 1.0 / 256, ["ps%d" % b, "SM0"], ["RS"])
            rsqrt_bc(RS[:, 16:32], psb[b][:, 16:32], 1.0 / 128, ["ps%d" % b, "SM0"], ["RS"])
            if CSTOP <= 0.3:
                return
            b = nb()
            for i in range(16):
                for k in range(8):
                    MM(psb[b][:, i * 32:(i + 1) * 32], HT[:, k, i * 128:(i + 1) * 128], WIN[:, k, 2176:2208], k == 0, k == 7,
                       ["WIN", "HT%d" % k], ["ps%d" % b])
            CP("act", KR[:].rearrange("p i n -> p (i n)"), psb[b][:], ["ps%d" % b], ["KR"])
            P.fence()
            if CSTOP <= 1:
                return
            gq = PBQ[:, l, 0:96]
            gk = PBQ[:, l, 96:192]

            def rope_apply(dst3, src3, i, nh, rkeys, wkey):
                cos = fview(ROPE[:, 0, i, :], [(0, nh), (1, 16)])
                sin = fview(ROPE[:, 1, i, :], [(0, nh), (1, 16)])
                x1, x2 = src3[:, :, 0:16], src3[:, :, 16:32]
                a = TA[:, 0:nh * 16].rearrange("p (h n) -> p h n", h=nh)
                bq = TB[:, 0:nh * 16].rearrange("p (h n) -> p h n", h=nh)
                TT("dve", a, x1, cos, ALU.mult, rkeys + ["ROPE"], ["rpA"])
                TT("pool", bq, x2, sin, ALU.mult, rkeys + ["ROPE"], ["rpB"])
                TT("dve", dst3[:, :, 0:16], a, bq, ALU.subtract, ["rpA", "rpB"], [wkey])
                TT("dve", a, x2, cos, ALU.mult, rkeys + ["ROPE"], ["rpA"])
                TT("pool", bq, x1, sin, ALU.mult, rkeys + ["ROPE"], ["rpB"])
                TT("dve", dst3[:, :, 16:32], a, bq, ALU.add, ["rpA", "rpB"], [wkey])

            def norm_rope_T(i, src_q3, g_bc, dstT, dcols, dkey):
                SQv = QS[:, 0:768].rearrange("p (h n) -> p h n", h=8)
                TT("dve", SQv, src_q3, src_q3, ALU.mult, ["QQ"], ["QS"])
                P.op("dve", lambda e, SQv=SQv: e.tensor_reduce(out=T8[:, 0:8], in_=SQv, axis=AX.X, op=ALU.add), ["QS"], ["T8"])
                rsqrt_bc(T8[:, 0:8], T8[:, 0:8], 1.0 / 96, ["T8", "SM0"], ["T8"])
                TT("dve", src_q3, src_q3, fview(T8[:, 0:8], [(1, 8), (0, 96)]), ALU.mult, ["QQ", "T8"], ["QQ"])
                TT("pool", src_q3, src_q3, fview(g_bc, [(0, 8), (1, 96)]), ALU.mult, ["QQ", "PBQ"], ["QQ"])
                Q3b = QB[:, 0:768].rearrange("p (h n) -> p h n", h=8)
                CP("dve", Q3b[:, :, 0:64], src_q3[:, :, 0:64], ["QQ"], ["QB"])
                rope_apply(Q3b[:, :, 64:96], src_q3[:, :, 64:96], i, 8, ["QQ"], "QB")
                for h in range(8):
                    bt = nb(0, 4)
                    pst = psb[bt][:].bitcast(BF16)
                    TR(pst[0:96, 0:128], Q3b[:, h, :], IDB[:], ["QB", "IDB"], ["ps%d" % bt])
                    CP("act" if h % 2 else "dve", dstT[0:96, h, dcols], pst[0:96, 0:128], ["ps%d" % bt], [dkey if dkey != "KTh" else "HT%d" % h])

            def prep_k():
                for i in range(16):
                    tsl = slice(i * 128, (i + 1) * 128)
                    b0, b1 = nb(0, 4), nb(0, 4)
                    for half, bnk in ((0, b0), (1, b1)):
                        MM(psb[bnk][:], CKG[:, tsl], WUK[:, half * 512:(half + 1) * 512], True, True, ["CQG", "WUK"], ["ps%d" % bnk])
                    for half, bnk in ((0, b0), (1, b1)):
                        P.op("act", lambda e, half=half, bnk=bnk, i=i: e.activation(out=QS[:, half * 512:(half + 1) * 512], in_=psb[bnk][:],
                                                                                   func=AF.Copy, scale=RS[:, 16 + i:17 + i]),
                             ["ps%d" % bnk, "RS"], ["QS"])
                    KV = QS[:].rearrange("p (h n) -> p h n", h=8)
                    CP("pool", VT[:, i, :, 0:64], fview(QS[:, 64:65], [(256, 4), (1, 64)]), ["QS"], ["VT"])
                    CP("pool", VT[:, i, :, 128:192], fview(QS[:, 192:193], [(256, 4), (1, 64)]), ["QS"], ["VT"])
                    K3 = QQ[:, 0:768].rearrange("p (h n) -> p h n", h=8)
                    CP("dve", K3[:, :, 0:64], KV[:, :, 0:64], ["QS"], ["QQ"])
                    CP("dve", K3[:, :, 64:96], fview(KR[:, i, :], [(0, 8), (1, 32)]), ["KR"], ["QQ"])
                    norm_rope_T(i, K3, gk, KTh, tsl, "KTh")

            def prep_q(qt, QTdst, qkey, only=None):
                for i4 in (range(4) if only is None else [only]):
                    i = qt * 4 + i4
                    tsl = slice(i * 128, (i + 1) * 128)
                    b0, b1 = nb(0, 4), nb(0, 4)
                    for kc in range(2):
                        MM(psb[b0][:], CQG[:, kc, tsl], WUQ[:, kc, 0:512], kc == 0, kc == 1, ["CQG", "WUQ"], ["ps%d" % b0])
                    for kc in range(2):
                        MM(psb[b1][:, 0:256], CQG[:, kc, tsl], WUQ[:, kc, 512:768], kc == 0, kc == 1, ["CQG", "WUQ"], ["ps%d" % b1])
                    P.op("act", lambda e, i=i, b0=b0: e.activation(out=QQ[:, 0:512], in_=psb[b0][:], func=AF.Copy, scale=RS[:, i:i + 1]),
                         ["ps%d" % b0, "RS"], ["QQ"])
                    P.op("act", lambda e, i=i, b1=b1: e.activation(out=QQ[:, 512:768], in_=psb[b1][:, 0:256], func=AF.Copy, scale=RS[:, i:i + 1]),
                         ["ps%d" % b1, "RS"], ["QQ"])
                    Q3 = QQ[:, 0:768].rearrange("p (h n) -> p h n", h=8)
                    norm_rope_T(i, Q3, gq, QTdst, slice(i4 * 128, (i4 + 1) * 128), qkey)

            MEMSET("pool", VT[:, :, :, 64:128], 1.0, ["VT"])
            prep_k()
            P.fence()
            if CSTOP <= 2:
                return
            scale = 96.0 ** -0.5
            NPT = 6
            PTB = arb(base, NPT * 512).rearrange("p (u t) -> p u t", u=NPT)
            QTh2 = [QTh, arb(base + NPT * 256, 8 * 512).rearrange("p (h t) -> p h t", h=8)]
            assert NPT * 256 + 2048 <= 4096
            LOOK = 3
            prep_q(0, QTh2[0], "QTh0")
            for qt in range(NT):
                QTc, qkey = QTh2[qt % 2], "QTh%d" % (qt % 2)
                steps = [(h, kt) for h in range(8) for kt in range(16)]
                sbank = {}

                def score(si):
                    h, kt = steps[si]
                    bs = nb(0, 4)
                    sbank[si] = bs
                    MM(psb[bs][:], KTh[0:96, h, kt * 128:(kt + 1) * 128], QTc[0:96, h, :], True, True, ["HT%d" % h, qkey], ["ps%d" % bs])
                    u = si % NPT
                    ACT(PTB[:, u, :], psb[bs][:], AF.Exp, ["ps%d" % bs], ["PT%d" % u], scale=scale)

                for si in range(min(LOOK, len(steps))):
                    score(si)
                for si, (h, kt) in enumerate(steps):
                    if si + LOOK < len(steps):
                        score(si + LOOK)
                    m, hb = h // 2, h % 2
                    bo = 4 + (h % 4)
                    ko = "ps%d" % bo
                    u = si % NPT
                    lw = VT[:, kt, m, 0:128] if hb == 0 else VT[:, kt, m, 64:192]
                    MM(psb[bo][:, :], lw, PTB[:, u, :], kt == 0, kt == 15, ["VT", "PT%d" % u], [ko])
                    if kt == 15:
                        pn, pd = 64 * hb, 64 * (1 - hb)
                        RECIP(RD[pd:pd + 64, :], psb[bo][pd:pd + 64, :], [ko], ["RD%d" % hb])
                        TT("dve", YC[pn:pn + 64, m, :], psb[bo][pn:pn + 64, :], RD[pd:pd + 64, :], ALU.mult, [ko, "RD%d" % hb], ["YC%d" % m])
                        if h % 2 == 1:
                            ACT(YSQ[:, m, :], YC[:, m, :], AF.Square, ["YC%d" % m], ["YSQ%d" % m])
                            if qt + 1 < NT:
                                prep_q(qt + 1, QTh2[(qt + 1) % 2], "QTh%d" % ((qt + 1) % 2), only=h // 2)
                b = nb(0, 4)
                for m in range(4):
                    MM(psb[b][:], ONESB[:], YSQ[:, m, :], m == 0, m == 3, ["ONESB", "YSQ%d" % m], ["ps%d" % b])
                rsqrt_bc(RBC[:], psb[b][:], 1.0 / 512, ["ps%d" % b, "SM0"], ["RBC"])
                for m in range(4):
                    STT("dve", YN[:, m, :], YC[:, m, :], PV[:, l, PV_GC + m:PV_GC + m + 1], RBC[:], ALU.mult, ALU.mult,
                        ["YC%d" % m, "PV", "RBC"], ["YSQ%d" % m])
                for dk in range(8):
                    b = nb(0, 4)
                    for m in range(4):
                        MM(psb[b][:], WOC[:, m, dk * 128:(dk + 1) * 128], YN[:, m, :], m == 0, m == 3, ["WOC", "YSQ%d" % m], ["ps%d" % b])
                    add_to_x(psb[b][:], dk, qt, ["ps%d" % b])

        def ffn(l):
            rmsnorm_to_HT(l, PV_GFFN, 16384)
            P.fence()
            moe = (l % 2 == 1)
            WS = [(arb(sl_ * 6144, 8 * 512).rearrange("p (k n) -> p k n", k=8),
                   arb(sl_ * 6144 + 2048, 8 * 512).rearrange("p (k n) -> p k n", k=8),
                   arb(sl_ * 6144 + 4096, 4 * D).rearrange("p (c n) -> p c n", c=4)) for sl_ in range(2)]
            o1 = 12288
            ACTB = [arb(o1 + a_ * 4096, 4 * T).rearrange("p (c t) -> p c t", c=4) for a_ in range(2)]
            o2 = o1 + 8192
            SG = arb(o2, 2 * 512).rearrange("p (u t) -> p u t", u=2)
            TU = arf(o2 + 512, 2 * 512).rearrange("p (u t) -> p u t", u=2)
            CBC = arb(o2 + 1536, 2 * T).rearrange("p (u t) -> p u t", u=2)
            o3 = o2 + 3584
            RB = arf(0, T)
            groups = []
            if not moe:
                nfull = D_FF // 512
                for g in range(nfull):
                    groups.append((None, g * 512, [128] * 4))
                groups.append((None, nfull * 512, [128, 64]))
            else:
                for e_ in range(NEXP):
                    for g in range(D_EXP // 512):
                        groups.append((e_, g * 512, [128] * 4))
            if moe:
                ot = 12288
                RG = arf(ot, 64).rearrange("p (k e) -> p k e", k=8)
                LG = arf(ot + 64, 128).rearrange("p (i e) -> p i e", i=16)
                MX = arf(ot + 192, 128).rearrange("p (i e) -> p i e", i=16)
                W12 = arf(ot + 320, 32).rearrange("p (a i) -> p a i", a=2)
                EQ = arf(ot + 352, 128).rearrange("p (i e) -> p i e", i=16)
                CMB = arf(ot + 480, 128).rearrange("p (i e) -> p i e", i=16)
                RSQ = arf(ot + 608, 16)
                SQB = arb(ot + 624, 128)
                CT = arf(o3, T)
                assert o3 + T <= ARW
                P.dma("sp", RG, rt_d[0].rearrange("(k p) e -> p k e", p=128), writes=["RG"])
                TT("dve", RG, RG, fview(PV[:, l, PV_GFFN:PV_GFFN + 8], [(1, 8), (0, 8)]), ALU.mult, ["RG", "PV"], ["RG"])
                bl = nb()
                for i in range(16):
                    for k in range(8):
                        MM(psb[bl][:, i * 8:(i + 1) * 8], X[:, k, i * 128:(i + 1) * 128], RG[:, k, :], k == 0, k == 7, ["X", "RG"], ["ps%d" % bl])
                br = nb()
                for i in range(16):
                    for k in range(8):
                        ACT(SQB[:], X[:, k, i * 128:(i + 1) * 128], AF.Square, ["X"], ["SQB"])
                        MM(psb[br][:, i:i + 1], SQB[:], ONESB[:, 0:1], k == 0, k == 7, ["SQB", "ONESB"], ["ps%d" % br])
                rsqrt_bc(RSQ[:], psb[br][:, 0:16], 1.0 / D, ["ps%d" % br, "SM0"], ["RSQ"])
                TT("dve", LG, psb[bl][:, 0:128].rearrange("p (i e) -> p i e", i=16), fview(RSQ, [(1, 16), (0, 8)]), ALU.mult,
                   ["ps%d" % bl, "RSQ"], ["LG"])
                for i in range(16):
                    P.op("dve", lambda e, i=i: e.max(out=MX[:, i, :], in_=LG[:, i, :]), ["LG"], ["MX"])
                TT("dve", W12[:, 0, :], MX[:, :, 0], MX[:, :, 1], ALU.subtract, ["MX"], ["W12"])
                TT("dve", W12[:, 1, :], MX[:, :, 1], MX[:, :, 0], ALU.subtract, ["MX"], ["W12"])
                ACT(W12[:].rearrange("p a i -> p (a i)"), W12[:].rearrange("p a i -> p (a i)"), AF.Sigmoid, ["W12"], ["W12"])
                TT("dve", EQ, LG, fview(MX[:, :, 0], [(8, 16), (0, 8)]), ALU.is_equal, ["LG", "MX"], ["EQ"])
                TT("dve", CMB, EQ, fview(W12[:, 0, :], [(1, 16), (0, 8)]), ALU.mult, ["EQ", "W12"], ["CMB"])
                TT("dve", EQ, LG, fview(MX[:, :, 1], [(8, 16), (0, 8)]), ALU.is_equal, ["LG", "MX", "CMB"], ["EQ"])
                TT("dve", EQ, EQ, fview(W12[:, 1, :], [(1, 16), (0, 8)]), ALU.mult, ["EQ", "W12"], ["EQ"])
                TT("dve", CMB, CMB, EQ, ALU.add, ["CMB", "EQ"], ["CMB"])
                for i in range(16):
                    bt = nb()
                    TR(psb[bt][0:8, 0:128], CMB[:, i, :], ident, ["CMB", "CONST"], ["ps%d" % bt])
                    CP("act", CT[0:8, i * 128:(i + 1) * 128], psb[bt][0:8, 0:128], ["ps%d" % bt], ["CT"])
            if moe:
                P.fence()
            rr = [0]

            def load13(gi):
                e_, c0, chunks = groups[gi]
                w1b, w3b, w2b = WS[gi % 2]
                n = sum(chunks)
                key = "WSA%d" % (gi % 2)
                if e_ is None:
                    P.dma("pool", w1b[:, :, 0:n], wgu_d[0, :, c0:c0 + n].rearrange("(k p) n -> p k n", p=128), writes=[key])
                    P.dma("pool", w3b[:, :, 0:n], wgu_d[0, :, D_FF + c0:D_FF + c0 + n].rearrange("(k p) n -> p k n", p=128), writes=[key])
                else:
                    P.dma("pool", w1b[:], w1_d[0, e_, :, c0:c0 + 512].rearrange("(k p) n -> p k n", p=128), writes=[key])
                    P.dma("pool", w3b[:], w3_d[0, e_, :, c0:c0 + 512].rearrange("(k p) n -> p k n", p=128), writes=[key])

            def load2(gi):
                e_, c0, chunks = groups[gi]
                w1b, w3b, w2b = WS[gi % 2]
                key = "WSB%d" % (gi % 2)
                if e_ is None:
                    r0 = c0
                    for ci, cs_ in enumerate(chunks):
                        if cs_ < 128:
                            MEMSET("pool", w2b[cs_:128, ci, :], 0.0, [key])
                            MEMSET("pool", ACTB[gi % 2][cs_:128, ci, :], 0.0, ["ACT%d" % (gi % 2)])
                        P.dma("pool", w2b[0:cs_, ci, :], wdn_d[0, r0:r0 + cs_, :], writes=[key])
                        r0 += cs_
                else:
                    P.dma("pool", w2b[:], w2_d[0, e_, c0:c0 + 512, :].rearrange("(c p) n -> p c n", p=128), writes=[key])

            def gu(gi):
                e_, c0, chunks = groups[gi]
                w1b, w3b, w2b = WS[gi % 2]
                key = "WSA%d" % (gi % 2)
                AB = ACTB[gi % 2]
                akey = "ACT%d" % (gi % 2)
                if e_ is not None and c0 == 0:
                    for tt in range(NT):
                        bt = nb(0, 4)
                        MM(psb[bt][:], fview(CONST[0:8, C_ID + e_:C_ID + e_ + 1], [(0, 128)]), CT[0:8, tt * 512:(tt + 1) * 512], True, True,
                           ["CONST", "CT"], ["ps%d" % bt])
                        CP("act", CBC[:, e_ % 2, tt * 512:(tt + 1) * 512], psb[bt][:], ["ps%d" % bt], ["CBC%d" % (e_ % 2)])
                off = 0
                for ci, cs_ in enumerate(chunks):
                    for tt in range(NT):
                        sl = slice(tt * 512, (tt + 1) * 512)
                        bg, bu = nb(0, 4), nb(0, 4)
                        for k in range(8):
                            MM(psb[bg][0:cs_, :], w1b[:, k, off:off + cs_], HT[:, k, sl], k == 0, k == 7, [key, "HT%d" % k], ["ps%d" % bg])
                        for k in range(8):
                            MM(psb[bu][0:cs_, :], w3b[:, k, off:off + cs_], HT[:, k, sl], k == 0, k == 7, [key, "HT%d" % k], ["ps%d" % bu])
                        u = rr[0] % 2
                        rr[0] += 1
                        ACT(SG[0:cs_, u, :], psb[bg][0:cs_, :], AF.Silu, ["ps%d" % bg], ["SG%d" % u])
                        if e_ is None:
                            TT("dve", AB[0:cs_, ci, sl], psb[bu][0:cs_, :], SG[0:cs_, u, :], ALU.mult, ["ps%d" % bu, "SG%d" % u], [akey])
                        else:
                            TT("dve", TU[:, u, :], psb[bu][:], CBC[:, e_ % 2, sl], ALU.mult, ["ps%d" % bu, "CBC%d" % (e_ % 2)], ["TU%d" % u])
                            TT("pool", AB[:, ci, sl], TU[:, u, :], SG[:, u, :], ALU.mult, ["TU%d" % u, "SG%d" % u], [akey])
                    off += cs_

            def down(gi):
                e_, c0, chunks = groups[gi]
                w1b, w3b, w2b = WS[gi % 2]
                key = "WSB%d" % (gi % 2)
                AB = ACTB[gi % 2]
                akey = "ACT%d" % (gi % 2)
                for dk in range(8):
                    for tt in range(NT):
                        b = nb(4, 8)
                        for ci, cs_ in enumerate(chunks):
                            MM(psb[b][:], w2b[:, ci, dk * 128:(dk + 1) * 128], AB[:, ci, tt * 512:(tt + 1) * 512],
                               ci == 0, ci == len(chunks) - 1, [key, akey], ["ps%d" % b])
                        add_to_x(psb[b][:], dk, tt, ["ps%d" % b])

            ng = len(groups)
            load13(0)
            load2(0)
            for gi in range(ng):
                if gi + 1 < ng:
                    load13(gi + 1)
                gu(gi)
                if gi >= 1:
                    down(gi - 1)
                if gi + 1 < ng:
                    load2(gi + 1)
            down(ng - 1)

        for s in range(nseq):
            load_x(s)
            P.fence()
            for l in range(2):
                if stop is not None and stop[1] == "norm":
                    rmsnorm_to_HT(l, PV_GMIX, 8832)
                    P.fence()
                    break
                mixer(l, s)
                if stop == (l, "mix"):
                    break
                ffn(l)
                P.fence()
                if stop == (l, "ffn"):
                    break
            store_x(s)
            P.fence()
        P.emit()
    return nc


def _consts():
    c = np.zeros((128, NCONST), np.float32)
    c[:, C_ID:C_ID + 128] = np.eye(128, dtype=np.float32)
    s = (np.arange(128) % 64)[:, None]
    t = np.arange(64)[None, :]
    c[:, C_MLOW:C_MLOW + 64] = (t >= s)
    c[:, C_MUP:C_MUP + 64] = (t <= s)
    bo = np.zeros((128, 128), np.float32)
    bo[:64, :64] = 1
    bo[64:, 64:] = 1
    c[:, C_BONES:C_BONES + 128] = bo
    inv = (10000.0 ** (-np.arange(0, 16, dtype=np.float32) * np.float32(2.0 / 32))).astype(np.float32)
    c[:, C_IFQ:C_IFQ + 16] = inv[None, :]
    return c


def _pack(inp):
    pv = np.zeros((2, 128, NPV), np.float32)
    pbq = np.zeros((2, 128, 192), np.float32)
    f = lambda a: np.asarray(a, np.float32)
    for l in range(2):
        pv[l, :, PV_GMIX:PV_GMIX + 8] = f(inp["norm_mix"][l]).reshape(8, 128).T
        pv[l, :, PV_GFFN:PV_GFFN + 8] = f(inp["norm_ffn"][l]).reshape(8, 128).T
        cw = f(inp["conv_w"][l])
        for j in range(2):
            pv[l, :, PV_CW + j * 4:PV_CW + j * 4 + 4] = cw[:, j * 128:(j + 1) * 128].T
            pv[l, :, PV_CB + j] = f(inp["conv_b"][l])[j * 128:(j + 1) * 128]
            pv[l, :, PV_GA + j] = f(inp["out_g_a"][l])[j * 128:(j + 1) * 128]
            pv[l, :, PV_GQ + j] = f(inp["mla_q_norm"][l])[j * 128:(j + 1) * 128]
            for d in range(2):
                pv[l, :, PV_BA + d * 2 + j] = f(inp["lru_ba"][l, d])[j * 128:(j + 1) * 128]
                pv[l, :, PV_BX + d * 2 + j] = f(inp["lru_bx"][l, d])[j * 128:(j + 1) * 128]
                pv[l, :, PV_LAM + d * 2 + j] = f(inp["lru_lam"][l, d])[j * 128:(j + 1) * 128]
                for ll in range(2):
                    pv[l, :, PV_LB + ll * 4 + d * 2 + j] = f(inp["hg_lb_logits"][ll, d])[j * 128:(j + 1) * 128]
        pv[l, :, PV_HGN] = np.tile(f(inp["hg_norm_g"][l]), 2)
        pv[l, :, PV_GKV] = f(inp["mla_kv_norm"][l])
        pv[l, :, PV_GC:PV_GC + 4] = f(inp["out_g_c"][l]).reshape(4, 128).T
        pbq[l, :, 0:96] = f(inp["qk_norm_q"][l])[None, :]
        pbq[l, :, 96:192] = f(inp["qk_norm_k"][l])[None, :]
    return pv, pbq


_NC_CACHE = {}


def kernel(**inp):
    ncores = 8
    nseq = 2
    key = (nseq, None)
    if key not in _NC_CACHE:
        _NC_CACHE[key] = build_nc(nseq, None)
    nc = _NC_CACHE[key]
    x = np.ascontiguousarray(np.asarray(inp["x"], np.float32))
    pos = np.asarray(inp["positions"], np.int32)
    pv, pbq = _pack(inp)
    consts = _consts()
    shared = {
        "consts": consts, "pv": pv, "pbq": pbq,
    }
    for nme in ("w_in", "lru_wa", "lru_wx", "mla_w_uq", "mla_w_ukv", "w_out", "ffn_w_gate_up", "ffn_w_down",
                "moe_router", "moe_w1", "moe_w3", "moe_w2"):
        shared[nme] = np.ascontiguousarray(np.asarray(inp[nme], np.float32))
    in_maps = []
    for c in range(ncores):
        m = dict(shared)
        m["x"] = x[c * nseq:(c + 1) * nseq]
        m["pos"] = np.ascontiguousarray(pos[c * nseq:(c + 1) * nseq].reshape(nseq, 16, 128).transpose(0, 2, 1))
        in_maps.append(m)
    res = run_bass_kernel_spmd(nc, in_maps, core_ids=list(range(ncores)))
    return np.concatenate([np.asarray(r["y"], np.float32) for r in res.results], axis=0)
```

```python
import contextlib
import numpy as np
import concourse.bass as bass
import concourse.mybir as mybir
from concourse.bass_utils import run_bass_kernel_spmd

F32 = mybir.dt.float32
BF16 = mybir.dt.bfloat16
I32 = mybir.dt.int32
ALU = mybir.AluOpType
AF = mybir.ActivationFunctionType
AX = mybir.AxisListType

T = 2048
D = 1024
NT = 4
EPS = 1e-6
D_IN = 2208
D_FF = 2752
D_EXP = 3584
NEXP = 8
ARW = 26112
CSTOP = 99


class _Op:
    __slots__ = ("eng", "fn", "deps", "signals", "ticket", "kind", "dsem", "dtarget", "prev_same_sem")

    def __init__(self, eng, fn, kind):
        self.eng = eng
        self.fn = fn
        self.deps = []
        self.signals = False
        self.ticket = None
        self.kind = kind
        self.dsem = None
        self.dtarget = None
        self.prev_same_sem = None


class Prog:
    ENGS = ("pe", "act", "dve", "pool", "sp")

    def __init__(self, nc, n_dma_sems=8):
        self.nc = nc
        self.ops = {e: [] for e in self.ENGS}
        self.last_w = {}
        self.readers = {}
        self.n_dma_sems = n_dma_sems
        self.dma_count = {e: 0 for e in self.ENGS}
        self.dma_last_on_sem = {}
        self.fence_deps = None
        self.fenced = set()

    def _add_dep(self, op, p):
        if p is None or p is op:
            return
        if p.eng == op.eng and p.kind == "c" and op.kind == "c" and p.eng == "pe":
            return
        op.deps.append(p)
        p.signals = True

    def fence(self):
        deps = []
        for e in self.ENGS:
            for o in reversed(self.ops[e]):
                if o.kind == "c":
                    deps.append(o)
                    break
        deps.extend(self.dma_last_on_sem.values())
        self.fence_deps = deps
        self.fenced = set()

    def op(self, eng, fn, reads=(), writes=(), kind="c"):
        o = _Op(eng, fn, kind)
        if eng != "pe":
            extra = [r for r in reads if isinstance(r, str) and r.startswith("ps") and r[2:].isdigit() and r not in writes]
            if extra:
                writes = list(writes) + extra
        if self.fence_deps is not None and eng not in self.fenced:
            self.fenced.add(eng)
            for p in self.fence_deps:
                if p.eng == eng and p.kind == "c":
                    continue
                o.deps.append(p)
                p.signals = True
        for r in reads:
            self._add_dep(o, self.last_w.get(r))
        for w in writes:
            self._add_dep(o, self.last_w.get(w))
            rd = self.readers.get(w)
            if rd:
                for p in rd.values():
                    self._add_dep(o, p)
        for r in reads:
            d = self.readers.setdefault(r, {})
            if kind == "c":
                d[eng] = o
            else:
                d[("dma", id(o))] = o
        for w in writes:
            self.last_w[w] = o
            self.readers[w] = {}
        if kind == "d":
            i = self.dma_count[eng]
            self.dma_count[eng] = i + 1
            slot = (eng, i % self.n_dma_sems)
            o.dsem = slot
            o.dtarget = 16 * (i // self.n_dma_sems + 1)
            o.prev_same_sem = self.dma_last_on_sem.get(slot)
            self.dma_last_on_sem[slot] = o
        self.ops[eng].append(o)
        return o

    def dma(self, eng, out, in_, reads=(), writes=(), **kw):
        return self.op(eng, lambda e: e.dma_start(out=out, in_=in_, **kw), reads, writes, kind="d")

    def emit(self, final_wait_eng="sp"):
        nc = self.nc
        with contextlib.ExitStack() as st:
            esem = {}
            for e in self.ENGS:
                if any(o.kind == "c" for o in self.ops[e]):
                    esem[e] = st.enter_context(nc.semaphore("s_" + e))
            dsem = {}
            for e in self.ENGS:
                for j in range(min(self.dma_count[e], self.n_dma_sems)):
                    dsem[(e, j)] = st.enter_context(nc.semaphore("d_%s%d" % (e, j)))
            for e in self.ENGS:
                t = 0
                for o in self.ops[e]:
                    if o.kind == "c" and o.signals:
                        t += 1
                        o.ticket = t
            final_dmas = list(self.dma_last_on_sem.values())
            block = st.enter_context(nc.Block())

            def make(e):
                def body(eng):
                    waited = {}

                    def wait(key, sem, val):
                        if waited.get(key, 0) >= val:
                            return
                        waited[key] = val
                        eng.wait_ge(sem, val)

                    for o in self.ops[e]:
                        for p in o.deps:
                            if p.kind == "c":
                                wait(("c", p.eng), esem[p.eng], p.ticket)
                            else:
                                wait(("d", p.dsem), dsem[p.dsem], p.dtarget)
                        if o.kind == "d" and o.prev_same_sem is not None:
                            p = o.prev_same_sem
                            wait(("d", p.dsem), dsem[p.dsem], p.dtarget)
                        ins = o.fn(eng)
                        if o.kind == "d":
                            ins.then_inc(dsem[o.dsem], 16)
                        elif o.signals:
                            ins.then_inc(esem[e], 1)
                    if e == final_wait_eng:
                        for p in final_dmas:
                            wait(("d", p.dsem), dsem[p.dsem], p.dtarget)
                return body

            names = {"pe": "tensor", "act": "scalar", "dve": "vector", "pool": "gpsimd", "sp": "sync"}
            for e in self.ENGS:
                if not self.ops[e] and e != final_wait_eng:
                    continue
                getattr(block, names[e])(make(e))


def rev_ap(a):
    (ps, pn), (s, n) = a.ap
    return bass.AP(a.tensor, a.offset + (n - 1) * s, [[ps, pn], [-s, n]])


def fview(a, dims):
    return bass.AP(a.tensor, a.offset, [list(a.ap[0])] + [list(d) for d in dims])


PV_GMIX, PV_GFFN, PV_CW, PV_CB, PV_BA, PV_BX, PV_LAM, PV_GA, PV_LB, PV_HGN, PV_GQ, PV_GKV, PV_GC = \
    0, 8, 16, 24, 26, 30, 34, 38, 40, 48, 49, 51, 52
NPV = 60
C_ID, C_MLOW, C_MUP, C_BONES, C_IFQ = 0, 128, 192, 256, 384
NCONST = C_IFQ + 16


def build_nc(nseq=2, stop=None, phases="abc"):
    nc = bass.Bass("TRN2", target_bir_lowering=False)
    dr = lambda n, s, dt=F32, kind="ExternalInput": nc.dram_tensor(n, list(s), dt, kind=kind).ap()
    x_d = dr("x", [nseq, T, D])
    pos_d = dr("pos", [nseq, 128, 16], I32)
    consts_d = dr("consts", [128, NCONST])
    pv_d = dr("pv", [2, 128, NPV])
    pbq_d = dr("pbq", [2, 128, 192])
    w_in_d = dr("w_in", [2, D, D_IN])
    wa_d = dr("lru_wa", [2, 2, 4, 64, 64])
    wx_d = dr("lru_wx", [2, 2, 4, 64, 64])
    wuq_d = dr("mla_w_uq", [2, 256, 768])
    wukv_d = dr("mla_w_ukv", [2, 128, 1024])
    wout_d = dr("w_out", [2, D, D])
    wgu_d = dr("ffn_w_gate_up", [1, D, 2 * D_FF])
    wdn_d = dr("ffn_w_down", [1, D_FF, D])
    rt_d = dr("moe_router", [1, D, NEXP])
    w1_d = dr("moe_w1", [1, NEXP, D, D_EXP])
    w3_d = dr("moe_w3", [1, NEXP, D, D_EXP])
    w2_d = dr("moe_w2", [1, NEXP, D_EXP, D])
    y_d = dr("y", [nseq, T, D], kind="ExternalOutput")

    with contextlib.ExitStack() as st:
        def sb(name, shape, dt=F32):
            return st.enter_context(nc.sbuf_tensor(name, list(shape), dt))

        X = sb("X", [128, 8, T])
        HT = sb("HT", [128, 8, T], BF16)
        AR = sb("AR", [128, ARW])
        CONST = sb("CONST", [128, NCONST])
        PV = sb("PV", [128, 2, NPV])
        PBQ = sb("PBQ", [128, 2, 192])
        IDB = sb("IDB", [128, 128], BF16)
        ONESB = sb("ONESB", [128, 128], BF16)
        BONESB = sb("BONESB", [128, 128], BF16)
        MASKS = sb("MASKS", [128, 2, 64])
        SM = sb("SM", [128, 512])
        ROPE = sb("ROPE", [128, 2, 16, 16])
        psb = [st.enter_context(nc.psum_tensor("ps%d" % i, [128, 512], F32)) for i in range(8)]

        P = Prog(nc)
        ident = CONST[:, C_ID:C_ID + 128]

        def MM(out, lhsT, rhs, start, stop_, r, w):
            P.op("pe", lambda e: e.matmul(out, lhsT=lhsT, rhs=rhs, start=start, stop=stop_), r, w)

        def TR(out, in_, idn, r, w):
            P.op("pe", lambda e: e.transpose(out, in_, idn), r, w)

        def ACT(out, in_, func, r, w, bias=0.0, scale=1.0):
            P.op("act", lambda e: e.activation(out=out, in_=in_, func=func, bias=bias, scale=scale), r, w)

        def TT(eng, out, in0, in1, op, r, w):
            P.op(eng, lambda e: e.tensor_tensor(out=out, in0=in0, in1=in1, op=op), r, w)

        def TS(eng, out, in0, s1, s2, op0, op1, r, w):
            P.op(eng, lambda e: e.tensor_scalar(out=out, in0=in0, scalar1=s1, scalar2=s2, op0=op0, op1=op1), r, w)

        def STT(eng, out, in0, scalar, in1, op0, op1, r, w):
            eng = "dve"
            P.op(eng, lambda e: e.scalar_tensor_tensor(out=out, in0=in0, scalar=scalar, in1=in1, op0=op0, op1=op1), r, w)

        def CP(eng, out, in_, r, w):
            if eng == "act":
                P.op("act", lambda e: e.copy(out=out, in_=in_), r, w)
            else:
                P.op(eng, lambda e: e.tensor_copy(out=out, in_=in_), r, w)

        def RECIP(out, in_, r, w):
            P.op("dve", lambda e: e.reciprocal(out=out, in_=in_), r, w)

        def MEMSET(eng, ap, val, w):
            P.op(eng, lambda e: e.memset(ap, val), (), w)

        bank_rr = [0]

        def nb(lo=0, hi=8):
            i = bank_rr[0]
            i = lo + (i - lo + 1) % (hi - lo) if lo <= i < hi else lo
            bank_rr[0] = i
            return i

        def arf(off, n):
            return AR[:, off:off + n]

        def arb(off, n):
            return AR[:, off:off + n // 2].bitcast(BF16)

        P.dma("sp", CONST[:], consts_d, writes=["CONST"])
        P.dma("sp", PV[:], pv_d.rearrange("l p n -> p l n"), writes=["PV"])
        P.dma("sp", PBQ[:], pbq_d.rearrange("l p n -> p l n"), writes=["PBQ"])
        CP("dve", IDB[:], ident, ["CONST"], ["IDB"])
        MEMSET("dve", ONESB[:], 1.0, ["ONESB"])
        CP("dve", BONESB[:], CONST[:, C_BONES:C_BONES + 128], ["CONST"], ["BONESB"])
        CP("dve", MASKS[:, 0, :], CONST[:, C_MLOW:C_MLOW + 64], ["CONST"], ["MASKS"])
        CP("dve", MASKS[:, 1, :], CONST[:, C_MUP:C_MUP + 64], ["CONST"], ["MASKS"])

        def rsqrt_bc(out, ps, scale, r, w):
            ACT(out, ps, AF.Sqrt, r, w, bias=SM[:, 0:1], scale=scale)
            RECIP(out, out, w, w)

        MEMSET("dve", SM[:, 0:1], EPS, ["SM0"])
        MEMSET("dve", SM[:, 1:2], 1.0, ["SM1"])

        def load_x(s):
            stg = [arf(0, 1024), arf(1024, 1024)]
            for i in range(16):
                sg = stg[i % 2]
                P.dma("sp", sg, x_d[s, i * 128:(i + 1) * 128, :], writes=["stg%d" % (i % 2)])
                for h in range(2):
                    b = nb()
                    for q in range(4):
                        k = h * 4 + q
                        TR(psb[b][:, q * 128:(q + 1) * 128], sg[:, k * 128:(k + 1) * 128], ident,
                           ["stg%d" % (i % 2), "CONST"], ["ps%d" % b])
                    eng = "act" if h == 0 else "dve"
                    CP(eng, X[:, h * 4:(h + 1) * 4, i * 128:(i + 1) * 128],
                       psb[b][:].rearrange("p (q t) -> p q t", q=4), ["ps%d" % b], ["X"])

        def store_x(s):
            stg = [arf(0, 1024), arf(1024, 1024)]
            for i in range(16):
                sg = stg[i % 2]
                for h in range(2):
                    b = nb()
                    for q in range(4):
                        k = h * 4 + q
                        TR(psb[b][:, q * 128:(q + 1) * 128], X[:, k, i * 128:(i + 1) * 128], ident,
                           ["X", "CONST"], ["ps%d" % b])
                    eng = "act" if h == 0 else "dve"
                    CP(eng, sg[:, h * 512:(h + 1) * 512], psb[b][:], ["ps%d" % b], ["stg%d" % (i % 2)])
                P.dma("sp", y_d[s, i * 128:(i + 1) * 128, :], sg, reads=["stg%d" % (i % 2)])

        def rmsnorm_to_HT(l, gcol, scr_off):
            RB = arf(scr_off, T)
            for k in range(8):
                ACT(HT[:, k, :], X[:, k, :], AF.Square, ["X"], ["HT%d" % k])
            for tt in range(NT):
                b = nb()
                for k in range(8):
                    MM(psb[b][:], ONESB[:], HT[:, k, tt * 512:(tt + 1) * 512], k == 0, k == 7,
                       ["ONESB", "HT%d" % k], ["ps%d" % b])
                rsqrt_bc(RB[:, tt * 512:(tt + 1) * 512], psb[b][:], 1.0 / D, ["ps%d" % b, "SM0"], ["RB%d" % tt])
            for k in range(8):
                eng = "dve" if k % 2 == 0 else "pool"
                STT(eng, HT[:, k, :], X[:, k, :], PV[:, l, gcol + k:gcol + k + 1], RB[:], ALU.mult, ALU.mult,
                    ["X", "PV"] + ["RB%d" % t_ for t_ in range(NT)], ["HT%d" % k])

        HTk = ["HT%d" % k for k in range(8)]

        def proj_fm(WIN, col0, m, tt, b, pbase=0):
            for k in range(8):
                MM(psb[b][pbase:pbase + m, :], WIN[:, k, col0:col0 + m], HT[:, k, tt * 512:(tt + 1) * 512],
                   k == 0, k == 7, ["WIN", "HT%d" % k], ["ps%d" % b])

        def add_to_x(ps, dk, tt, r):
            TT("dve", X[:, dk, tt * 512:(tt + 1) * 512], ps, X[:, dk, tt * 512:(tt + 1) * 512], ALU.add,
               r + ["X"], ["X"])

        def mixer(l, s):
            WIN = arb(0, 8 * D_IN).rearrange("p (k n) -> p k n", k=8)
            P.dma("pool", WIN[:, 0:4, :], w_in_d[l, 0:512, :].rearrange("(k p) n -> p k n", p=128), writes=["WIN"])
            P.dma("pool", WIN[:, 4:8, :], w_in_d[l, 512:1024, :].rearrange("(k p) n -> p k n", p=128), writes=["WIN"])
            SC0 = 8832
            rmsnorm_to_HT(l, PV_GMIX, SC0)
            if "a" in phases:
                phase_a(l, WIN, SC0)
                P.fence()
            if "b" in phases:
                phase_b(l, WIN, SC0)
                P.fence()
            if "c" in phases:
                phase_c(l, s, WIN, SC0)
                P.fence()

        def phase_a(l, WIN, o0):
            S = [arf(o0 + i * T, T) for i in range(6)]
            XCB = arb(o0 + 6 * T, T)
            YAB = [arb(o0 + 6 * T + 1024 + j * 1024, T) for j in range(2)]
            WOA = arb(o0 + 6 * T + 3072, 2 * D).rearrange("p (k n) -> p k n", k=2)
            BD = arb(o0 + 6 * T + 3072 + 1024, 8 * 128).rearrange("p (g n) -> p g n", g=8)
            P.dma("pool", WOA, wout_d[l, 0:256, :].rearrange("(k p) n -> p k n", p=128), writes=["WOA"])
            MEMSET("pool", BD, 0.0, ["BD"])
            for g, wd in enumerate((wa_d, wx_d)):
                for d in range(2):
                    for j in range(2):
                        gi = g * 4 + d * 2 + j
                        for hb in range(2):
                            P.dma("pool", BD[hb * 64:(hb + 1) * 64, gi, hb * 64:(hb + 1) * 64],
                                  wd[l, d, 2 * j + hb], writes=["BD"])
            ACT(SM[:, 8:12], PV[:, l, PV_LAM:PV_LAM + 4], AF.Exp, ["PV"], ["SMA"], scale=-1.0)
            ACT(SM[:, 8:12], SM[:, 8:12], AF.Ln, ["SMA"], ["SMA"], bias=SM[:, 1:2], scale=1.0)
            TS("dve", SM[:, 12:16], SM[:, 8:12], -16.0, None, ALU.mult, ALU.mult, ["SMA"], ["SMB"]) if False else None
            P.op("dve", lambda e: e.tensor_scalar_mul(out=SM[:, 12:16], in0=SM[:, 8:12], scalar1=-16.0), ["SMA"], ["SMB"])
            P.op("dve", lambda e: e.tensor_scalar_mul(out=SM[:, 8:12], in0=SM[:, 8:12], scalar1=-8.0), ["SMA", "SMB"], ["SMA"])
            for j in range(2):
                XA, XC, R, I, A2, HF = S
                cw = lambda kk: PV[:, l, PV_CW + j * 4 + kk:PV_CW + j * 4 + kk + 1]
                for tt in range(NT):
                    b = nb()
                    proj_fm(WIN, j * 128, 128, tt, b)
                    CP("act", XA[:, tt * 512:(tt + 1) * 512], psb[b][:], ["ps%d" % b], ["XA"])
                TS("dve", XC[:], XA[:], cw(2), PV[:, l, PV_CB + j:PV_CB + j + 1], ALU.mult, ALU.add, ["XA", "PV"], ["XC"])
                STT("dve", XC[:, 2:T], XA[:, 0:T - 2], cw(0), XC[:, 2:T], ALU.mult, ALU.add, ["XA", "PV", "XC"], ["XC"])
                STT("dve", XC[:, 1:T], XA[:, 0:T - 1], cw(1), XC[:, 1:T], ALU.mult, ALU.add, ["XA", "PV", "XC"], ["XC"])
                STT("dve", XC[:, 0:T - 1], XA[:, 1:T], cw(3), XC[:, 0:T - 1], ALU.mult, ALU.add, ["XA", "PV", "XC"], ["XC"])
                CP("pool", XCB[:], XC[:], ["XC"], ["XCB"])
                HB = XA
                for d in range(2):
                    for tt in range(NT):
                        sl = slice(tt * 512, (tt + 1) * 512)
                        b = nb()
                        MM(psb[b][:], BD[:, 0 * 4 + d * 2 + j, :], XCB[:, sl], True, True, ["BD", "XCB"], ["ps%d" % b])
                        ACT(R[:, sl], psb[b][:], AF.Sigmoid, ["ps%d" % b, "PV"], ["R"],
                            bias=PV[:, l, PV_BA + d * 2 + j:PV_BA + d * 2 + j + 1])
                        b = nb()
                        MM(psb[b][:], BD[:, 1 * 4 + d * 2 + j, :], XCB[:, sl], True, True, ["BD", "XCB"], ["ps%d" % b])
                        ACT(I[:, sl], psb[b][:], AF.Sigmoid, ["ps%d" % b, "PV"], ["I"],
                            bias=PV[:, l, PV_BX + d * 2 + j:PV_BX + d * 2 + j + 1])
                    ci = d * 2 + j
                    ACT(A2[:], R[:], AF.Exp, ["R", "SMB"], ["A2"], scale=SM[:, 12 + ci:13 + ci])
                    ACT(R[:], R[:], AF.Exp, ["R", "SMA"], ["R"], scale=SM[:, 8 + ci:9 + ci])
                    TS("dve", A2[:], A2[:], -1.0, 1.0, ALU.mult, ALU.add, ["A2"], ["A2"])
                    P.op("dve", lambda e: e.tensor_scalar_max(out=A2[:], in0=A2[:], scalar1=0.0), ["A2"], ["A2"])
                    ACT(A2[:], A2[:], AF.Sqrt, ["A2"], ["A2"])
                    TT("pool", I[:], I[:], XC[:], ALU.mult, ["I", "XC"], ["I"])
                    TT("dve", I[:], I[:], A2[:], ALU.mult, ["I", "A2"], ["I"])
                    if d == 0:
                        P.op("dve", lambda e: e.tensor_tensor_scan(out=HF[:], data0=R[:], data1=I[:], initial=0.0,
                                                                  op0=ALU.mult, op1=ALU.add), ["R", "I"], ["HF"])
                    else:
                        P.op("dve", lambda e: e.tensor_tensor_scan(out=rev_ap(HB[:]), data0=rev_ap(R[:]), data1=rev_ap(I[:]),
                                                                  initial=0.0, op0=ALU.mult, op1=ALU.add), ["R", "I", "XA"], ["XA"])
                TT("pool", HF[:], HF[:], HB[:], ALU.add, ["HF", "XA"], ["HF"])
                G, G2 = R, I
                for tt in range(NT):
                    b = nb()
                    proj_fm(WIN, 256 + j * 128, 128, tt, b)
                    CP("act", G[:, tt * 512:(tt + 1) * 512], psb[b][:], ["ps%d" % b], ["R"])
                TT("pool", G2[:], G[:], G[:], ALU.mult, ["R"], ["I"])
                TS("dve", G2[:], G2[:], 0.044715, 1.0, ALU.mult, ALU.add, ["I"], ["I"])
                TT("pool", G2[:], G2[:], G[:], ALU.mult, ["I", "R"], ["I"])
                ACT(G2[:], G2[:], AF.Sigmoid, ["I"], ["I"], scale=1.5957691216057308)
                TT("dve", G[:], G[:], G2[:], ALU.mult, ["R", "I"], ["R"])
                TT("dve", YAB[j][:], HF[:], G[:], ALU.mult, ["HF", "R"], ["YAB%d" % j])
            SQ = [arb(o0 + jj * 1024, T) for jj in range(2)]
            RB = S[1]
            for j in range(2):
                ACT(SQ[j][:], YAB[j][:], AF.Square, ["YAB%d" % j], ["XA"])
            for tt in range(NT):
                sl = slice(tt * 512, (tt + 1) * 512)
                b = nb()
                for j in range(2):
                    MM(psb[b][:], ONESB[:], SQ[j][:, sl], j == 0, j == 1, ["ONESB", "XA"], ["ps%d" % b])
                rsqrt_bc(RB[:, sl], psb[b][:], 1.0 / 256, ["ps%d" % b, "SM0"], ["XC"])
            for j in range(2):
                STT("dve", YAB[j][:], YAB[j][:], PV[:, l, PV_GA + j:PV_GA + j + 1], RB[:], ALU.mult, ALU.mult,
                    ["YAB%d" % j, "PV", "XC"], ["YAB%d" % j])
            for dk in range(8):
                for tt in range(NT):
                    b = nb()
                    for j in range(2):
                        MM(psb[b][:], WOA[:, j, dk * 128:(dk + 1) * 128], YAB[j][:, tt * 512:(tt + 1) * 512],
                           j == 0, j == 1, ["WOA", "YAB%d" % j], ["ps%d" % b])
                    add_to_x(psb[b][:], dk, tt, ["ps%d" % b])

        def phase_b(l, WIN, o0):
            HN = 1024
            bb = [arf(o0 + i * HN, HN) for i in range(4)]
            o1 = o0 + 4 * HN
            QT = arb(o1, T)
            KZ = [arb(o1 + 1024 + hh * 1024, T) for hh in range(2)]
            KTOK = arb(o1 + 3072, T).rearrange("p (i n) -> p i n", i=16)
            VZ = [arb(o1 + 4096 + cp * 1024, T).rearrange("p (i n) -> p i n", i=16) for cp in range(2)]
            o3 = o1 + 6144
            O = arf(o3, T)
            UF = arf(o3 + 2048, 2048)
            SBFall = arb(o3 + 2048, 32 * 128).rearrange("p (r n) -> p r n", r=32)
            D3 = arb(o3 + 4096, 2048)
            SALL = arb(o3 + 5120, 2048)
            WOB = arb(o3 + 6144, D)
            SCB = arb(o3 + 6656, 4 * 128).rearrange("p (u n) -> p u n", u=4)
            assert o3 + 6912 <= ARW
            YB = arb(o1 + 3072, T)
            FAC = SM[:, 64:224].rearrange("p (f c) -> p f c", f=5)
            FACR = SM[:, 224:320].rearrange("p (f c) -> p f c", f=3)
            MEMSET("pool", KZ[0][64:128, :], 0.0, ["KZ"])
            MEMSET("pool", KZ[1][0:64, :], 0.0, ["KZ"])
            MEMSET("pool", VZ[0][64:128, :, :], 0.0, ["VZ"])
            MEMSET("pool", VZ[1][0:64, :, :], 0.0, ["VZ"])
            MEMSET("pool", SCB[64:128, 0:2, :], 0.0, ["SCB0", "SCB1"])
            MEMSET("pool", SCB[0:64, 2:4, :], 0.0, ["SCB2", "SCB3"])
            LBT = SM[:, 16:32]
            ACT(LBT[:, 0:8], PV[:, l, PV_LB:PV_LB + 8], AF.Exp, ["PV"], ["LBT"])
            TT("dve", LBT[:, 8:12], LBT[:, 0:4], LBT[:, 4:8], ALU.add, ["LBT"], ["LBT"])
            RECIP(LBT[:, 8:12], LBT[:, 8:12], ["LBT"], ["LBT"])
            if l == 0:
                MEMSET("dve", LBT[:, 12:16], 0.0, ["LBT"])
            else:
                TT("dve", LBT[:, 12:16], LBT[:, 4:8], LBT[:, 8:12], ALU.mult, ["LBT"], ["LBT"])
            TS("dve", LBT[:, 8:12], LBT[:, 12:16], -1.0, 1.0, ALU.mult, ALU.add, ["LBT"], ["LBT"])
            for jp in range(2):
                P.dma("pool", WOB, wout_d[l, 256 + jp * 128:256 + (jp + 1) * 128, :], writes=["WOB"])
                for i in range(16):
                    b = nb(0, 4)
                    for k in range(8):
                        MM(psb[b][:, 0:128], HT[:, k, i * 128:(i + 1) * 128], WIN[:, k, 1280 + jp * 128:1280 + (jp + 1) * 128],
                           k == 0, k == 7, ["WIN", "HT%d" % k], ["ps%d" % b])
                    CP("act", VZ[0][0:64, i, :], psb[b][0:64, 0:128], ["ps%d" % b], ["VZ"])
                    CP("dve", VZ[1][64:128, i, :], psb[b][64:128, 0:128], ["ps%d" % b], ["VZ"])
                for d in range(2):
                    zc = (768 if d == 0 else 1024) + jp * 128
                    lbc = d * 2 + jp
                    for hf in range(2):
                        t0 = hf * HN
                        F_, LG_, B_, X_ = bb
                        for t2 in range(2):
                            tt = hf * 2 + t2
                            sl = slice(t2 * 512, (t2 + 1) * 512)
                            b = nb(0, 4)
                            proj_fm(WIN, zc, 128, tt, b)
                            ACT(F_[:, sl], psb[b][:], AF.Sigmoid, ["ps%d" % b], ["bF"])
                        TS("dve", F_[:], F_[:], LBT[:, 8 + lbc:9 + lbc], LBT[:, 12 + lbc:13 + lbc], ALU.mult, ALU.add,
                           ["bF", "LBT"], ["bF"])
                        ACT(LG_[:], F_[:], AF.Ln, ["bF"], ["bL"])
                        TS("pool", F_[:], F_[:], -1.0, 1.0, ALU.mult, ALU.add, ["bF"], ["bF"])
                        init = 0.0 if hf == 0 else SM[:, 40:41]
                        ones_bc = fview(SM[:, 1:2], [(0, HN)])
                        P.op("dve", lambda e, init=init, B_=B_, LG_=LG_, ones_bc=ones_bc: e.tensor_tensor_scan(
                            out=B_[:], data0=ones_bc, data1=LG_[:], initial=init, op0=ALU.mult, op1=ALU.add),
                            ["SM1", "bL", "SMc"], ["bB"])
                        CP("dve", SM[:, 40:41], B_[:, HN - 1:HN], ["bB"], ["SMc"])
                        B3 = B_[:].rearrange("p (c n) -> p c n", n=64)
                        LG3 = LG_[:].rearrange("p (c n) -> p c n", n=64)
                        c0 = hf * 16
                        if d == 1:
                            CP("dve", FAC[:, 2, c0:c0 + 16], B3[:, :, 63], ["bB"], ["FAC"])
                            TT("dve", B_[:], B_[:], LG_[:], ALU.subtract, ["bB", "bL"], ["bB"])
                        CP("dve", FAC[:, 0, c0:c0 + 16], B3[:, :, 32], ["bB"], ["FAC"])
                        if d == 0:
                            CP("dve", FAC[:, 1, c0:c0 + 16], B3[:, :, 63], ["bB"], ["FAC"])
                        else:
                            CP("dve", FAC[:, 1, c0:c0 + 16], B3[:, :, 0], ["bB"], ["FAC"])
                        TT("dve", B3, B3, FAC[:, 0, c0:c0 + 16].to_broadcast([128, 16, 64]), ALU.subtract,
                           ["bB", "FAC"], ["bB"])
                        sgn_q = 1.0 if d == 0 else -1.0
                        ACT(LG_[:], B_[:], AF.Exp, ["bB"], ["bL"], scale=sgn_q)
                        for t2 in range(2):
                            tt = hf * 2 + t2
                            sl = slice(t2 * 512, (t2 + 1) * 512)
                            b = nb(0, 4)
                            proj_fm(WIN, 512 + jp * 128, 128, tt, b)
                            ACT(X_[:, sl], psb[b][:], AF.Silu, ["ps%d" % b], ["bX"])
                        STT("dve", QT[:, t0:t0 + HN], X_[:], 0.125, LG_[:], ALU.mult, ALU.mult, ["bX", "bL"], ["QT"])
                        ACT(LG_[:], B_[:], AF.Exp, ["bB", "QT"], ["bL"], scale=-sgn_q)
                        TT("pool", KZ[0][0:64, t0:t0 + HN], F_[0:64, :], LG_[0:64, :], ALU.mult, ["bF", "bL"], ["KZ"])
                        TT("dve", KZ[1][64:128, t0:t0 + HN], F_[64:128, :], LG_[64:128, :], ALU.mult, ["bF", "bL"], ["KZ"])
                    Fm, Fl = FAC[:, 0, :], FAC[:, 1, :]
                    if d == 0:
                        CP("dve", FAC[:, 2, 0:1], Fm[:, 0:1], ["FAC"], ["FAC"])
                        TT("dve", FAC[:, 2, 1:32], Fm[:, 1:32], Fl[:, 0:31], ALU.subtract, ["FAC"], ["FAC"])
                        TT("dve", FAC[:, 3, :], Fl, Fm, ALU.subtract, ["FAC"], ["FAC"])
                        CP("dve", FAC[:, 4, 0:1], Fl[:, 0:1], ["FAC"], ["FAC"])
                        TT("dve", FAC[:, 4, 1:32], Fl[:, 1:32], Fl[:, 0:31], ALU.subtract, ["FAC"], ["FAC"])
                    else:
                        TT("dve", FAC[:, 3, :], Fm, Fl, ALU.subtract, ["FAC"], ["FAC"])
                        TT("dve", FAC[:, 4, :], FAC[:, 2, :], Fl, ALU.subtract, ["FAC"], ["FAC"])
                        TT("dve", FAC[:, 2, :], FAC[:, 2, :], Fm, ALU.subtract, ["FAC"], ["FAC"])
                    ACT(FAC[:, 2:5, :], FAC[:, 2:5, :], AF.Exp, ["FAC"], ["FAC"])
                    for i in range(16):
                        b = nb(0, 4)
                        for hh in range(2):
                            MM(psb[b][:, 0:128], KZ[hh][:, i * 128:(i + 1) * 128], IDB[:], hh == 0, hh == 1, ["KZ", "IDB"], ["ps%d" % b])
                        CP("act" if i % 2 else "dve", KTOK[:, i, :], psb[b][:, 0:128], ["ps%d" % b], ["KTOK"])
                    if d == 0:
                        CP("dve", FACR[:, :, :], FAC[:, 2:5, :], ["FAC"], ["FACR"])
                    else:
                        f0 = FAC[:, 2, 31:32]
                        CP("dve", FACR[:, :, :], bass.AP(f0.tensor, f0.offset, [list(f0.ap[0]), [32, 3], [-1, 32]]), ["FAC"], ["FACR"])
                    MEMSET("dve", FACR[:, 2, 0:1], 0.0, ["FACR"])
                    CP("dve", D3[:].rearrange("p (e r) -> p e r", r=32), fview(FACR[:, 2, :], [(0, 64), (1, 32)]), ["FACR"], ["D3"])
                    for g in range(8):
                        b = nb(0, 4)
                        for q in range(4):
                            r = g * 4 + q
                            c = r if d == 0 else 31 - r
                            i, cp = c // 2, c % 2
                            MM(psb[b][:, q * 128:(q + 1) * 128], KTOK[:, i, :], VZ[cp][:, i, :], True, True, ["KTOK", "VZ"], ["ps%d" % b])
                        for hh in range(2):
                            pb = 64 * hh
                            src = fview(psb[b][pb:pb + 64, hh * 64:hh * 64 + 1], [(128, 4), (1, 64)])
                            dst = fview(UF[pb:pb + 64, g * 4:g * 4 + 1], [(1, 4), (32, 64)])
                            CP("act" if hh == 0 else "dve", dst, src, ["ps%d" % b], ["UF"])
                    UF3 = UF[:].rearrange("p (e r) -> p e r", r=32)
                    TT("dve", UF3, UF3, fview(FACR[:, 1, :], [(0, 64), (1, 32)]), ALU.mult, ["UF", "FACR"], ["UF"])
                    P.op("dve", lambda e: e.tensor_tensor_scan(out=SALL[:], data0=D3[:], data1=UF[:], initial=0.0,
                                                              op0=ALU.mult, op1=ALU.add), ["D3", "UF"], ["SALL"])
                    MEMSET("pool", SBFall[0:64, :, 64:128], 0.0, ["UF"])
                    MEMSET("pool", SBFall[64:128, :, 0:64], 0.0, ["UF"])
                    for hh in range(2):
                        pb = 64 * hh
                        src = fview(SALL[pb:pb + 64, 0:1], [(1, 31), (32, 64)])
                        TT("dve", SBFall[pb:pb + 64, 1:32, hh * 64:(hh + 1) * 64], src,
                           fview(FACR[pb:pb + 64, 0, 1:2], [(1, 31), (0, 64)]), ALU.mult, ["SALL", "FACR"], ["UF"])
                    def cinfo(r):
                        c = r if d == 0 else 31 - r
                        return c, c // 2, c % 2

                    def b_scores(r):
                        c, i, cp = cinfo(r)
                        tb = 64 * cp
                        cs = slice(c * 64, (c + 1) * 64)
                        su = cp * 2 + (r // 2) % 2
                        bs = nb(0, 4)
                        for hh in range(2):
                            MM(psb[bs][tb:tb + 64, hh * 64:(hh + 1) * 64], KZ[hh][:, cs], QT[:, cs], True, True,
                               ["KZ", "QT"], ["ps%d" % bs])
                        TT("dve", SCB[tb:tb + 64, su, :].rearrange("p (h n) -> p h n", h=2),
                           psb[bs][tb:tb + 64, 0:128].rearrange("p (h n) -> p h n", h=2),
                           fview(MASKS[tb:tb + 64, d, :], [(0, 2), (1, 64)]), ALU.mult,
                           ["ps%d" % bs, "MASKS"], ["SCB%d" % su])

                    b_scores(0)
                    b_scores(1)
                    pso_b = None
                    for r in range(32):
                        if r + 2 < 32:
                            b_scores(r + 2)
                        c, i, cp = cinfo(r)
                        cs = slice(c * 64, (c + 1) * 64)
                        if r % 8 == 0:
                            pso_b = 4 + (r // 8) % 4
                        pso = psb[pso_b]
                        ocol = (c % 8) * 64
                        su = cp * 2 + (r // 2) % 2
                        for hh in range(2):
                            pb = 64 * hh
                            MM(pso[pb:pb + 64, ocol:ocol + 64], VZ[cp][:, i, pb:pb + 64], SCB[:, su, hh * 64:(hh + 1) * 64],
                               True, r == 0, ["VZ", "SCB%d" % su], ["ps%d" % pso_b])
                        if r > 0:
                            MM(pso[:, ocol:ocol + 64], SBFall[:, r, :], QT[:, cs], False, True, ["UF", "QT"], ["ps%d" % pso_b])
                        if r % 8 == 7:
                            g0 = (c // 8) * 512
                            if d == 0:
                                CP("act", O[:, g0:g0 + 512], pso[:], ["ps%d" % pso_b], ["O"])
                            else:
                                TT("dve", O[:, g0:g0 + 512], pso[:], O[:, g0:g0 + 512], ALU.add, ["ps%d" % pso_b, "O"], ["O"])
                SQ = arb(o0 + 2048, T)
                RB = arf(o0, T)
                GG = arf(o0 + 3072, 1024)
                ACT(SQ[:], O[:], AF.Square, ["O"], ["bB"])
                for tt in range(NT):
                    sl = slice(tt * 512, (tt + 1) * 512)
                    b = nb(0, 4)
                    MM(psb[b][:], BONESB[:], SQ[:, sl], True, True, ["BONESB", "bB"], ["ps%d" % b])
                    rsqrt_bc(RB[:, sl], psb[b][:], 1.0 / 64, ["ps%d" % b, "SM0"], ["bF", "bL"])
                    STT("dve", O[:, sl], O[:, sl], PV[:, l, PV_HGN:PV_HGN + 1], RB[:, sl], ALU.mult, ALU.mult, ["O", "PV", "bF", "bL"], ["O"])
                    b = nb(0, 4)
                    proj_fm(WIN, 1536 + jp * 128, 128, tt, b)
                    ACT(GG[:, 0:512], psb[b][:], AF.Silu, ["ps%d" % b], ["bX"])
                    TT("dve", YB[:, sl], O[:, sl], GG[:, 0:512], ALU.mult, ["O", "bX"], ["KTOK"])
                for dk in range(8):
                    for tt in range(NT):
                        b = nb(0, 4)
                        MM(psb[b][:], WOB[:, dk * 128:(dk + 1) * 128], YB[:, tt * 512:(tt + 1) * 512], True, True,
                           ["WOB", "KTOK"], ["ps%d" % b])
                        add_to_x(psb[b][:], dk, tt, ["ps%d" % b])
                for kk_ in ("bF", "bL", "bB", "bX"):
                    pass

        def rope_tables(s, R0):
            PI = arf(R0, 16).bitcast(I32)
            PF = arf(R0 + 16, 16)
            ANG = arf(R0 + 32, 512).rearrange("p (c i f) -> p c i f", c=2, i=16)
            KK = arf(R0 + 544, 512)
            KI = arf(R0 + 1056, 512).bitcast(I32)
            P.dma("sp", PI, pos_d[s], writes=["PI"])
            CP("dve", PF, PI, ["PI"], ["PF"])
            ifq = CONST[:, C_IFQ:C_IFQ + 16]
            TT("dve", ANG[:, 1], fview(PF, [(1, 16), (0, 16)]), fview(ifq, [(0, 16), (1, 16)]), ALU.mult, ["PF", "CONST"], ["ANG"])
            P.op("dve", lambda e: e.tensor_scalar_add(out=ANG[:, 0], in0=ANG[:, 1], scalar1=float(np.pi / 2)), ["ANG"], ["ANG"])
            A = ANG[:].rearrange("p c i f -> p (c i f)")
            K = KK
            TS("dve", K, A, float(1.0 / (2 * np.pi)), 0.5, ALU.mult, ALU.add, ["ANG"], ["KK"])
            CP("dve", KI, K, ["KK"], ["KI"])
            CP("dve", K, KI, ["KI"], ["KK"])
            C1 = 6.28125
            C2 = float(2 * np.pi - 6.28125)
            STT("dve", A, K, -C1, A, ALU.mult, ALU.add, ["KK", "ANG"], ["ANG"])
            STT("dve", A, K, -C2, A, ALU.mult, ALU.add, ["KK", "ANG"], ["ANG"])
            TS("dve", K, A, float(np.pi), float(-2 * np.pi), ALU.is_gt, ALU.mult, ["ANG"], ["KK"])
            TT("dve", A, A, K, ALU.add, ["ANG", "KK"], ["ANG"])
            TS("dve", K, A, float(-np.pi), float(2 * np.pi), ALU.is_lt, ALU.mult, ["ANG"], ["KK"])
            TT("dve", A, A, K, ALU.add, ["ANG", "KK"], ["ANG"])
            P.op("dve", lambda e: e.tensor_scalar_min(out=A, in0=A, scalar1=3.1415925), ["ANG"], ["ANG"])
            P.op("dve", lambda e: e.tensor_scalar_max(out=A, in0=A, scalar1=-3.1415925), ["ANG"], ["ANG"])
            ACT(ROPE[:].rearrange("p c i f -> p (c i f)"), A, AF.Sin, ["ANG"], ["ROPE"])

        def phase_c(l, s, WIN, base):
            KTh = HT
            VT = arb(0, 16 * 4 * 192).rearrange("p (i m e) -> p i m e", i=16, m=4)
            RD = arf(6144, 512)
            CQG = arb(base + 4096, 2 * T).rearrange("p (k t) -> p k t", k=2)
            WUQ = arb(base + 6144, 2 * 768).rearrange("p (k n) -> p k n", k=2)
            WOC = arb(base + 6912, 4 * D).rearrange("p (m n) -> p m n", m=4)
            sm0 = base + 8960
            RS = arf(sm0, 64)
            T8 = arf(sm0 + 64, 64)
            TA = arf(sm0 + 128, 128)
            TB = arf(sm0 + 256, 128)
            QS = arf(sm0 + 384, 1024)
            QQ = arf(sm0 + 1408, 1024)
            QB = arb(sm0 + 2432, 1024)
            KB = QB
            R0 = sm0 + 2944
            assert R0 + 5376 <= ARW, R0
            CKG = arb(R0, T)
            CSQ = arb(R0 + 1024, 3 * T).rearrange("p (k t) -> p k t", k=3)
            WUK = arb(R0 + 4096, 1024)
            KR = arf(R0 + 4608, 512).rearrange("p (i n) -> p i n", i=16)
            QTh = arb(R0, 8 * 512).rearrange("p (h t) -> p h t", h=8)
            PT = arb(R0 + 2048, 3 * 512).rearrange("p (u t) -> p u t", u=3)
            YC = arb(R0 + 2816, 4 * 512).rearrange("p (m t) -> p m t", m=4)
            YSQ = arb(R0 + 3840, 4 * 512).rearrange("p (m t) -> p m t", m=4)
            YN = YSQ
            RBC = arf(R0 + 4864, 512)

            if l == 0:
                rope_tables(s, R0)
                P.fence()
            if CSTOP <= 0.1:
                return
            P.dma("pool", WUQ, wuq_d[l].rearrange("(k p) n -> p k n", p=128), writes=["WUQ"])
            P.dma("pool", WUK, wukv_d[l], writes=["WUK"])
            P.dma("pool", WOC, wout_d[l, 512:1024, :].rearrange("(m p) n -> p m n", p=128), writes=["WOC"])
            for kc in range(3):
                col = 1792 + kc * 128
                for tt in range(NT):
                    sl = slice(tt * 512, (tt + 1) * 512)
                    b = nb()
                    proj_fm(WIN, col, 128, tt, b)
                    ACT(CSQ[:, kc, sl], psb[b][:], AF.Square, ["ps%d" % b], ["CSQ"])
                    gcol = (PV_GQ + kc) if kc < 2 else PV_GKV
                    dst = CQG[:, kc, sl] if kc < 2 else CKG[:, sl]
                    P.op("dve", lambda e, dst=dst, b=b, gcol=gcol: e.tensor_scalar_mul(out=dst, in0=psb[b][:], scalar1=PV[:, l, gcol:gcol + 1]),
                         ["ps%d" % b, "PV"], ["CQG"])
            if CSTOP <= 0.2:
                return
            b = nb()
            for i in range(16):
                for kc in range(2):
                    MM(psb[b][:, i:i + 1], CSQ[:, kc, i * 128:(i + 1) * 128], ONESB[:, 0:1], kc == 0, kc == 1, ["CSQ", "ONESB"], ["ps%d" % b])
                MM(psb[b][:, 16 + i:17 + i], CSQ[:, 2, i * 128:(i + 1) * 128], ONESB[:, 0:1], True, True, ["CSQ", "ONESB"], ["ps%d" % b])
            rsqrt_bc(RS[:, 0:16], psb[b][:, 0:16], 1.0 / 256, ["ps%d" % b, "SM0"], ["RS"])
            rsqrt_bc(RS[:, 16:32], psb[b][:, 16:32], 1.0 / 128, ["ps%d" % b, "SM0"], ["RS"])
            if CSTOP <= 0.3:
                return
            b = nb()
            for i in range(16):
                for k in range(8):
                    MM(psb[b][:, i * 32:(i + 1) * 32], HT[:, k, i * 128:(i + 1) * 128], WIN[:, k, 2176:2208], k == 0, k == 7,
                       ["WIN", "HT%d" % k], ["ps%d" % b])
            CP("act", KR[:].rearrange("p i n -> p (i n)"), psb[b][:], ["ps%d" % b], ["KR"])
            P.fence()
            if CSTOP <= 1:
                return
            gq = PBQ[:, l, 0:96]
            gk = PBQ[:, l, 96:192]

            def rope_apply(dst3, src3, i, nh, rkeys, wkey):
                cos = fview(ROPE[:, 0, i, :], [(0, nh), (1, 16)])
                sin = fview(ROPE[:, 1, i, :], [(0, nh), (1, 16)])
                x1, x2 = src3[:, :, 0:16], src3[:, :, 16:32]
                a = TA[:, 0:nh * 16].rearrange("p (h n) -> p h n", h=nh)
                bq = TB[:, 0:nh * 16].rearrange("p (h n) -> p h n", h=nh)
                TT("dve", a, x1, cos, ALU.mult, rkeys + ["ROPE"], ["rpA"])
                TT("pool", bq, x2, sin, ALU.mult, rkeys + ["ROPE"], ["rpB"])
                TT("dve", dst3[:, :, 0:16], a, bq, ALU.subtract, ["rpA", "rpB"], [wkey])
                TT("dve", a, x2, cos, ALU.mult, rkeys + ["ROPE"], ["rpA"])
                TT("pool", bq, x1, sin, ALU.mult, rkeys + ["ROPE"], ["rpB"])
                TT("dve", dst3[:, :, 16:32], a, bq, ALU.add, ["rpA", "rpB"], [wkey])

            QBs = [QB, arb(base + 3584, 1024), arb(R0 + 2048, 768), arb(R0 + 2048 + 384, 768)]

            def nr_part1(i, src_q3, g_bc, qi):
                SQv = QS[:, 0:768].rearrange("p (h n) -> p h n", h=8)
                TT("dve", SQv, src_q3, src_q3, ALU.mult, ["QQ"], ["QS"])
                P.op("dve", lambda e, SQv=SQv: e.tensor_reduce(out=T8[:, 0:8], in_=SQv, axis=AX.X, op=ALU.add), ["QS"], ["T8"])
                ACT(T8[:, 0:8], T8[:, 0:8], AF.Ln, ["T8", "SM0"], ["T8"], bias=SM[:, 0:1], scale=1.0 / 96)
                ACT(T8[:, 0:8], T8[:, 0:8], AF.Exp, ["T8"], ["T8"], scale=-0.5)
                TT("dve", src_q3, src_q3, fview(T8[:, 0:8], [(1, 8), (0, 96)]), ALU.mult, ["QQ", "T8"], ["QQ"])
                TT("pool", src_q3, src_q3, fview(g_bc, [(0, 8), (1, 96)]), ALU.mult, ["QQ", "PBQ"], ["QQ"])
                Q3b = QBs[qi][:, 0:768].rearrange("p (h n) -> p h n", h=8)
                CP("dve", Q3b[:, :, 0:64], src_q3[:, :, 0:64], ["QQ"], ["QB%d" % qi])
                rope_apply(Q3b[:, :, 64:96], src_q3[:, :, 64:96], i, 8, ["QQ"], "QB%d" % qi)

            def nr_part2(qi, dstT, dcols, dkey):
                Q3b = QBs[qi][:, 0:768].rearrange("p (h n) -> p h n", h=8)
                for h in range(8):
                    bt = nb(0, 4)
                    pst = psb[bt][:].bitcast(BF16)
                    TR(pst[0:96, 0:128], Q3b[:, h, :], IDB[:], ["QB%d" % qi, "IDB"], ["ps%d" % bt])
                    CP("act" if h % 2 else "dve", dstT[0:96, h, dcols], pst[0:96, 0:128], ["ps%d" % bt], [dkey if dkey != "KTh" else "HT%d" % h])

            def k_part1(i):
                tsl = slice(i * 128, (i + 1) * 128)
                b0, b1 = nb(0, 4), nb(0, 4)
                for half, bnk in ((0, b0), (1, b1)):
                    MM(psb[bnk][:], CKG[:, tsl], WUK[:, half * 512:(half + 1) * 512], True, True, ["CQG", "WUK"], ["ps%d" % bnk])
                for half, bnk in ((0, b0), (1, b1)):
                    P.op("act", lambda e, half=half, bnk=bnk, i=i: e.activation(out=QS[:, half * 512:(half + 1) * 512], in_=psb[bnk][:],
                                                                               func=AF.Copy, scale=RS[:, 16 + i:17 + i]),
                         ["ps%d" % bnk, "RS"], ["QS"])
                KV = QS[:].rearrange("p (h n) -> p h n", h=8)
                CP("pool", VT[:, i, :, 0:64], fview(QS[:, 64:65], [(256, 4), (1, 64)]), ["QS"], ["VT"])
                CP("pool", VT[:, i, :, 128:192], fview(QS[:, 192:193], [(256, 4), (1, 64)]), ["QS"], ["VT"])
                K3 = QQ[:, 0:768].rearrange("p (h n) -> p h n", h=8)
                CP("dve", K3[:, :, 0:64], KV[:, :, 0:64], ["QS"], ["QQ"])
                CP("dve", K3[:, :, 64:96], fview(KR[:, i, :], [(0, 8), (1, 32)]), ["KR"], ["QQ"])
                nr_part1(i, K3, gk, i % 4)

            def prep_k():
                k_part1(0)
                for i in range(16):
                    if i + 1 < 16:
                        k_part1(i + 1)
                    nr_part2(i % 4, KTh, slice(i * 128, (i + 1) * 128), "KTh")

            def q_part1(qt, i4):
                i = qt * 4 + i4
                tsl = slice(i * 128, (i + 1) * 128)
                b0, b1 = nb(0, 4), nb(0, 4)
                for kc in range(2):
                    MM(psb[b0][:], CQG[:, kc, tsl], WUQ[:, kc, 0:512], kc == 0, kc == 1, ["CQG", "WUQ"], ["ps%d" % b0])
                for kc in range(2):
                    MM(psb[b1][:, 0:256], CQG[:, kc, tsl], WUQ[:, kc, 512:768], kc == 0, kc == 1, ["CQG", "WUQ"], ["ps%d" % b1])
                P.op("dve", lambda e, i=i, b0=b0: e.tensor_scalar_mul(out=QQ[:, 0:512], in0=psb[b0][:], scalar1=RS[:, i:i + 1]),
                     ["ps%d" % b0, "RS"], ["QQ"])
                P.op("dve", lambda e, i=i, b1=b1: e.tensor_scalar_mul(out=QQ[:, 512:768], in0=psb[b1][:, 0:256], scalar1=RS[:, i:i + 1]),
                     ["ps%d" % b1, "RS"], ["QQ"])
                Q3 = QQ[:, 0:768].rearrange("p (h n) -> p h n", h=8)
                nr_part1(i, Q3, gq, i4)

            def q_part2(QTdst, qkey, i4):
                nr_part2(i4, QTdst, slice(i4 * 128, (i4 + 1) * 128), qkey)

            def prep_q(qt, QTdst, qkey):
                for i4 in range(4):
                    q_part1(qt, i4)
                for i4 in range(4):
                    q_part2(QTdst, qkey, i4)

            MEMSET("pool", VT[:, :, :, 64:128], 1.0, ["VT"])
            prep_k()
            P.fence()
            if CSTOP <= 2:
                return
            scale = 96.0 ** -0.5
            NPT = 6
            PTB = arb(base, NPT * 512).rearrange("p (u t) -> p u t", u=NPT)
            QTh2 = [QTh, arb(base + NPT * 256, 8 * 512).rearrange("p (h t) -> p h t", h=8)]
            assert NPT * 256 + 2048 <= 4096
            LOOK = 3
            prep_q(0, QTh2[0], "QTh0")
            for qt in range(NT):
                QTc, qkey = QTh2[qt % 2], "QTh%d" % (qt % 2)
                steps = [(h, kt) for h in range(8) for kt in range(16)]
                sbank = {}

                def score(si):
                    h, kt = steps[si]
                    bs = nb(0, 4)
                    sbank[si] = bs
                    MM(psb[bs][:], KTh[0:96, h, kt * 128:(kt + 1) * 128], QTc[0:96, h, :], True, True, ["HT%d" % h, qkey], ["ps%d" % bs])
                    u = si % NPT
                    ACT(PTB[:, u, :], psb[bs][:], AF.Exp, ["ps%d" % bs], ["PT%d" % u], scale=scale)

                for si in range(min(LOOK, len(steps))):
                    score(si)
                for si, (h, kt) in enumerate(steps):
                    if si + LOOK < len(steps):
                        score(si + LOOK)
                    m, hb = h // 2, h % 2
                    bo = 4 + (h % 4)
                    ko = "ps%d" % bo
                    u = si % NPT
                    lw = VT[:, kt, m, 0:128] if hb == 0 else VT[:, kt, m, 64:192]
                    MM(psb[bo][:, :], lw, PTB[:, u, :], kt == 0, kt == 15, ["VT", "PT%d" % u], [ko])
                    if kt == 15:
                        pn, pd = 64 * hb, 64 * (1 - hb)
                        RECIP(RD[pd:pd + 64, :], psb[bo][pd:pd + 64, :], [ko], ["RD%d" % hb])
                        TT("dve", YC[pn:pn + 64, m, :], psb[bo][pn:pn + 64, :], RD[pd:pd + 64, :], ALU.mult, [ko, "RD%d" % hb], ["YC%d" % m])
                        if h % 2 == 1:
                            ACT(YSQ[:, m, :], YC[:, m, :], AF.Square, ["YC%d" % m], ["YSQ%d" % m])
                        if qt + 1 < NT:
                            if h < 4:
                                q_part1(qt + 1, h)
                            else:
                                q_part2(QTh2[(qt + 1) % 2], "QTh%d" % ((qt + 1) % 2), h - 4)
                b = nb(0, 4)
                for m in range(4):
                    MM(psb[b][:], ONESB[:], YSQ[:, m, :], m == 0, m == 3, ["ONESB", "YSQ%d" % m], ["ps%d" % b])
                rsqrt_bc(RBC[:], psb[b][:], 1.0 / 512, ["ps%d" % b, "SM0"], ["RBC"])
                for m in range(4):
                    STT("dve", YN[:, m, :], YC[:, m, :], PV[:, l, PV_GC + m:PV_GC + m + 1], RBC[:], ALU.mult, ALU.mult,
                        ["YC%d" % m, "PV", "RBC"], ["YSQ%d" % m])
                for dk in range(8):
                    b = nb(0, 4)
                    for m in range(4):
                        MM(psb[b][:], WOC[:, m, dk * 128:(dk + 1) * 128], YN[:, m, :], m == 0, m == 3, ["WOC", "YSQ%d" % m], ["ps%d" % b])
                    add_to_x(psb[b][:], dk, qt, ["ps%d" % b])

        def ffn(l):
            rmsnorm_to_HT(l, PV_GFFN, 16384)
            P.fence()
            moe = (l % 2 == 1)
            WS = [(arb(sl_ * 6144, 8 * 512).rearrange("p (k n) -> p k n", k=8),
                   arb(sl_ * 6144 + 2048, 8 * 512).rearrange("p (k n) -> p k n", k=8),
                   arb(sl_ * 6144 + 4096, 4 * D).rearrange("p (c n) -> p c n", c=4)) for sl_ in range(2)]
            o1 = 12288
            ACTB = [arb(o1 + a_ * 4096, 4 * T).rearrange("p (c t) -> p c t", c=4) for a_ in range(2)]
            o2 = o1 + 8192
            SG = arb(o2, 2 * 512).rearrange("p (u t) -> p u t", u=2)
            TU = arf(o2 + 512, 2 * 512).rearrange("p (u t) -> p u t", u=2)
            CBC = arb(o2 + 1536, 2 * T).rearrange("p (u t) -> p u t", u=2)
            o3 = o2 + 3584
            RB = arf(0, T)
            groups = []
            if not moe:
                nfull = D_FF // 512
                for g in range(nfull):
                    groups.append((None, g * 512, [128] * 4))
                groups.append((None, nfull * 512, [128, 64]))
            else:
                for e_ in range(NEXP):
                    for g in range(D_EXP // 512):
                        groups.append((e_, g * 512, [128] * 4))
            if moe:
                ot = 12288
                RG = arf(ot, 64).rearrange("p (k e) -> p k e", k=8)
                LG = arf(ot + 64, 128).rearrange("p (i e) -> p i e", i=16)
                MX = arf(ot + 192, 128).rearrange("p (i e) -> p i e", i=16)
                W12 = arf(ot + 320, 32).rearrange("p (a i) -> p a i", a=2)
                EQ = arf(ot + 352, 128).rearrange("p (i e) -> p i e", i=16)
                CMB = arf(ot + 480, 128).rearrange("p (i e) -> p i e", i=16)
                RSQ = arf(ot + 608, 16)
                SQB = arb(ot + 624, 128)
                CT = arf(o3, T)
                assert o3 + T <= ARW
                P.dma("sp", RG, rt_d[0].rearrange("(k p) e -> p k e", p=128), writes=["RG"])
                TT("dve", RG, RG, fview(PV[:, l, PV_GFFN:PV_GFFN + 8], [(1, 8), (0, 8)]), ALU.mult, ["RG", "PV"], ["RG"])
                bl = nb()
                for i in range(16):
                    for k in range(8):
                        MM(psb[bl][:, i * 8:(i + 1) * 8], X[:, k, i * 128:(i + 1) * 128], RG[:, k, :], k == 0, k == 7, ["X", "RG"], ["ps%d" % bl])
                br = nb()
                for i in range(16):
                    for k in range(8):
                        ACT(SQB[:], X[:, k, i * 128:(i + 1) * 128], AF.Square, ["X"], ["SQB"])
                        MM(psb[br][:, i:i + 1], SQB[:], ONESB[:, 0:1], k == 0, k == 7, ["SQB", "ONESB"], ["ps%d" % br])
                rsqrt_bc(RSQ[:], psb[br][:, 0:16], 1.0 / D, ["ps%d" % br, "SM0"], ["RSQ"])
                TT("dve", LG, psb[bl][:, 0:128].rearrange("p (i e) -> p i e", i=16), fview(RSQ, [(1, 16), (0, 8)]), ALU.mult,
                   ["ps%d" % bl, "RSQ"], ["LG"])
                for i in range(16):
                    P.op("dve", lambda e, i=i: e.max(out=MX[:, i, :], in_=LG[:, i, :]), ["LG"], ["MX"])
                TT("dve", W12[:, 0, :], MX[:, :, 0], MX[:, :, 1], ALU.subtract, ["MX"], ["W12"])
                TT("dve", W12[:, 1, :], MX[:, :, 1], MX[:, :, 0], ALU.subtract, ["MX"], ["W12"])
                ACT(W12[:].rearrange("p a i -> p (a i)"), W12[:].rearrange("p a i -> p (a i)"), AF.Sigmoid, ["W12"], ["W12"])
                TT("dve", EQ, LG, fview(MX[:, :, 0], [(8, 16), (0, 8)]), ALU.is_equal, ["LG", "MX"], ["EQ"])
                TT("dve", CMB, EQ, fview(W12[:, 0, :], [(1, 16), (0, 8)]), ALU.mult, ["EQ", "W12"], ["CMB"])
                TT("dve", EQ, LG, fview(MX[:, :, 1], [(8, 16), (0, 8)]), ALU.is_equal, ["LG", "MX", "CMB"], ["EQ"])
                TT("dve", EQ, EQ, fview(W12[:, 1, :], [(1, 16), (0, 8)]), ALU.mult, ["EQ", "W12"], ["EQ"])
                TT("dve", CMB, CMB, EQ, ALU.add, ["CMB", "EQ"], ["CMB"])
                for i in range(16):
                    bt = nb()
                    TR(psb[bt][0:8, 0:128], CMB[:, i, :], ident, ["CMB", "CONST"], ["ps%d" % bt])
                    CP("act", CT[0:8, i * 128:(i + 1) * 128], psb[bt][0:8, 0:128], ["ps%d" % bt], ["CT"])
            if moe:
                P.fence()
            rr = [0]

            def load13(gi):
                e_, c0, chunks = groups[gi]
                w1b, w3b, w2b = WS[gi % 2]
                n = sum(chunks)
                key = "WSA%d" % (gi % 2)
                if e_ is None:
                    P.dma("pool", w1b[:, :, 0:n], wgu_d[0, :, c0:c0 + n].rearrange("(k p) n -> p k n", p=128), writes=[key])
                    P.dma("pool", w3b[:, :, 0:n], wgu_d[0, :, D_FF + c0:D_FF + c0 + n].rearrange("(k p) n -> p k n", p=128), writes=[key])
                else:
                    P.dma("pool", w1b[:], w1_d[0, e_, :, c0:c0 + 512].rearrange("(k p) n -> p k n", p=128), writes=[key])
                    P.dma("pool", w3b[:], w3_d[0, e_, :, c0:c0 + 512].rearrange("(k p) n -> p k n", p=128), writes=[key])

            def load2(gi):
                e_, c0, chunks = groups[gi]
                w1b, w3b, w2b = WS[gi % 2]
                key = "WSB%d" % (gi % 2)
                if e_ is None:
                    r0 = c0
                    for ci, cs_ in enumerate(chunks):
                        if cs_ < 128:
                            MEMSET("pool", w2b[cs_:128, ci, :], 0.0, [key])
                            MEMSET("pool", ACTB[gi % 2][cs_:128, ci, :], 0.0, ["ACT%d" % (gi % 2)])
                        P.dma("pool", w2b[0:cs_, ci, :], wdn_d[0, r0:r0 + cs_, :], writes=[key])
                        r0 += cs_
                else:
                    P.dma("pool", w2b[:], w2_d[0, e_, c0:c0 + 512, :].rearrange("(c p) n -> p c n", p=128), writes=[key])

            def gu(gi):
                e_, c0, chunks = groups[gi]
                w1b, w3b, w2b = WS[gi % 2]
                key = "WSA%d" % (gi % 2)
                AB = ACTB[gi % 2]
                akey = "ACT%d" % (gi % 2)
                if e_ is not None and c0 == 0:
                    for tt in range(NT):
                        bt = nb(0, 4)
                        MM(psb[bt][:], fview(CONST[0:8, C_ID + e_:C_ID + e_ + 1], [(0, 128)]), CT[0:8, tt * 512:(tt + 1) * 512], True, True,
                           ["CONST", "CT"], ["ps%d" % bt])
                        CP("act", CBC[:, e_ % 2, tt * 512:(tt + 1) * 512], psb[bt][:], ["ps%d" % bt], ["CBC%d" % (e_ % 2)])
                off = 0
                for ci, cs_ in enumerate(chunks):
                    for tt in range(NT):
                        sl = slice(tt * 512, (tt + 1) * 512)
                        bg, bu = nb(0, 4), nb(0, 4)
                        for k in range(8):
                            MM(psb[bg][0:cs_, :], w1b[:, k, off:off + cs_], HT[:, k, sl], k == 0, k == 7, [key, "HT%d" % k], ["ps%d" % bg])
                        for k in range(8):
                            MM(psb[bu][0:cs_, :], w3b[:, k, off:off + cs_], HT[:, k, sl], k == 0, k == 7, [key, "HT%d" % k], ["ps%d" % bu])
                        u = rr[0] % 2
                        rr[0] += 1
                        ACT(SG[0:cs_, u, :], psb[bg][0:cs_, :], AF.Silu, ["ps%d" % bg], ["SG%d" % u])
                        if e_ is None:
                            TT("dve", AB[0:cs_, ci, sl], psb[bu][0:cs_, :], SG[0:cs_, u, :], ALU.mult, ["ps%d" % bu, "SG%d" % u], [akey])
                        else:
                            TT("dve", TU[:, u, :], psb[bu][:], CBC[:, e_ % 2, sl], ALU.mult, ["ps%d" % bu, "CBC%d" % (e_ % 2)], ["TU%d" % u])
                            TT("pool", AB[:, ci, sl], TU[:, u, :], SG[:, u, :], ALU.mult, ["TU%d" % u, "SG%d" % u], [akey])
                    off += cs_

            def down(gi):
                e_, c0, chunks = groups[gi]
                w1b, w3b, w2b = WS[gi % 2]
                key = "WSB%d" % (gi % 2)
                AB = ACTB[gi % 2]
                akey = "ACT%d" % (gi % 2)
                for dk in range(8):
                    for tt in range(NT):
                        b = nb(4, 8)
                        for ci, cs_ in enumerate(chunks):
                            MM(psb[b][:], w2b[:, ci, dk * 128:(dk + 1) * 128], AB[:, ci, tt * 512:(tt + 1) * 512],
                               ci == 0, ci == len(chunks) - 1, [key, akey], ["ps%d" % b])
                        add_to_x(psb[b][:], dk, tt, ["ps%d" % b])

            ng = len(groups)
            load13(0)
            load2(0)
            for gi in range(ng):
                if gi + 1 < ng:
                    load13(gi + 1)
                gu(gi)
                if gi >= 1:
                    down(gi - 1)
                if gi + 1 < ng:
                    load2(gi + 1)
            down(ng - 1)

        for s in range(nseq):
            load_x(s)
            P.fence()
            for l in range(2):
                if stop is not None and stop[1] == "norm":
                    rmsnorm_to_HT(l, PV_GMIX, 8832)
                    P.fence()
                    break
                mixer(l, s)
                if stop == (l, "mix"):
                    break
                ffn(l)
                P.fence()
                if stop == (l, "ffn"):
                    break
            store_x(s)
            P.fence()
        P.emit()
    return nc


def _consts():
    c = np.zeros((128, NCONST), np.float32)
    c[:, C_ID:C_ID + 128] = np.eye(128, dtype=np.float32)
    s = (np.arange(128) % 64)[:, None]
    t = np.arange(64)[None, :]
    c[:, C_MLOW:C_MLOW + 64] = (t >= s)
    c[:, C_MUP:C_MUP + 64] = (t <= s)
    bo = np.zeros((128, 128), np.float32)
    bo[:64, :64] = 1
    bo[64:, 64:] = 1
    c[:, C_BONES:C_BONES + 128] = bo
    inv = (10000.0 ** (-np.arange(0, 16, dtype=np.float32) * np.float32(2.0 / 32))).astype(np.float32)
    c[:, C_IFQ:C_IFQ + 16] = inv[None, :]
    return c


def _pack(inp):
    pv = np.zeros((2, 128, NPV), np.float32)
    pbq = np.zeros((2, 128, 192), np.float32)
    f = lambda a: np.asarray(a, np.float32)
    for l in range(2):
        pv[l, :, PV_GMIX:PV_GMIX + 8] = f(inp["norm_mix"][l]).reshape(8, 128).T
        pv[l, :, PV_GFFN:PV_GFFN + 8] = f(inp["norm_ffn"][l]).reshape(8, 128).T
        cw = f(inp["conv_w"][l])
        for j in range(2):
            pv[l, :, PV_CW + j * 4:PV_CW + j * 4 + 4] = cw[:, j * 128:(j + 1) * 128].T
            pv[l, :, PV_CB + j] = f(inp["conv_b"][l])[j * 128:(j + 1) * 128]
            pv[l, :, PV_GA + j] = f(inp["out_g_a"][l])[j * 128:(j + 1) * 128]
            pv[l, :, PV_GQ + j] = f(inp["mla_q_norm"][l])[j * 128:(j + 1) * 128]
            for d in range(2):
                pv[l, :, PV_BA + d * 2 + j] = f(inp["lru_ba"][l, d])[j * 128:(j + 1) * 128]
                pv[l, :, PV_BX + d * 2 + j] = f(inp["lru_bx"][l, d])[j * 128:(j + 1) * 128]
                pv[l, :, PV_LAM + d * 2 + j] = f(inp["lru_lam"][l, d])[j * 128:(j + 1) * 128]
                for ll in range(2):
                    pv[l, :, PV_LB + ll * 4 + d * 2 + j] = f(inp["hg_lb_logits"][ll, d])[j * 128:(j + 1) * 128]
        pv[l, :, PV_HGN] = np.tile(f(inp["hg_norm_g"][l]), 2)
        pv[l, :, PV_GKV] = f(inp["mla_kv_norm"][l])
        pv[l, :, PV_GC:PV_GC + 4] = f(inp["out_g_c"][l]).reshape(4, 128).T
        pbq[l, :, 0:96] = f(inp["qk_norm_q"][l])[None, :]
        pbq[l, :, 96:192] = f(inp["qk_norm_k"][l])[None, :]
    return pv, pbq


_NC_CACHE = {}


def kernel(**inp):
    ncores = 8
    nseq = 2
    key = (nseq, None)
    if key not in _NC_CACHE:
        _NC_CACHE[key] = build_nc(nseq, None)
    nc = _NC_CACHE[key]
    x = np.ascontiguousarray(np.asarray(inp["x"], np.float32))
    pos = np.asarray(inp["positions"], np.int32)
    pv, pbq = _pack(inp)
    consts = _consts()
    shared = {
        "consts": consts, "pv": pv, "pbq": pbq,
    }
    for nme in ("w_in", "lru_wa", "lru_wx", "mla_w_uq", "mla_w_ukv", "w_out", "ffn_w_gate_up", "ffn_w_down",
                "moe_router", "moe_w1", "moe_w3", "moe_w2"):
        shared[nme] = np.ascontiguousarray(np.asarray(inp[nme], np.float32))
    in_maps = []
    for c in range(ncores):
        m = dict(shared)
        m["x"] = x[c * nseq:(c + 1) * nseq]
        m["pos"] = np.ascontiguousarray(pos[c * nseq:(c + 1) * nseq].reshape(nseq, 16, 128).transpose(0, 2, 1))
        in_maps.append(m)
    res = run_bass_kernel_spmd(nc, in_maps, core_ids=list(range(ncores)))
    return np.concatenate([np.asarray(r["y"], np.float32) for r in res.results], axis=0)
```

```python
import contextlib
import numpy as np
import concourse.bass as bass
import concourse.mybir as mybir
from concourse.bass_utils import run_bass_kernel_spmd

F32 = mybir.dt.float32
BF16 = mybir.dt.bfloat16
I32 = mybir.dt.int32
ALU = mybir.AluOpType
AF = mybir.ActivationFunctionType
AX = mybir.AxisListType

T = 2048
D = 1024
NT = 4
EPS = 1e-6
D_IN = 2208
D_FF = 2752
D_EXP = 3584
NEXP = 8
ARW = 26112
CSTOP = 99


class _Op:
    __slots__ = ("eng", "fn", "deps", "signals", "ticket", "kind", "dsem", "dtarget", "prev_same_sem")

    def __init__(self, eng, fn, kind):
        self.eng = eng
        self.fn = fn
        self.deps = []
        self.signals = False
        self.ticket = None
        self.kind = kind
        self.dsem = None
        self.dtarget = None
        self.prev_same_sem = None


class Prog:
    ENGS = ("pe", "act", "dve", "pool", "sp")

    def __init__(self, nc, n_dma_sems=8):
        self.nc = nc
        self.ops = {e: [] for e in self.ENGS}
        self.last_w = {}
        self.readers = {}
        self.n_dma_sems = n_dma_sems
        self.dma_count = {e: 0 for e in self.ENGS}
        self.dma_last_on_sem = {}
        self.fence_deps = None
        self.fenced = set()

    def _add_dep(self, op, p):
        if p is None or p is op:
            return
        if p.eng == op.eng and p.kind == "c" and op.kind == "c" and p.eng == "pe":
            return
        op.deps.append(p)
        p.signals = True

    def fence(self):
        deps = []
        for e in self.ENGS:
            for o in reversed(self.ops[e]):
                if o.kind == "c":
                    deps.append(o)
                    break
        deps.extend(self.dma_last_on_sem.values())
        self.fence_deps = deps
        self.fenced = set()

    def op(self, eng, fn, reads=(), writes=(), kind="c"):
        o = _Op(eng, fn, kind)
        if eng != "pe":
            extra = [r for r in reads if isinstance(r, str) and r.startswith("ps") and r[2:].isdigit() and r not in writes]
            if extra:
                writes = list(writes) + extra
        if self.fence_deps is not None and eng not in self.fenced:
            self.fenced.add(eng)
            for p in self.fence_deps:
                if p.eng == eng and p.kind == "c":
                    continue
                o.deps.append(p)
                p.signals = True
        for r in reads:
            self._add_dep(o, self.last_w.get(r))
        for w in writes:
            self._add_dep(o, self.last_w.get(w))
            rd = self.readers.get(w)
            if rd:
                for p in rd.values():
                    self._add_dep(o, p)
        for r in reads:
            d = self.readers.setdefault(r, {})
            if kind == "c":
                d[eng] = o
            else:
                d[("dma", id(o))] = o
        for w in writes:
            self.last_w[w] = o
            self.readers[w] = {}
        if kind == "d":
            i = self.dma_count[eng]
            self.dma_count[eng] = i + 1
            slot = (eng, i % self.n_dma_sems)
            o.dsem = slot
            o.dtarget = 16 * (i // self.n_dma_sems + 1)
            o.prev_same_sem = self.dma_last_on_sem.get(slot)
            self.dma_last_on_sem[slot] = o
        self.ops[eng].append(o)
        return o

    def dma(self, eng, out, in_, reads=(), writes=(), **kw):
        return self.op(eng, lambda e: e.dma_start(out=out, in_=in_, **kw), reads, writes, kind="d")

    def emit(self, final_wait_eng="sp"):
        nc = self.nc
        with contextlib.ExitStack() as st:
            esem = {}
            for e in self.ENGS:
                if any(o.kind == "c" for o in self.ops[e]):
                    esem[e] = st.enter_context(nc.semaphore("s_" + e))
            dsem = {}
            for e in self.ENGS:
                for j in range(min(self.dma_count[e], self.n_dma_sems)):
                    dsem[(e, j)] = st.enter_context(nc.semaphore("d_%s%d" % (e, j)))
            for e in self.ENGS:
                t = 0
                for o in self.ops[e]:
                    if o.kind == "c" and o.signals:
                        t += 1
                        o.ticket = t
            final_dmas = list(self.dma_last_on_sem.values())
            block = st.enter_context(nc.Block())

            def make(e):
                def body(eng):
                    waited = {}

                    def wait(key, sem, val):
                        if waited.get(key, 0) >= val:
                            return
                        waited[key] = val
                        eng.wait_ge(sem, val)

                    for o in self.ops[e]:
                        for p in o.deps:
                            if p.kind == "c":
                                wait(("c", p.eng), esem[p.eng], p.ticket)
                            else:
                                wait(("d", p.dsem), dsem[p.dsem], p.dtarget)
                        if o.kind == "d" and o.prev_same_sem is not None:
                            p = o.prev_same_sem
                            wait(("d", p.dsem), dsem[p.dsem], p.dtarget)
                        ins = o.fn(eng)
                        if o.kind == "d":
                            ins.then_inc(dsem[o.dsem], 16)
                        elif o.signals:
                            ins.then_inc(esem[e], 1)
                    if e == final_wait_eng:
                        for p in final_dmas:
                            wait(("d", p.dsem), dsem[p.dsem], p.dtarget)
                return body

            names = {"pe": "tensor", "act": "scalar", "dve": "vector", "pool": "gpsimd", "sp": "sync"}
            for e in self.ENGS:
                if not self.ops[e] and e != final_wait_eng:
                    continue
                getattr(block, names[e])(make(e))


def rev_ap(a):
    (ps, pn), (s, n) = a.ap
    return bass.AP(a.tensor, a.offset + (n - 1) * s, [[ps, pn], [-s, n]])


def fview(a, dims):
    return bass.AP(a.tensor, a.offset, [list(a.ap[0])] + [list(d) for d in dims])


PV_GMIX, PV_GFFN, PV_CW, PV_CB, PV_BA, PV_BX, PV_LAM, PV_GA, PV_LB, PV_HGN, PV_GQ, PV_GKV, PV_GC = \
    0, 8, 16, 24, 26, 30, 34, 38, 40, 48, 49, 51, 52
NPV = 60
C_ID, C_MLOW, C_MUP, C_BONES, C_IFQ = 0, 128, 192, 256, 384
NCONST = C_IFQ + 16


def build_nc(nseq=2, stop=None, phases="abc"):
    nc = bass.Bass("TRN2", target_bir_lowering=False)
    dr = lambda n, s, dt=F32, kind="ExternalInput": nc.dram_tensor(n, list(s), dt, kind=kind).ap()
    x_d = dr("x", [nseq, T, D])
    pos_d = dr("pos", [nseq, 128, 16], I32)
    consts_d = dr("consts", [128, NCONST])
    pv_d = dr("pv", [2, 128, NPV])
    pbq_d = dr("pbq", [2, 128, 192])
    w_in_d = dr("w_in", [2, D, D_IN])
    wa_d = dr("lru_wa", [2, 2, 4, 64, 64])
    wx_d = dr("lru_wx", [2, 2, 4, 64, 64])
    wuq_d = dr("mla_w_uq", [2, 256, 768])
    wukv_d = dr("mla_w_ukv", [2, 128, 1024])
    wout_d = dr("w_out", [2, D, D])
    wgu_d = dr("ffn_w_gate_up", [1, D, 2 * D_FF])
    wdn_d = dr("ffn_w_down", [1, D_FF, D])
    rt_d = dr("moe_router", [1, D, NEXP])
    w1_d = dr("moe_w1", [1, NEXP, D, D_EXP])
    w3_d = dr("moe_w3", [1, NEXP, D, D_EXP])
    w2_d = dr("moe_w2", [1, NEXP, D_EXP, D])
    y_d = dr("y", [nseq, T, D], kind="ExternalOutput")

    with contextlib.ExitStack() as st:
        def sb(name, shape, dt=F32):
            return st.enter_context(nc.sbuf_tensor(name, list(shape), dt))

        X = sb("X", [128, 8, T])
        HT = sb("HT", [128, 8, T], BF16)
        AR = sb("AR", [128, ARW])
        CONST = sb("CONST", [128, NCONST])
        PV = sb("PV", [128, 2, NPV])
        PBQ = sb("PBQ", [128, 2, 192])
        IDB = sb("IDB", [128, 128], BF16)
        ONESB = sb("ONESB", [128, 128], BF16)
        BONESB = sb("BONESB", [128, 128], BF16)
        MASKS = sb("MASKS", [128, 2, 64])
        SM = sb("SM", [128, 512])
        ROPE = sb("ROPE", [128, 2, 16, 16])
        psb = [st.enter_context(nc.psum_tensor("ps%d" % i, [128, 512], F32)) for i in range(8)]

        P = Prog(nc)
        ident = CONST[:, C_ID:C_ID + 128]

        def MM(out, lhsT, rhs, start, stop_, r, w):
            P.op("pe", lambda e: e.matmul(out, lhsT=lhsT, rhs=rhs, start=start, stop=stop_), r, w)

        def TR(out, in_, idn, r, w):
            P.op("pe", lambda e: e.transpose(out, in_, idn), r, w)

        def ACT(out, in_, func, r, w, bias=0.0, scale=1.0):
            P.op("act", lambda e: e.activation(out=out, in_=in_, func=func, bias=bias, scale=scale), r, w)

        def TT(eng, out, in0, in1, op, r, w):
            P.op(eng, lambda e: e.tensor_tensor(out=out, in0=in0, in1=in1, op=op), r, w)

        def TS(eng, out, in0, s1, s2, op0, op1, r, w):
            P.op(eng, lambda e: e.tensor_scalar(out=out, in0=in0, scalar1=s1, scalar2=s2, op0=op0, op1=op1), r, w)

        def STT(eng, out, in0, scalar, in1, op0, op1, r, w):
            eng = "dve"
            P.op(eng, lambda e: e.scalar_tensor_tensor(out=out, in0=in0, scalar=scalar, in1=in1, op0=op0, op1=op1), r, w)

        def CP(eng, out, in_, r, w):
            if eng == "act":
                P.op("act", lambda e: e.copy(out=out, in_=in_), r, w)
            else:
                P.op(eng, lambda e: e.tensor_copy(out=out, in_=in_), r, w)

        def RECIP(out, in_, r, w):
            P.op("dve", lambda e: e.reciprocal(out=out, in_=in_), r, w)

        def MEMSET(eng, ap, val, w):
            P.op(eng, lambda e: e.memset(ap, val), (), w)

        bank_rr = [0]

        def nb(lo=0, hi=8):
            i = bank_rr[0]
            i = lo + (i - lo + 1) % (hi - lo) if lo <= i < hi else lo
            bank_rr[0] = i
            return i

        def arf(off, n):
            return AR[:, off:off + n]

        def arb(off, n):
            return AR[:, off:off + n // 2].bitcast(BF16)

        P.dma("sp", CONST[:], consts_d, writes=["CONST"])
        P.dma("sp", PV[:], pv_d.rearrange("l p n -> p l n"), writes=["PV"])
        P.dma("sp", PBQ[:], pbq_d.rearrange("l p n -> p l n"), writes=["PBQ"])
        CP("dve", IDB[:], ident, ["CONST"], ["IDB"])
        MEMSET("dve", ONESB[:], 1.0, ["ONESB"])
        CP("dve", BONESB[:], CONST[:, C_BONES:C_BONES + 128], ["CONST"], ["BONESB"])
        CP("dve", MASKS[:, 0, :], CONST[:, C_MLOW:C_MLOW + 64], ["CONST"], ["MASKS"])
        CP("dve", MASKS[:, 1, :], CONST[:, C_MUP:C_MUP + 64], ["CONST"], ["MASKS"])

        def rsqrt_bc(out, ps, scale, r, w):
            ACT(out, ps, AF.Ln, r, w, bias=SM[:, 0:1], scale=scale)
            ACT(out, out, AF.Exp, w, w, scale=-0.5)

        MEMSET("dve", SM[:, 0:1], EPS, ["SM0"])
        MEMSET("dve", SM[:, 1:2], 1.0, ["SM1"])

        def load_x(s):
            stg = [arf(0, 1024), arf(1024, 1024)]
            for i in range(16):
                sg = stg[i % 2]
                P.dma("sp", sg, x_d[s, i * 128:(i + 1) * 128, :], writes=["stg%d" % (i % 2)])
                for h in range(2):
                    b = nb()
                    for q in range(4):
                        k = h * 4 + q
                        TR(psb[b][:, q * 128:(q + 1) * 128], sg[:, k * 128:(k + 1) * 128], ident,
                           ["stg%d" % (i % 2), "CONST"], ["ps%d" % b])
                    eng = "act" if h == 0 else "dve"
                    CP(eng, X[:, h * 4:(h + 1) * 4, i * 128:(i + 1) * 128],
                       psb[b][:].rearrange("p (q t) -> p q t", q=4), ["ps%d" % b], ["X"])

        def store_x(s):
            stg = [arf(0, 1024), arf(1024, 1024)]
            for i in range(16):
                sg = stg[i % 2]
                for h in range(2):
                    b = nb()
                    for q in range(4):
                        k = h * 4 + q
                        TR(psb[b][:, q * 128:(q + 1) * 128], X[:, k, i * 128:(i + 1) * 128], ident,
                           ["X", "CONST"], ["ps%d" % b])
                    eng = "act" if h == 0 else "dve"
                    CP(eng, sg[:, h * 512:(h + 1) * 512], psb[b][:], ["ps%d" % b], ["stg%d" % (i % 2)])
                P.dma("sp", y_d[s, i * 128:(i + 1) * 128, :], sg, reads=["stg%d" % (i % 2)])

        def rmsnorm_to_HT(l, gcol, scr_off):
            RB = arf(scr_off, T)
            for k in range(8):
                ACT(HT[:, k, :], X[:, k, :], AF.Square, ["X"], ["HT%d" % k])
            for tt in range(NT):
                b = nb()
                for k in range(8):
                    MM(psb[b][:], ONESB[:], HT[:, k, tt * 512:(tt + 1) * 512], k == 0, k == 7,
                       ["ONESB", "HT%d" % k], ["ps%d" % b])
                rsqrt_bc(RB[:, tt * 512:(tt + 1) * 512], psb[b][:], 1.0 / D, ["ps%d" % b, "SM0"], ["RB%d" % tt])
            for k in range(8):
                eng = "dve" if k % 2 == 0 else "pool"
                STT(eng, HT[:, k, :], X[:, k, :], PV[:, l, gcol + k:gcol + k + 1], RB[:], ALU.mult, ALU.mult,
                    ["X", "PV"] + ["RB%d" % t_ for t_ in range(NT)], ["HT%d" % k])

        HTk = ["HT%d" % k for k in range(8)]

        def proj_fm(WIN, col0, m, tt, b, pbase=0):
            for k in range(8):
                MM(psb[b][pbase:pbase + m, :], WIN[:, k, col0:col0 + m], HT[:, k, tt * 512:(tt + 1) * 512],
                   k == 0, k == 7, ["WIN", "HT%d" % k], ["ps%d" % b])

        def add_to_x(ps, dk, tt, r):
            TT("dve", X[:, dk, tt * 512:(tt + 1) * 512], ps, X[:, dk, tt * 512:(tt + 1) * 512], ALU.add,
               r + ["X"], ["X"])

        def mixer(l, s):
            WIN = arb(0, 8 * D_IN).rearrange("p (k n) -> p k n", k=8)
            P.dma("pool", WIN[:, 0:4, :], w_in_d[l, 0:512, :].rearrange("(k p) n -> p k n", p=128), writes=["WIN"])
            P.dma("pool", WIN[:, 4:8, :], w_in_d[l, 512:1024, :].rearrange("(k p) n -> p k n", p=128), writes=["WIN"])
            SC0 = 8832
            rmsnorm_to_HT(l, PV_GMIX, SC0)
            if "a" in phases:
                phase_a(l, WIN, SC0)
                P.fence()
            if "b" in phases:
                phase_b(l, WIN, SC0)
                P.fence()
            if "c" in phases:
                phase_c(l, s, WIN, SC0)
                P.fence()

        def phase_a(l, WIN, o0):
            S = [arf(o0 + i * T, T) for i in range(6)]
            XCB = arb(o0 + 6 * T, T)
            YAB = [arb(o0 + 6 * T + 1024 + j * 1024, T) for j in range(2)]
            WOA = arb(o0 + 6 * T + 3072, 2 * D).rearrange("p (k n) -> p k n", k=2)
            BD = arb(o0 + 6 * T + 3072 + 1024, 8 * 128).rearrange("p (g n) -> p g n", g=8)
            P.dma("pool", WOA, wout_d[l, 0:256, :].rearrange("(k p) n -> p k n", p=128), writes=["WOA"])
            MEMSET("pool", BD, 0.0, ["BD"])
            for g, wd in enumerate((wa_d, wx_d)):
                for d in range(2):
                    for j in range(2):
                        gi = g * 4 + d * 2 + j
                        for hb in range(2):
                            P.dma("pool", BD[hb * 64:(hb + 1) * 64, gi, hb * 64:(hb + 1) * 64],
                                  wd[l, d, 2 * j + hb], writes=["BD"])
            ACT(SM[:, 8:12], PV[:, l, PV_LAM:PV_LAM + 4], AF.Exp, ["PV"], ["SMA"], scale=-1.0)
            ACT(SM[:, 8:12], SM[:, 8:12], AF.Ln, ["SMA"], ["SMA"], bias=SM[:, 1:2], scale=1.0)
            TS("dve", SM[:, 12:16], SM[:, 8:12], -16.0, None, ALU.mult, ALU.mult, ["SMA"], ["SMB"]) if False else None
            P.op("dve", lambda e: e.tensor_scalar_mul(out=SM[:, 12:16], in0=SM[:, 8:12], scalar1=-16.0), ["SMA"], ["SMB"])
            P.op("dve", lambda e: e.tensor_scalar_mul(out=SM[:, 8:12], in0=SM[:, 8:12], scalar1=-8.0), ["SMA", "SMB"], ["SMA"])
            for j in range(2):
                XA, XC, R, I, A2, HF = S
                cw = lambda kk: PV[:, l, PV_CW + j * 4 + kk:PV_CW + j * 4 + kk + 1]
                for tt in range(NT):
                    b = nb()
                    proj_fm(WIN, j * 128, 128, tt, b)
                    CP("act", XA[:, tt * 512:(tt + 1) * 512], psb[b][:], ["ps%d" % b], ["XA"])
                TS("dve", XC[:], XA[:], cw(2), PV[:, l, PV_CB + j:PV_CB + j + 1], ALU.mult, ALU.add, ["XA", "PV"], ["XC"])
                STT("dve", XC[:, 2:T], XA[:, 0:T - 2], cw(0), XC[:, 2:T], ALU.mult, ALU.add, ["XA", "PV", "XC"], ["XC"])
                STT("dve", XC[:, 1:T], XA[:, 0:T - 1], cw(1), XC[:, 1:T], ALU.mult, ALU.add, ["XA", "PV", "XC"], ["XC"])
                STT("dve", XC[:, 0:T - 1], XA[:, 1:T], cw(3), XC[:, 0:T - 1], ALU.mult, ALU.add, ["XA", "PV", "XC"], ["XC"])
                CP("pool", XCB[:], XC[:], ["XC"], ["XCB"])
                HB = XA
                for d in range(2):
                    for tt in range(NT):
                        sl = slice(tt * 512, (tt + 1) * 512)
                        b = nb()
                        MM(psb[b][:], BD[:, 0 * 4 + d * 2 + j, :], XCB[:, sl], True, True, ["BD", "XCB"], ["ps%d" % b])
                        ACT(R[:, sl], psb[b][:], AF.Sigmoid, ["ps%d" % b, "PV"], ["R"],
                            bias=PV[:, l, PV_BA + d * 2 + j:PV_BA + d * 2 + j + 1])
                        b = nb()
                        MM(psb[b][:], BD[:, 1 * 4 + d * 2 + j, :], XCB[:, sl], True, True, ["BD", "XCB"], ["ps%d" % b])
                        ACT(I[:, sl], psb[b][:], AF.Sigmoid, ["ps%d" % b, "PV"], ["I"],
                            bias=PV[:, l, PV_BX + d * 2 + j:PV_BX + d * 2 + j + 1])
                    ci = d * 2 + j
                    ACT(A2[:], R[:], AF.Exp, ["R", "SMB"], ["A2"], scale=SM[:, 12 + ci:13 + ci])
                    ACT(R[:], R[:], AF.Exp, ["R", "SMA"], ["R"], scale=SM[:, 8 + ci:9 + ci])
                    TS("dve", A2[:], A2[:], -1.0, 1.0, ALU.mult, ALU.add, ["A2"], ["A2"])
                    P.op("dve", lambda e: e.tensor_scalar_max(out=A2[:], in0=A2[:], scalar1=0.0), ["A2"], ["A2"])
                    ACT(A2[:], A2[:], AF.Sqrt, ["A2"], ["A2"])
                    TT("pool", I[:], I[:], XC[:], ALU.mult, ["I", "XC"], ["I"])
                    TT("dve", I[:], I[:], A2[:], ALU.mult, ["I", "A2"], ["I"])
                    if d == 0:
                        P.op("dve", lambda e: e.tensor_tensor_scan(out=HF[:], data0=R[:], data1=I[:], initial=0.0,
                                                                  op0=ALU.mult, op1=ALU.add), ["R", "I"], ["HF"])
                    else:
                        P.op("dve", lambda e: e.tensor_tensor_scan(out=rev_ap(HB[:]), data0=rev_ap(R[:]), data1=rev_ap(I[:]),
                                                                  initial=0.0, op0=ALU.mult, op1=ALU.add), ["R", "I", "XA"], ["XA"])
                TT("pool", HF[:], HF[:], HB[:], ALU.add, ["HF", "XA"], ["HF"])
                G, G2 = R, I
                for tt in range(NT):
                    b = nb()
                    proj_fm(WIN, 256 + j * 128, 128, tt, b)
                    CP("act", G[:, tt * 512:(tt + 1) * 512], psb[b][:], ["ps%d" % b], ["R"])
                TT("pool", G2[:], G[:], G[:], ALU.mult, ["R"], ["I"])
                TS("dve", G2[:], G2[:], 0.044715, 1.0, ALU.mult, ALU.add, ["I"], ["I"])
                TT("pool", G2[:], G2[:], G[:], ALU.mult, ["I", "R"], ["I"])
                ACT(G2[:], G2[:], AF.Sigmoid, ["I"], ["I"], scale=1.5957691216057308)
                TT("dve", G[:], G[:], G2[:], ALU.mult, ["R", "I"], ["R"])
                TT("dve", YAB[j][:], HF[:], G[:], ALU.mult, ["HF", "R"], ["YAB%d" % j])
            SQ = [arb(o0 + jj * 1024, T) for jj in range(2)]
            RB = S[1]
            for j in range(2):
                ACT(SQ[j][:], YAB[j][:], AF.Square, ["YAB%d" % j], ["XA"])
            for tt in range(NT):
                sl = slice(tt * 512, (tt + 1) * 512)
                b = nb()
                for j in range(2):
                    MM(psb[b][:], ONESB[:], SQ[j][:, sl], j == 0, j == 1, ["ONESB", "XA"], ["ps%d" % b])
                rsqrt_bc(RB[:, sl], psb[b][:], 1.0 / 256, ["ps%d" % b, "SM0"], ["XC"])
            for j in range(2):
                STT("dve", YAB[j][:], YAB[j][:], PV[:, l, PV_GA + j:PV_GA + j + 1], RB[:], ALU.mult, ALU.mult,
                    ["YAB%d" % j, "PV", "XC"], ["YAB%d" % j])
            for dk in range(8):
                for tt in range(NT):
                    b = nb()
                    for j in range(2):
                        MM(psb[b][:], WOA[:, j, dk * 128:(dk + 1) * 128], YAB[j][:, tt * 512:(tt + 1) * 512],
                           j == 0, j == 1, ["WOA", "YAB%d" % j], ["ps%d" % b])
                    add_to_x(psb[b][:], dk, tt, ["ps%d" % b])

        def phase_b(l, WIN, o0):
            HN = 1024
            bb = [arf(o0 + i * HN, HN) for i in range(4)]
            o1 = o0 + 4 * HN
            QT = arb(o1, T)
            KZ = [arb(o1 + 1024 + hh * 1024, T) for hh in range(2)]
            KTOK = arb(o1 + 3072, T).rearrange("p (i n) -> p i n", i=16)
            VZ = [arb(o1 + 4096 + cp * 1024, T).rearrange("p (i n) -> p i n", i=16) for cp in range(2)]
            o3 = o1 + 6144
            O = arf(o3, T)
            UF = arf(o3 + 2048, 2048)
            SBFall = arb(o3 + 2048, 32 * 128).rearrange("p (r n) -> p r n", r=32)
            D3 = arb(o3 + 4096, 2048)
            SALL = arb(o3 + 5120, 2048)
            WOB = arb(o3 + 6144, D)
            SCB = arb(o3 + 6656, 4 * 128).rearrange("p (u n) -> p u n", u=4)
            assert o3 + 6912 <= ARW
            YB = arb(o1 + 3072, T)
            FAC = SM[:, 64:224].rearrange("p (f c) -> p f c", f=5)
            FACR = SM[:, 224:320].rearrange("p (f c) -> p f c", f=3)
            MEMSET("pool", KZ[0][64:128, :], 0.0, ["KZ"])
            MEMSET("pool", KZ[1][0:64, :], 0.0, ["KZ"])
            MEMSET("pool", VZ[0][64:128, :, :], 0.0, ["VZ"])
            MEMSET("pool", VZ[1][0:64, :, :], 0.0, ["VZ"])
            MEMSET("pool", SCB[64:128, 0:2, :], 0.0, ["SCB0", "SCB1"])
            MEMSET("pool", SCB[0:64, 2:4, :], 0.0, ["SCB2", "SCB3"])
            LBT = SM[:, 16:32]
            ACT(LBT[:, 0:8], PV[:, l, PV_LB:PV_LB + 8], AF.Exp, ["PV"], ["LBT"])
            TT("dve", LBT[:, 8:12], LBT[:, 0:4], LBT[:, 4:8], ALU.add, ["LBT"], ["LBT"])
            RECIP(LBT[:, 8:12], LBT[:, 8:12], ["LBT"], ["LBT"])
            if l == 0:
                MEMSET("dve", LBT[:, 12:16], 0.0, ["LBT"])
            else:
                TT("dve", LBT[:, 12:16], LBT[:, 4:8], LBT[:, 8:12], ALU.mult, ["LBT"], ["LBT"])
            TS("dve", LBT[:, 8:12], LBT[:, 12:16], -1.0, 1.0, ALU.mult, ALU.add, ["LBT"], ["LBT"])
            for jp in range(2):
                P.dma("pool", WOB, wout_d[l, 256 + jp * 128:256 + (jp + 1) * 128, :], writes=["WOB"])
                for i in range(16):
                    b = nb(0, 4)
                    for k in range(8):
                        MM(psb[b][:, 0:128], HT[:, k, i * 128:(i + 1) * 128], WIN[:, k, 1280 + jp * 128:1280 + (jp + 1) * 128],
                           k == 0, k == 7, ["WIN", "HT%d" % k], ["ps%d" % b])
                    CP("act", VZ[0][0:64, i, :], psb[b][0:64, 0:128], ["ps%d" % b], ["VZ"])
                    CP("dve", VZ[1][64:128, i, :], psb[b][64:128, 0:128], ["ps%d" % b], ["VZ"])
                for d in range(2):
                    zc = (768 if d == 0 else 1024) + jp * 128
                    lbc = d * 2 + jp
                    for hf in range(2):
                        t0 = hf * HN
                        F_, LG_, B_, X_ = bb
                        for t2 in range(2):
                            tt = hf * 2 + t2
                            sl = slice(t2 * 512, (t2 + 1) * 512)
                            b = nb(0, 4)
                            proj_fm(WIN, zc, 128, tt, b)
                            ACT(F_[:, sl], psb[b][:], AF.Sigmoid, ["ps%d" % b], ["bF"])
                        TS("dve", F_[:], F_[:], LBT[:, 8 + lbc:9 + lbc], LBT[:, 12 + lbc:13 + lbc], ALU.mult, ALU.add,
                           ["bF", "LBT"], ["bF"])
                        ACT(LG_[:], F_[:], AF.Ln, ["bF"], ["bL"])
                        TS("pool", F_[:], F_[:], -1.0, 1.0, ALU.mult, ALU.add, ["bF"], ["bF"])
                        init = 0.0 if hf == 0 else SM[:, 40:41]
                        ones_bc = fview(SM[:, 1:2], [(0, HN)])
                        P.op("dve", lambda e, init=init, B_=B_, LG_=LG_, ones_bc=ones_bc: e.tensor_tensor_scan(
                            out=B_[:], data0=ones_bc, data1=LG_[:], initial=init, op0=ALU.mult, op1=ALU.add),
                            ["SM1", "bL", "SMc"], ["bB"])
                        CP("dve", SM[:, 40:41], B_[:, HN - 1:HN], ["bB"], ["SMc"])
                        B3 = B_[:].rearrange("p (c n) -> p c n", n=64)
                        LG3 = LG_[:].rearrange("p (c n) -> p c n", n=64)
                        c0 = hf * 16
                        if d == 1:
                            CP("dve", FAC[:, 2, c0:c0 + 16], B3[:, :, 63], ["bB"], ["FAC"])
                            TT("dve", B_[:], B_[:], LG_[:], ALU.subtract, ["bB", "bL"], ["bB"])
                        CP("dve", FAC[:, 0, c0:c0 + 16], B3[:, :, 32], ["bB"], ["FAC"])
                        if d == 0:
                            CP("dve", FAC[:, 1, c0:c0 + 16], B3[:, :, 63], ["bB"], ["FAC"])
                        else:
                            CP("dve", FAC[:, 1, c0:c0 + 16], B3[:, :, 0], ["bB"], ["FAC"])
                        TT("dve", B3, B3, FAC[:, 0, c0:c0 + 16].to_broadcast([128, 16, 64]), ALU.subtract,
                           ["bB", "FAC"], ["bB"])
                        sgn_q = 1.0 if d == 0 else -1.0
                        ACT(LG_[:], B_[:], AF.Exp, ["bB"], ["bL"], scale=sgn_q)
                        for t2 in range(2):
                            tt = hf * 2 + t2
                            sl = slice(t2 * 512, (t2 + 1) * 512)
                            b = nb(0, 4)
                            proj_fm(WIN, 512 + jp * 128, 128, tt, b)
                            ACT(X_[:, sl], psb[b][:], AF.Silu, ["ps%d" % b], ["bX"])
                        STT("dve", QT[:, t0:t0 + HN], X_[:], 0.125, LG_[:], ALU.mult, ALU.mult, ["bX", "bL"], ["QT"])
                        ACT(LG_[:], B_[:], AF.Exp, ["bB", "QT"], ["bL"], scale=-sgn_q)
                        TT("pool", KZ[0][0:64, t0:t0 + HN], F_[0:64, :], LG_[0:64, :], ALU.mult, ["bF", "bL"], ["KZ"])
                        TT("dve", KZ[1][64:128, t0:t0 + HN], F_[64:128, :], LG_[64:128, :], ALU.mult, ["bF", "bL"], ["KZ"])
                    Fm, Fl = FAC[:, 0, :], FAC[:, 1, :]
                    if d == 0:
                        CP("dve", FAC[:, 2, 0:1], Fm[:, 0:1], ["FAC"], ["FAC"])
                        TT("dve", FAC[:, 2, 1:32], Fm[:, 1:32], Fl[:, 0:31], ALU.subtract, ["FAC"], ["FAC"])
                        TT("dve", FAC[:, 3, :], Fl, Fm, ALU.subtract, ["FAC"], ["FAC"])
                        CP("dve", FAC[:, 4, 0:1], Fl[:, 0:1], ["FAC"], ["FAC"])
                        TT("dve", FAC[:, 4, 1:32], Fl[:, 1:32], Fl[:, 0:31], ALU.subtract, ["FAC"], ["FAC"])
                    else:
                        TT("dve", FAC[:, 3, :], Fm, Fl, ALU.subtract, ["FAC"], ["FAC"])
                        TT("dve", FAC[:, 4, :], FAC[:, 2, :], Fl, ALU.subtract, ["FAC"], ["FAC"])
                        TT("dve", FAC[:, 2, :], FAC[:, 2, :], Fm, ALU.subtract, ["FAC"], ["FAC"])
                    ACT(FAC[:, 2:5, :], FAC[:, 2:5, :], AF.Exp, ["FAC"], ["FAC"])
                    for i in range(16):
                        b = nb(0, 4)
                        for hh in range(2):
                            MM(psb[b][:, 0:128], KZ[hh][:, i * 128:(i + 1) * 128], IDB[:], hh == 0, hh == 1, ["KZ", "IDB"], ["ps%d" % b])
                        CP("act" if i % 2 else "dve", KTOK[:, i, :], psb[b][:, 0:128], ["ps%d" % b], ["KTOK"])
                    if d == 0:
                        CP("dve", FACR[:, :, :], FAC[:, 2:5, :], ["FAC"], ["FACR"])
                    else:
                        f0 = FAC[:, 2, 31:32]
                        CP("dve", FACR[:, :, :], bass.AP(f0.tensor, f0.offset, [list(f0.ap[0]), [32, 3], [-1, 32]]), ["FAC"], ["FACR"])
                    MEMSET("dve", FACR[:, 2, 0:1], 0.0, ["FACR"])
                    CP("dve", D3[:].rearrange("p (e r) -> p e r", r=32), fview(FACR[:, 2, :], [(0, 64), (1, 32)]), ["FACR"], ["D3"])
                    for g in range(8):
                        b = nb(0, 4)
                        for q in range(4):
                            r = g * 4 + q
                            c = r if d == 0 else 31 - r
                            i, cp = c // 2, c % 2
                            MM(psb[b][:, q * 128:(q + 1) * 128], KTOK[:, i, :], VZ[cp][:, i, :], True, True, ["KTOK", "VZ"], ["ps%d" % b])
                        for hh in range(2):
                            pb = 64 * hh
                            src = fview(psb[b][pb:pb + 64, hh * 64:hh * 64 + 1], [(128, 4), (1, 64)])
                            dst = fview(UF[pb:pb + 64, g * 4:g * 4 + 1], [(1, 4), (32, 64)])
                            CP("act" if hh == 0 else "dve", dst, src, ["ps%d" % b], ["UF"])
                    UF3 = UF[:].rearrange("p (e r) -> p e r", r=32)
                    TT("dve", UF3, UF3, fview(FACR[:, 1, :], [(0, 64), (1, 32)]), ALU.mult, ["UF", "FACR"], ["UF"])
                    P.op("dve", lambda e: e.tensor_tensor_scan(out=SALL[:], data0=D3[:], data1=UF[:], initial=0.0,
                                                              op0=ALU.mult, op1=ALU.add), ["D3", "UF"], ["SALL"])
                    MEMSET("pool", SBFall[0:64, :, 64:128], 0.0, ["UF"])
                    MEMSET("pool", SBFall[64:128, :, 0:64], 0.0, ["UF"])
                    for hh in range(2):
                        pb = 64 * hh
                        src = fview(SALL[pb:pb + 64, 0:1], [(1, 31), (32, 64)])
                        TT("dve", SBFall[pb:pb + 64, 1:32, hh * 64:(hh + 1) * 64], src,
                           fview(FACR[pb:pb + 64, 0, 1:2], [(1, 31), (0, 64)]), ALU.mult, ["SALL", "FACR"], ["UF"])
                    def cinfo(r):
                        c = r if d == 0 else 31 - r
                        return c, c // 2, c % 2

                    def b_scores(r):
                        c, i, cp = cinfo(r)
                        tb = 64 * cp
                        cs = slice(c * 64, (c + 1) * 64)
                        su = cp * 2 + (r // 2) % 2
                        bs = nb(0, 4)
                        for hh in range(2):
                            MM(psb[bs][tb:tb + 64, hh * 64:(hh + 1) * 64], KZ[hh][:, cs], QT[:, cs], True, True,
                               ["KZ", "QT"], ["ps%d" % bs])
                        TT("dve", SCB[tb:tb + 64, su, :].rearrange("p (h n) -> p h n", h=2),
                           psb[bs][tb:tb + 64, 0:128].rearrange("p (h n) -> p h n", h=2),
                           fview(MASKS[tb:tb + 64, d, :], [(0, 2), (1, 64)]), ALU.mult,
                           ["ps%d" % bs, "MASKS"], ["SCB%d" % su])

                    b_scores(0)
                    b_scores(1)
                    pso_b = None
                    for r in range(32):
                        if r + 2 < 32:
                            b_scores(r + 2)
                        c, i, cp = cinfo(r)
                        cs = slice(c * 64, (c + 1) * 64)
                        if r % 8 == 0:
                            pso_b = 4 + (r // 8) % 4
                        pso = psb[pso_b]
                        ocol = (c % 8) * 64
                        su = cp * 2 + (r // 2) % 2
                        for hh in range(2):
                            pb = 64 * hh
                            MM(pso[pb:pb + 64, ocol:ocol + 64], VZ[cp][:, i, pb:pb + 64], SCB[:, su, hh * 64:(hh + 1) * 64],
                               True, r == 0, ["VZ", "SCB%d" % su], ["ps%d" % pso_b])
                        if r > 0:
                            MM(pso[:, ocol:ocol + 64], SBFall[:, r, :], QT[:, cs], False, True, ["UF", "QT"], ["ps%d" % pso_b])
                        if r % 8 == 7:
                            g0 = (c // 8) * 512
                            if d == 0:
                                CP("act", O[:, g0:g0 + 512], pso[:], ["ps%d" % pso_b], ["O"])
                            else:
                                TT("dve", O[:, g0:g0 + 512], pso[:], O[:, g0:g0 + 512], ALU.add, ["ps%d" % pso_b, "O"], ["O"])
                SQ = arb(o0 + 2048, T)
                RB = arf(o0, T)
                GG = arf(o0 + 3072, 1024)
                ACT(SQ[:], O[:], AF.Square, ["O"], ["bB"])
                for tt in range(NT):
                    sl = slice(tt * 512, (tt + 1) * 512)
                    b = nb(0, 4)
                    MM(psb[b][:], BONESB[:], SQ[:, sl], True, True, ["BONESB", "bB"], ["ps%d" % b])
                    rsqrt_bc(RB[:, sl], psb[b][:], 1.0 / 64, ["ps%d" % b, "SM0"], ["bF", "bL"])
                    STT("dve", O[:, sl], O[:, sl], PV[:, l, PV_HGN:PV_HGN + 1], RB[:, sl], ALU.mult, ALU.mult, ["O", "PV", "bF", "bL"], ["O"])
                    b = nb(0, 4)
                    proj_fm(WIN, 1536 + jp * 128, 128, tt, b)
                    ACT(GG[:, 0:512], psb[b][:], AF.Silu, ["ps%d" % b], ["bX"])
                    TT("dve", YB[:, sl], O[:, sl], GG[:, 0:512], ALU.mult, ["O", "bX"], ["KTOK"])
                for dk in range(8):
                    for tt in range(NT):
                        b = nb(0, 4)
                        MM(psb[b][:], WOB[:, dk * 128:(dk + 1) * 128], YB[:, tt * 512:(tt + 1) * 512], True, True,
                           ["WOB", "KTOK"], ["ps%d" % b])
                        add_to_x(psb[b][:], dk, tt, ["ps%d" % b])
                for kk_ in ("bF", "bL", "bB", "bX"):
                    pass

        def rope_tables(s, R0):
            PI = arf(R0, 16).bitcast(I32)
            PF = arf(R0 + 16, 16)
            ANG = arf(R0 + 32, 512).rearrange("p (c i f) -> p c i f", c=2, i=16)
            KK = arf(R0 + 544, 512)
            KI = arf(R0 + 1056, 512).bitcast(I32)
            P.dma("sp", PI, pos_d[s], writes=["PI"])
            CP("dve", PF, PI, ["PI"], ["PF"])
            ifq = CONST[:, C_IFQ:C_IFQ + 16]
            TT("dve", ANG[:, 1], fview(PF, [(1, 16), (0, 16)]), fview(ifq, [(0, 16), (1, 16)]), ALU.mult, ["PF", "CONST"], ["ANG"])
            P.op("dve", lambda e: e.tensor_scalar_add(out=ANG[:, 0], in0=ANG[:, 1], scalar1=float(np.pi / 2)), ["ANG"], ["ANG"])
            A = ANG[:].rearrange("p c i f -> p (c i f)")
            K = KK
            TS("dve", K, A, float(1.0 / (2 * np.pi)), 0.5, ALU.mult, ALU.add, ["ANG"], ["KK"])
            CP("dve", KI, K, ["KK"], ["KI"])
            CP("dve", K, KI, ["KI"], ["KK"])
            C1 = 6.28125
            C2 = float(2 * np.pi - 6.28125)
            STT("dve", A, K, -C1, A, ALU.mult, ALU.add, ["KK", "ANG"], ["ANG"])
            STT("dve", A, K, -C2, A, ALU.mult, ALU.add, ["KK", "ANG"], ["ANG"])
            TS("dve", K, A, float(np.pi), float(-2 * np.pi), ALU.is_gt, ALU.mult, ["ANG"], ["KK"])
            TT("dve", A, A, K, ALU.add, ["ANG", "KK"], ["ANG"])
            TS("dve", K, A, float(-np.pi), float(2 * np.pi), ALU.is_lt, ALU.mult, ["ANG"], ["KK"])
            TT("dve", A, A, K, ALU.add, ["ANG", "KK"], ["ANG"])
            P.op("dve", lambda e: e.tensor_scalar_min(out=A, in0=A, scalar1=3.1415925), ["ANG"], ["ANG"])
            P.op("dve", lambda e: e.tensor_scalar_max(out=A, in0=A, scalar1=-3.1415925), ["ANG"], ["ANG"])
            ACT(ROPE[:].rearrange("p c i f -> p (c i f)"), A, AF.Sin, ["ANG"], ["ROPE"])

        def phase_c(l, s, WIN, base):
            KTh = HT
            VT = arb(0, 16 * 4 * 192).rearrange("p (i m e) -> p i m e", i=16, m=4)
            RD = arf(6144, 512)
            CQG = arb(base + 4096, 2 * T).rearrange("p (k t) -> p k t", k=2)
            WUQ = arb(base + 6144, 2 * 768).rearrange("p (k n) -> p k n", k=2)
            WOC = arb(base + 6912, 4 * D).rearrange("p (m n) -> p m n", m=4)
            sm0 = base + 8960
            RS = arf(sm0, 64)
            T8 = arf(sm0 + 64, 64)
            TA = arf(sm0 + 128, 128)
            TB = arf(sm0 + 256, 128)
            QS = arf(sm0 + 384, 1024)
            QQ = arf(sm0 + 1408, 1024)
            QB = arb(sm0 + 2432, 1024)
            KB = QB
            R0 = sm0 + 2944
            assert R0 + 5376 <= ARW, R0
            CKG = arb(R0, T)
            CSQ = arb(R0 + 1024, 3 * T).rearrange("p (k t) -> p k t", k=3)
            WUK = arb(R0 + 4096, 1024)
            KR = arf(R0 + 4608, 512).rearrange("p (i n) -> p i n", i=16)
            QTh = arb(R0, 8 * 512).rearrange("p (h t) -> p h t", h=8)
            PT = arb(R0 + 2048, 3 * 512).rearrange("p (u t) -> p u t", u=3)
            YC = arb(R0 + 2816, 4 * 512).rearrange("p (m t) -> p m t", m=4)
            YSQ = arb(R0 + 3840, 4 * 512).rearrange("p (m t) -> p m t", m=4)
            YN = YSQ
            RBC = arf(R0 + 4864, 512)

            if l == 0:
                rope_tables(s, R0)
                P.fence()
            if CSTOP <= 0.1:
                return
            P.dma("pool", WUQ, wuq_d[l].rearrange("(k p) n -> p k n", p=128), writes=["WUQ"])
            P.dma("pool", WUK, wukv_d[l], writes=["WUK"])
            P.dma("pool", WOC, wout_d[l, 512:1024, :].rearrange("(m p) n -> p m n", p=128), writes=["WOC"])
            for kc in range(3):
                col = 1792 + kc * 128
                for tt in range(NT):
                    sl = slice(tt * 512, (tt + 1) * 512)
                    b = nb()
                    proj_fm(WIN, col, 128, tt, b)
                    ACT(CSQ[:, kc, sl], psb[b][:], AF.Square, ["ps%d" % b], ["CSQ"])
                    gcol = (PV_GQ + kc) if kc < 2 else PV_GKV
                    dst = CQG[:, kc, sl] if kc < 2 else CKG[:, sl]
                    P.op("dve", lambda e, dst=dst, b=b, gcol=gcol: e.tensor_scalar_mul(out=dst, in0=psb[b][:], scalar1=PV[:, l, gcol:gcol + 1]),
                         ["ps%d" % b, "PV"], ["CQG"])
            if CSTOP <= 0.2:
                return
            b = nb()
            for i in range(16):
                for kc in range(2):
                    MM(psb[b][:, i:i + 1], CSQ[:, kc, i * 128:(i + 1) * 128], ONESB[:, 0:1], kc == 0, kc == 1, ["CSQ", "ONESB"], ["ps%d" % b])
                MM(psb[b][:, 16 + i:17 + i], CSQ[:, 2, i * 128:(i + 1) * 128], ONESB[:, 0:1], True, True, ["CSQ", "ONESB"], ["ps%d" % b])
            rsqrt_bc(RS[:, 0:16], psb[b][:, 0:16], 1.0 / 256, ["ps%d" % b, "SM0"], ["RS"])
            rsqrt_bc(RS[:, 16:32], psb[b][:, 16:32], 1.0 / 128, ["ps%d" % b, "SM0"], ["RS"])
            if CSTOP <= 0.3:
                return
            b = nb()
            for i in range(16):
                for k in range(8):
                    MM(psb[b][:, i * 32:(i + 1) * 32], HT[:, k, i * 128:(i + 1) * 128], WIN[:, k, 2176:2208], k == 0, k == 7,
                       ["WIN", "HT%d" % k], ["ps%d" % b])
            CP("act", KR[:].rearrange("p i n -> p (i n)"), psb[b][:], ["ps%d" % b], ["KR"])
            P.fence()
            if CSTOP <= 1:
                return
            gq = PBQ[:, l, 0:96]
            gk = PBQ[:, l, 96:192]

            def rope_apply(dst3, src3, i, nh, rkeys, wkey):
                cos = fview(ROPE[:, 0, i, :], [(0, nh), (1, 16)])
                sin = fview(ROPE[:, 1, i, :], [(0, nh), (1, 16)])
                x1, x2 = src3[:, :, 0:16], src3[:, :, 16:32]
                a = TA[:, 0:nh * 16].rearrange("p (h n) -> p h n", h=nh)
                bq = TB[:, 0:nh * 16].rearrange("p (h n) -> p h n", h=nh)
                TT("dve", a, x1, cos, ALU.mult, rkeys + ["ROPE"], ["rpA"])
                TT("pool", bq, x2, sin, ALU.mult, rkeys + ["ROPE"], ["rpB"])
                TT("dve", dst3[:, :, 0:16], a, bq, ALU.subtract, ["rpA", "rpB"], [wkey])
                TT("dve", a, x2, cos, ALU.mult, rkeys + ["ROPE"], ["rpA"])
                TT("pool", bq, x1, sin, ALU.mult, rkeys + ["ROPE"], ["rpB"])
                TT("dve", dst3[:, :, 16:32], a, bq, ALU.add, ["rpA", "rpB"], [wkey])

            QBs = [QB, arb(base + 3584, 1024), arb(R0 + 2048, 768), arb(R0 + 2048 + 384, 768)]

            def nr_part1a(src_q3):
                SQv = QS[:, 0:768].rearrange("p (h n) -> p h n", h=8)
                TT("dve", SQv, src_q3, src_q3, ALU.mult, ["QQ"], ["QS"])
                P.op("dve", lambda e, SQv=SQv: e.tensor_reduce(out=T8[:, 0:8], in_=SQv, axis=AX.X, op=ALU.add), ["QS"], ["T8"])

            def nr_part1b(i, src_q3, g_bc, qi):
                ACT(T8[:, 0:8], T8[:, 0:8], AF.Ln, ["T8", "SM0"], ["T8"], bias=SM[:, 0:1], scale=1.0 / 96)
                ACT(T8[:, 0:8], T8[:, 0:8], AF.Exp, ["T8"], ["T8"], scale=-0.5)
                TT("dve", src_q3, src_q3, fview(T8[:, 0:8], [(1, 8), (0, 96)]), ALU.mult, ["QQ", "T8"], ["QQ"])
                TT("pool", src_q3, src_q3, fview(g_bc, [(0, 8), (1, 96)]), ALU.mult, ["QQ", "PBQ"], ["QQ"])
                Q3b = QBs[qi][:, 0:768].rearrange("p (h n) -> p h n", h=8)
                CP("dve", Q3b[:, :, 0:64], src_q3[:, :, 0:64], ["QQ"], ["QB%d" % qi])
                rope_apply(Q3b[:, :, 64:96], src_q3[:, :, 64:96], i, 8, ["QQ"], "QB%d" % qi)

            def nr_part1(i, src_q3, g_bc, qi):
                nr_part1a(src_q3)
                nr_part1b(i, src_q3, g_bc, qi)

            def nr_part2(qi, dstT, dcols, dkey):
                Q3b = QBs[qi][:, 0:768].rearrange("p (h n) -> p h n", h=8)
                for h in range(8):
                    bt = nb(0, 4)
                    pst = psb[bt][:].bitcast(BF16)
                    TR(pst[0:96, 0:128], Q3b[:, h, :], IDB[:], ["QB%d" % qi, "IDB"], ["ps%d" % bt])
                    CP("act" if h % 2 else "dve", dstT[0:96, h, dcols], pst[0:96, 0:128], ["ps%d" % bt], [dkey if dkey != "KTh" else "HT%d" % h])

            def k_part1(i):
                tsl = slice(i * 128, (i + 1) * 128)
                b0, b1 = nb(0, 4), nb(0, 4)
                for half, bnk in ((0, b0), (1, b1)):
                    MM(psb[bnk][:], CKG[:, tsl], WUK[:, half * 512:(half + 1) * 512], True, True, ["CQG", "WUK"], ["ps%d" % bnk])
                for half, bnk in ((0, b0), (1, b1)):
                    P.op("act", lambda e, half=half, bnk=bnk, i=i: e.activation(out=QS[:, half * 512:(half + 1) * 512], in_=psb[bnk][:],
                                                                               func=AF.Copy, scale=RS[:, 16 + i:17 + i]),
                         ["ps%d" % bnk, "RS"], ["QS"])
                KV = QS[:].rearrange("p (h n) -> p h n", h=8)
                CP("pool", VT[:, i, :, 0:64], fview(QS[:, 64:65], [(256, 4), (1, 64)]), ["QS"], ["VT"])
                CP("pool", VT[:, i, :, 128:192], fview(QS[:, 192:193], [(256, 4), (1, 64)]), ["QS"], ["VT"])
                K3 = QQ[:, 0:768].rearrange("p (h n) -> p h n", h=8)
                CP("dve", K3[:, :, 0:64], KV[:, :, 0:64], ["QS"], ["QQ"])
                CP("dve", K3[:, :, 64:96], fview(KR[:, i, :], [(0, 8), (1, 32)]), ["KR"], ["QQ"])
                nr_part1(i, K3, gk, i % 4)

            def prep_k():
                k_part1(0)
                for i in range(16):
                    if i + 1 < 16:
                        k_part1(i + 1)
                    nr_part2(i % 4, KTh, slice(i * 128, (i + 1) * 128), "KTh")

            def q_part1a(qt, i4):
                i = qt * 4 + i4
                tsl = slice(i * 128, (i + 1) * 128)
                b0, b1 = nb(0, 4), nb(0, 4)
                for kc in range(2):
                    MM(psb[b0][:], CQG[:, kc, tsl], WUQ[:, kc, 0:512], kc == 0, kc == 1, ["CQG", "WUQ"], ["ps%d" % b0])
                for kc in range(2):
                    MM(psb[b1][:, 0:256], CQG[:, kc, tsl], WUQ[:, kc, 512:768], kc == 0, kc == 1, ["CQG", "WUQ"], ["ps%d" % b1])
                P.op("dve", lambda e, i=i, b0=b0: e.tensor_scalar_mul(out=QQ[:, 0:512], in0=psb[b0][:], scalar1=RS[:, i:i + 1]),
                     ["ps%d" % b0, "RS"], ["QQ"])
                P.op("dve", lambda e, i=i, b1=b1: e.tensor_scalar_mul(out=QQ[:, 512:768], in0=psb[b1][:, 0:256], scalar1=RS[:, i:i + 1]),
                     ["ps%d" % b1, "RS"], ["QQ"])
                nr_part1a(QQ[:, 0:768].rearrange("p (h n) -> p h n", h=8))

            def q_part1b(qt, i4):
                nr_part1b(qt * 4 + i4, QQ[:, 0:768].rearrange("p (h n) -> p h n", h=8), gq, i4)

            def q_part1(qt, i4):
                q_part1a(qt, i4)
                q_part1b(qt, i4)

            def q_part2(QTdst, qkey, i4):
                nr_part2(i4, QTdst, slice(i4 * 128, (i4 + 1) * 128), qkey)

            def prep_q(qt, QTdst, qkey):
                for i4 in range(4):
                    q_part1(qt, i4)
                for i4 in range(4):
                    q_part2(QTdst, qkey, i4)

            MEMSET("pool", VT[:, :, :, 64:128], 1.0, ["VT"])
            prep_k()
            P.fence()
            if CSTOP <= 2:
                return
            scale = 96.0 ** -0.5
            NPT = 6
            PTB = arb(base, NPT * 512).rearrange("p (u t) -> p u t", u=NPT)
            QTh2 = [QTh, arb(base + NPT * 256, 8 * 512).rearrange("p (h t) -> p h t", h=8)]
            assert NPT * 256 + 2048 <= 4096
            LOOK = 3
            prep_q(0, QTh2[0], "QTh0")
            for qt in range(NT):
                QTc, qkey = QTh2[qt % 2], "QTh%d" % (qt % 2)
                steps = [(h, kt) for h in range(8) for kt in range(16)]
                sbank = {}

                def score(si):
                    h, kt = steps[si]
                    bs = nb(0, 4)
                    sbank[si] = bs
                    MM(psb[bs][:], KTh[0:96, h, kt * 128:(kt + 1) * 128], QTc[0:96, h, :], True, True, ["HT%d" % h, qkey], ["ps%d" % bs])
                    u = si % NPT
                    ACT(PTB[:, u, :], psb[bs][:], AF.Exp, ["ps%d" % bs], ["PT%d" % u], scale=scale)

                for si in range(min(LOOK, len(steps))):
                    score(si)
                for si, (h, kt) in enumerate(steps):
                    if si + LOOK < len(steps):
                        score(si + LOOK)
                    m, hb = h // 2, h % 2
                    bo = 4 + (h % 4)
                    ko = "ps%d" % bo
                    u = si % NPT
                    lw = VT[:, kt, m, 0:128] if hb == 0 else VT[:, kt, m, 64:192]
                    MM(psb[bo][:, :], lw, PTB[:, u, :], kt == 0, kt == 15, ["VT", "PT%d" % u], [ko])
                    if kt == 15:
                        pn, pd = 64 * hb, 64 * (1 - hb)
                        RECIP(RD[pd:pd + 64, :], psb[bo][pd:pd + 64, :], [ko], ["RD%d" % hb])
                        TT("dve", YC[pn:pn + 64, m, :], psb[bo][pn:pn + 64, :], RD[pd:pd + 64, :], ALU.mult, [ko, "RD%d" % hb], ["YC%d" % m])
                        if qt + 1 < NT:
                            if h < 4:
                                q_part1a(qt + 1, h)
                            else:
                                q_part2(QTh2[(qt + 1) % 2], "QTh%d" % ((qt + 1) % 2), h - 4)
                    if kt == 12 and 1 <= h <= 4 and qt + 1 < NT:
                        q_part1b(qt + 1, h - 1)
                for m in range(4):
                    ACT(YSQ[:, m, :], YC[:, m, :], AF.Square, ["YC%d" % m], ["YSQ%d" % m])
                b = nb(0, 4)
                for m in range(4):
                    MM(psb[b][:], ONESB[:], YSQ[:, m, :], m == 0, m == 3, ["ONESB", "YSQ%d" % m], ["ps%d" % b])
                rsqrt_bc(RBC[:], psb[b][:], 1.0 / 512, ["ps%d" % b, "SM0"], ["RBC"])
                for m in range(4):
                    STT("dve", YN[:, m, :], YC[:, m, :], PV[:, l, PV_GC + m:PV_GC + m + 1], RBC[:], ALU.mult, ALU.mult,
                        ["YC%d" % m, "PV", "RBC"], ["YSQ%d" % m])
                for dk in range(8):
                    b = nb(0, 4)
                    for m in range(4):
                        MM(psb[b][:], WOC[:, m, dk * 128:(dk + 1) * 128], YN[:, m, :], m == 0, m == 3, ["WOC", "YSQ%d" % m], ["ps%d" % b])
                    add_to_x(psb[b][:], dk, qt, ["ps%d" % b])

        def ffn(l):
            rmsnorm_to_HT(l, PV_GFFN, 16384)
            P.fence()
            moe = (l % 2 == 1)
            WS = [(arb(sl_ * 6144, 8 * 512).rearrange("p (k n) -> p k n", k=8),
                   arb(sl_ * 6144 + 2048, 8 * 512).rearrange("p (k n) -> p k n", k=8),
                   arb(sl_ * 6144 + 4096, 4 * D).rearrange("p (c n) -> p c n", c=4)) for sl_ in range(2)]
            o1 = 12288
            ACTB = [arb(o1 + a_ * 4096, 4 * T).rearrange("p (c t) -> p c t", c=4) for a_ in range(2)]
            o2 = o1 + 8192
            SG = arb(o2, 2 * 512).rearrange("p (u t) -> p u t", u=2)
            TU = arf(o2 + 512, 2 * 512).rearrange("p (u t) -> p u t", u=2)
            CBC = arb(o2 + 1536, 2 * T).rearrange("p (u t) -> p u t", u=2)
            o3 = o2 + 3584
            RB = arf(0, T)
            groups = []
            if not moe:
                nfull = D_FF // 512
                for g in range(nfull):
                    groups.append((None, g * 512, [128] * 4))
                groups.append((None, nfull * 512, [128, 64]))
            else:
                for e_ in range(NEXP):
                    for g in range(D_EXP // 512):
                        groups.append((e_, g * 512, [128] * 4))
            if moe:
                ot = 12288
                RG = arf(ot, 64).rearrange("p (k e) -> p k e", k=8)
                LG = arf(ot + 64, 128).rearrange("p (i e) -> p i e", i=16)
                MX = arf(ot + 192, 128).rearrange("p (i e) -> p i e", i=16)
                W12 = arf(ot + 320, 32).rearrange("p (a i) -> p a i", a=2)
                EQ = arf(ot + 352, 128).rearrange("p (i e) -> p i e", i=16)
                CMB = arf(ot + 480, 128).rearrange("p (i e) -> p i e", i=16)
                RSQ = arf(ot + 608, 16)
                SQB = arb(ot + 624, 128)
                CT = arf(o3, T)
                assert o3 + T <= ARW
                P.dma("sp", RG, rt_d[0].rearrange("(k p) e -> p k e", p=128), writes=["RG"])
                TT("dve", RG, RG, fview(PV[:, l, PV_GFFN:PV_GFFN + 8], [(1, 8), (0, 8)]), ALU.mult, ["RG", "PV"], ["RG"])
                bl = nb()
                for i in range(16):
                    for k in range(8):
                        MM(psb[bl][:, i * 8:(i + 1) * 8], X[:, k, i * 128:(i + 1) * 128], RG[:, k, :], k == 0, k == 7, ["X", "RG"], ["ps%d" % bl])
                br = nb()
                for i in range(16):
                    for k in range(8):
                        ACT(SQB[:], X[:, k, i * 128:(i + 1) * 128], AF.Square, ["X"], ["SQB"])
                        MM(psb[br][:, i:i + 1], SQB[:], ONESB[:, 0:1], k == 0, k == 7, ["SQB", "ONESB"], ["ps%d" % br])
                rsqrt_bc(RSQ[:], psb[br][:, 0:16], 1.0 / D, ["ps%d" % br, "SM0"], ["RSQ"])
                TT("dve", LG, psb[bl][:, 0:128].rearrange("p (i e) -> p i e", i=16), fview(RSQ, [(1, 16), (0, 8)]), ALU.mult,
                   ["ps%d" % bl, "RSQ"], ["LG"])
                for i in range(16):
                    P.op("dve", lambda e, i=i: e.max(out=MX[:, i, :], in_=LG[:, i, :]), ["LG"], ["MX"])
                TT("dve", W12[:, 0, :], MX[:, :, 0], MX[:, :, 1], ALU.subtract, ["MX"], ["W12"])
                TT("dve", W12[:, 1, :], MX[:, :, 1], MX[:, :, 0], ALU.subtract, ["MX"], ["W12"])
                ACT(W12[:].rearrange("p a i -> p (a i)"), W12[:].rearrange("p a i -> p (a i)"), AF.Sigmoid, ["W12"], ["W12"])
                TT("dve", EQ, LG, fview(MX[:, :, 0], [(8, 16), (0, 8)]), ALU.is_equal, ["LG", "MX"], ["EQ"])
                TT("dve", CMB, EQ, fview(W12[:, 0, :], [(1, 16), (0, 8)]), ALU.mult, ["EQ", "W12"], ["CMB"])
                TT("dve", EQ, LG, fview(MX[:, :, 1], [(8, 16), (0, 8)]), ALU.is_equal, ["LG", "MX", "CMB"], ["EQ"])
                TT("dve", EQ, EQ, fview(W12[:, 1, :], [(1, 16), (0, 8)]), ALU.mult, ["EQ", "W12"], ["EQ"])
                TT("dve", CMB, CMB, EQ, ALU.add, ["CMB", "EQ"], ["CMB"])
                for i in range(16):
                    bt = nb()
                    TR(psb[bt][0:8, 0:128], CMB[:, i, :], ident, ["CMB", "CONST"], ["ps%d" % bt])
                    CP("act", CT[0:8, i * 128:(i + 1) * 128], psb[bt][0:8, 0:128], ["ps%d" % bt], ["CT"])
            if moe:
                P.fence()
            rr = [0]

            def load13(gi):
                e_, c0, chunks = groups[gi]
                w1b, w3b, w2b = WS[gi % 2]
                n = sum(chunks)
                key = "WSA%d" % (gi % 2)
                if e_ is None:
                    P.dma("pool", w1b[:, :, 0:n], wgu_d[0, :, c0:c0 + n].rearrange("(k p) n -> p k n", p=128), writes=[key])
                    P.dma("pool", w3b[:, :, 0:n], wgu_d[0, :, D_FF + c0:D_FF + c0 + n].rearrange("(k p) n -> p k n", p=128), writes=[key])
                else:
                    P.dma("pool", w1b[:], w1_d[0, e_, :, c0:c0 + 512].rearrange("(k p) n -> p k n", p=128), writes=[key])
                    P.dma("pool", w3b[:], w3_d[0, e_, :, c0:c0 + 512].rearrange("(k p) n -> p k n", p=128), writes=[key])

            def load2(gi):
                e_, c0, chunks = groups[gi]
                w1b, w3b, w2b = WS[gi % 2]
                key = "WSB%d" % (gi % 2)
                if e_ is None:
                    r0 = c0
                    for ci, cs_ in enumerate(chunks):
                        if cs_ < 128:
                            MEMSET("pool", w2b[cs_:128, ci, :], 0.0, [key])
                            MEMSET("pool", ACTB[gi % 2][cs_:128, ci, :], 0.0, ["ACT%d" % (gi % 2)])
                        P.dma("pool", w2b[0:cs_, ci, :], wdn_d[0, r0:r0 + cs_, :], writes=[key])
                        r0 += cs_
                else:
                    P.dma("pool", w2b[:], w2_d[0, e_, c0:c0 + 512, :].rearrange("(c p) n -> p c n", p=128), writes=[key])

            def gu(gi):
                e_, c0, chunks = groups[gi]
                w1b, w3b, w2b = WS[gi % 2]
                key = "WSA%d" % (gi % 2)
                AB = ACTB[gi % 2]
                akey = "ACT%d" % (gi % 2)
                if e_ is not None and c0 == 0:
                    for tt in range(NT):
                        bt = nb(0, 4)
                        MM(psb[bt][:], fview(CONST[0:8, C_ID + e_:C_ID + e_ + 1], [(0, 128)]), CT[0:8, tt * 512:(tt + 1) * 512], True, True,
                           ["CONST", "CT"], ["ps%d" % bt])
                        CP("act", CBC[:, e_ % 2, tt * 512:(tt + 1) * 512], psb[bt][:], ["ps%d" % bt], ["CBC%d" % (e_ % 2)])
                off = 0
                for ci, cs_ in enumerate(chunks):
                    for tt in range(NT):
                        sl = slice(tt * 512, (tt + 1) * 512)
                        bg, bu = nb(0, 4), nb(0, 4)
                        for k in range(8):
                            MM(psb[bg][0:cs_, :], w1b[:, k, off:off + cs_], HT[:, k, sl], k == 0, k == 7, [key, "HT%d" % k], ["ps%d" % bg])
                        for k in range(8):
                            MM(psb[bu][0:cs_, :], w3b[:, k, off:off + cs_], HT[:, k, sl], k == 0, k == 7, [key, "HT%d" % k], ["ps%d" % bu])
                        u = rr[0] % 2
                        rr[0] += 1
                        ACT(SG[0:cs_, u, :], psb[bg][0:cs_, :], AF.Silu, ["ps%d" % bg], ["SG%d" % u])
                        if e_ is None:
                            TT("dve", AB[0:cs_, ci, sl], psb[bu][0:cs_, :], SG[0:cs_, u, :], ALU.mult, ["ps%d" % bu, "SG%d" % u], [akey])
                        else:
                            TT("dve", TU[:, u, :], psb[bu][:], CBC[:, e_ % 2, sl], ALU.mult, ["ps%d" % bu, "CBC%d" % (e_ % 2)], ["TU%d" % u])
                            TT("pool", AB[:, ci, sl], TU[:, u, :], SG[:, u, :], ALU.mult, ["TU%d" % u, "SG%d" % u], [akey])
                    off += cs_

            def down(gi):
                e_, c0, chunks = groups[gi]
                w1b, w3b, w2b = WS[gi % 2]
                key = "WSB%d" % (gi % 2)
                AB = ACTB[gi % 2]
                akey = "ACT%d" % (gi % 2)
                for dk in range(8):
                    for tt in range(NT):
                        b = nb(4, 8)
                        for ci, cs_ in enumerate(chunks):
                            MM(psb[b][:], w2b[:, ci, dk * 128:(dk + 1) * 128], AB[:, ci, tt * 512:(tt + 1) * 512],
                               ci == 0, ci == len(chunks) - 1, [key, akey], ["ps%d" % b])
                        add_to_x(psb[b][:], dk, tt, ["ps%d" % b])

            ng = len(groups)
            load13(0)
            load2(0)
            for gi in range(ng):
                if gi + 1 < ng:
                    load13(gi + 1)
                gu(gi)
                if gi >= 1:
                    down(gi - 1)
                if gi + 1 < ng:
                    load2(gi + 1)
            down(ng - 1)

        for s in range(nseq):
            load_x(s)
            P.fence()
            for l in range(2):
                if stop is not None and stop[1] == "norm":
                    rmsnorm_to_HT(l, PV_GMIX, 8832)
                    P.fence()
                    break
                mixer(l, s)
                if stop == (l, "mix"):
                    break
                ffn(l)
                P.fence()
                if stop == (l, "ffn"):
                    break
            store_x(s)
            P.fence()
        P.emit()
    return nc


def _consts():
    c = np.zeros((128, NCONST), np.float32)
    c[:, C_ID:C_ID + 128] = np.eye(128, dtype=np.float32)
    s = (np.arange(128) % 64)[:, None]
    t = np.arange(64)[None, :]
    c[:, C_MLOW:C_MLOW + 64] = (t >= s)
    c[:, C_MUP:C_MUP + 64] = (t <= s)
    bo = np.zeros((128, 128), np.float32)
    bo[:64, :64] = 1
    bo[64:, 64:] = 1
    c[:, C_BONES:C_BONES + 128] = bo
    inv = (10000.0 ** (-np.arange(0, 16, dtype=np.float32) * np.float32(2.0 / 32))).astype(np.float32)
    c[:, C_IFQ:C_IFQ + 16] = inv[None, :]
    return c


def _pack(inp):
    pv = np.zeros((2, 128, NPV), np.float32)
    pbq = np.zeros((2, 128, 192), np.float32)
    f = lambda a: np.asarray(a, np.float32)
    for l in range(2):
        pv[l, :, PV_GMIX:PV_GMIX + 8] = f(inp["norm_mix"][l]).reshape(8, 128).T
        pv[l, :, PV_GFFN:PV_GFFN + 8] = f(inp["norm_ffn"][l]).reshape(8, 128).T
        cw = f(inp["conv_w"][l])
        for j in range(2):
            pv[l, :, PV_CW + j * 4:PV_CW + j * 4 + 4] = cw[:, j * 128:(j + 1) * 128].T
            pv[l, :, PV_CB + j] = f(inp["conv_b"][l])[j * 128:(j + 1) * 128]
            pv[l, :, PV_GA + j] = f(inp["out_g_a"][l])[j * 128:(j + 1) * 128]
            pv[l, :, PV_GQ + j] = f(inp["mla_q_norm"][l])[j * 128:(j + 1) * 128]
            for d in range(2):
                pv[l, :, PV_BA + d * 2 + j] = f(inp["lru_ba"][l, d])[j * 128:(j + 1) * 128]
                pv[l, :, PV_BX + d * 2 + j] = f(inp["lru_bx"][l, d])[j * 128:(j + 1) * 128]
                pv[l, :, PV_LAM + d * 2 + j] = f(inp["lru_lam"][l, d])[j * 128:(j + 1) * 128]
                for ll in range(2):
                    pv[l, :, PV_LB + ll * 4 + d * 2 + j] = f(inp["hg_lb_logits"][ll, d])[j * 128:(j + 1) * 128]
        pv[l, :, PV_HGN] = np.tile(f(inp["hg_norm_g"][l]), 2)
        pv[l, :, PV_GKV] = f(inp["mla_kv_norm"][l])
        pv[l, :, PV_GC:PV_GC + 4] = f(inp["out_g_c"][l]).reshape(4, 128).T
        pbq[l, :, 0:96] = f(inp["qk_norm_q"][l])[None, :]
        pbq[l, :, 96:192] = f(inp["qk_norm_k"][l])[None, :]
    return pv, pbq


_NC_CACHE = {}


def kernel(**inp):
    ncores = 8
    nseq = 2
    key = (nseq, None)
    if key not in _NC_CACHE:
        _NC_CACHE[key] = build_nc(nseq, None)
    nc = _NC_CACHE[key]
    x = np.ascontiguousarray(np.asarray(inp["x"], np.float32))
    pos = np.asarray(inp["positions"], np.int32)
    pv, pbq = _pack(inp)
    consts = _consts()
    shared = {
        "consts": consts, "pv": pv, "pbq": pbq,
    }
    for nme in ("w_in", "lru_wa", "lru_wx", "mla_w_uq", "mla_w_ukv", "w_out", "ffn_w_gate_up", "ffn_w_down",
                "moe_router", "moe_w1", "moe_w3", "moe_w2"):
        shared[nme] = np.ascontiguousarray(np.asarray(inp[nme], np.float32))
    in_maps = []
    for c in range(ncores):
        m = dict(shared)
        m["x"] = x[c * nseq:(c + 1) * nseq]
        m["pos"] = np.ascontiguousarray(pos[c * nseq:(c + 1) * nseq].reshape(nseq, 16, 128).transpose(0, 2, 1))
        in_maps.append(m)
    res = run_bass_kernel_spmd(nc, in_maps, core_ids=list(range(ncores)))
    return np.concatenate([np.asarray(r["y"], np.float32) for r in res.results], axis=0)
```
